# Optimizing a Trainium2 kernel written in Bass

```python
import jax, jax.numpy as jnp
from jax import lax
import numpy as np

D_MODEL = 1024
BATCH = 8
SEQ = 4096
DEPTH = 2

CHUNK = 64
Q_BLOCK = 128
HEAD_DIM = 64
SB_HEADS = D_MODEL // 256
SB_WIDTH = SB_HEADS * HEAD_DIM
RET_HEADS = D_MODEL // 128
RET_WIDTH = RET_HEADS * HEAD_DIM
CONV_WIDTH = D_MODEL - SB_WIDTH - RET_WIDTH
CONV_KERNEL = 31
D_FF = 4 * D_MODEL
EPS = 1e-6
ROPE_BASE = 10000.0
IN_COLS = 3 * SB_WIDTH + 4 * RET_WIDTH + 2 * CONV_WIDTH

kernel_name = "hybrid_sb_retention_conformer_block"


def rms_norm(x, g):
    x32 = x.astype(jnp.float32)
    y = x32 * lax.rsqrt(jnp.mean(x32 * x32, axis=-1, keepdims=True) + EPS)
    return (y * g.astype(jnp.float32)).astype(x.dtype)


def layer_norm(x, g, b):
    x32 = x.astype(jnp.float32)
    mu = jnp.mean(x32, axis=-1, keepdims=True)
    xc = x32 - mu
    y = xc * lax.rsqrt(jnp.mean(xc * xc, axis=-1, keepdims=True) + EPS)
    return y * g.astype(jnp.float32) + b.astype(jnp.float32)


def to_heads(t, n_heads):
    B, T, _ = t.shape
    return t.reshape(B, T, n_heads, HEAD_DIM).transpose(0, 2, 1, 3)


def from_heads(t):
    B, H, T, d = t.shape
    return t.transpose(0, 2, 1, 3).reshape(B, T, H * d)


def stick_breaking_attention(q, k, v):
    B, H, T, d = q.shape
    nb = T // Q_BLOCK
    q_blocks = jnp.moveaxis(q.reshape(B, H, nb, Q_BLOCK, d), 2, 0)
    key_pos = jnp.arange(T)
    scale = d ** -0.5

    def one_block(args):
        q_blk, b_idx = args
        z = jnp.einsum('bhqd,bhkd->bhqk', q_blk, k) * scale
        q_pos = b_idx * Q_BLOCK + jnp.arange(Q_BLOCK)
        mask = key_pos[None, :] < q_pos[:, None]
        log_beta = jax.nn.log_sigmoid(z)
        log_keep = jnp.where(mask, jax.nn.log_sigmoid(-z), 0.0)
        shifted = jnp.concatenate([log_keep[..., 1:], jnp.zeros_like(log_keep[..., :1])], axis=-1)
        tail = lax.cumsum(shifted, axis=3, reverse=True)
        w = jnp.where(mask, jnp.exp(log_beta + tail), 0.0)
        return jnp.einsum('bhqk,bhkd->bhqd', w, v)

    out = lax.map(one_block, (q_blocks, jnp.arange(nb)))
    return jnp.moveaxis(out, 0, 2).reshape(B, H, T, d)


def retention_rotary(x, pos):
    d = x.shape[-1]
    inv = 1.0 / (ROPE_BASE ** jnp.linspace(0.0, 1.0, d // 2, dtype=jnp.float32))
    ang = pos.astype(jnp.float32)[:, None] * inv[None, :]
    cos, sin = jnp.cos(ang), jnp.sin(ang)
    x1, x2 = x[..., 0::2], x[..., 1::2]
    return jnp.stack([x1 * cos - x2 * sin, x1 * sin + x2 * cos], axis=-1).reshape(x.shape)


def multiscale_retention(q, k, v):
    B, H, T, d = q.shape
    C = CHUNK
    nc = T // C
    log_g = jnp.log(1.0 - jnp.exp2(-5.0 - jnp.arange(H, dtype=jnp.float32)))
    j = jnp.arange(C, dtype=jnp.float32)
    rel = j[:, None] - j[None, :]
    intra_decay = jnp.where(rel >= 0, jnp.exp(log_g[:, None, None] * jnp.maximum(rel, 0.0)), 0.0)

    qc = q.reshape(B, H, nc, C, d)
    kc = k.reshape(B, H, nc, C, d)
    vc = v.reshape(B, H, nc, C, d)

    scores = jnp.einsum('bhncd,bhnsd->bhncs', qc, kc) * intra_decay[None, :, None]
    out_intra = jnp.einsum('bhncs,bhnsd->bhncd', scores, vc)

    k_dec = kc * jnp.exp(log_g[:, None] * (C - 1.0 - j)[None, :])[None, :, None, :, None]
    chunk_kv = jnp.einsum('bhncd,bhnce->bhnde', k_dec, vc)
    chunk_decay = jnp.exp(log_g * C)[None, :, None, None]

    def step(state, kv):
        return state * chunk_decay + kv, state

    _, prev_states = lax.scan(step, jnp.zeros((B, H, d, d), jnp.float32), jnp.moveaxis(chunk_kv, 2, 0))
    prev_states = jnp.moveaxis(prev_states, 0, 2)

    q_dec = qc * jnp.exp(log_g[:, None] * (j + 1.0)[None, :])[None, :, None, :, None]
    out_inter = jnp.einsum('bhncd,bhnde->bhnce', q_dec, prev_states)
    return (out_intra + out_inter).reshape(B, H, T, d)


def conformer_conv(a, gate, pw_b, dw_w, dw_b, ln_g, ln_b):
    c = a.shape[-1]
    pw_b = pw_b.astype(jnp.float32)
    h = (a.astype(jnp.float32) + pw_b[:c]) * jax.nn.sigmoid(gate.astype(jnp.float32) + pw_b[c:])
    h = lax.conv_general_dilated(
        h, dw_w.astype(jnp.float32)[:, None, :], window_strides=(1,),
        padding=[(CONV_KERNEL - 1, 0)], dimension_numbers=('NWC', 'WIO', 'NWC'),
        feature_group_count=c) + dw_b.astype(jnp.float32)
    return jax.nn.silu(layer_norm(h, ln_g, ln_b))


def setup_inputs(seed: int = 0) -> dict:
    key = jax.random.key(seed)
    ks = jax.random.split(key, 16)
    f32 = jnp.float32
    nrm = lambda k, shape, s: jax.random.normal(k, shape, f32) * s
    return {
        "x": nrm(ks[0], (BATCH, SEQ, D_MODEL), 1.0),
        "mix_norm_g": 1.0 + nrm(ks[1], (DEPTH, D_MODEL), 0.02),
        "w_in": nrm(ks[2], (DEPTH, D_MODEL, IN_COLS), D_MODEL ** -0.5),
        "sb_q_norm_g": 1.0 + nrm(ks[3], (DEPTH, HEAD_DIM), 0.02),
        "sb_k_norm_g": 1.0 + nrm(ks[4], (DEPTH, HEAD_DIM), 0.02),
        "ret_norm_g": 1.0 + nrm(ks[5], (DEPTH, RET_WIDTH), 0.02),
        "conv_pw_b": nrm(ks[6], (DEPTH, 2 * CONV_WIDTH), 0.02),
        "conv_dw_w": nrm(ks[7], (DEPTH, CONV_KERNEL, CONV_WIDTH), CONV_KERNEL ** -0.5),
        "conv_dw_b": nrm(ks[8], (DEPTH, CONV_WIDTH), 0.02),
        "conv_ln_g": 1.0 + nrm(ks[9], (DEPTH, CONV_WIDTH), 0.02),
        "conv_ln_b": nrm(ks[10], (DEPTH, CONV_WIDTH), 0.02),
        "w_out": nrm(ks[11], (DEPTH, D_MODEL, D_MODEL), D_MODEL ** -0.5),
        "mlp_norm_g": 1.0 + nrm(ks[12], (DEPTH, D_MODEL), 0.02),
        "w_ff1": nrm(ks[13], (DEPTH, D_MODEL, D_FF), D_MODEL ** -0.5),
        "w_ff2": nrm(ks[14], (DEPTH, D_FF, D_MODEL), D_FF ** -0.5),
    }


def reference(x, mix_norm_g, w_in, sb_q_norm_g, sb_k_norm_g, ret_norm_g, conv_pw_b,
              conv_dw_w, conv_dw_b, conv_ln_g, conv_ln_b, w_out, mlp_norm_g, w_ff1, w_ff2):
    B, T, _ = x.shape
    pos = jnp.arange(T)
    split_at = np.cumsum([SB_WIDTH] * 3 + [RET_WIDTH] * 4 + [CONV_WIDTH])
    for l in range(DEPTH):
        h = rms_norm(x, mix_norm_g[l])
        u = jnp.einsum('btd,dc->btc', h, w_in[l])
        sb_q, sb_k, sb_v, r_q, r_k, r_v, r_g, c_a, c_gate = jnp.split(u, split_at, axis=-1)

        q = to_heads(rms_norm(sb_q.reshape(B, T, SB_HEADS, HEAD_DIM), sb_q_norm_g[l]).reshape(B, T, SB_WIDTH), SB_HEADS)
        k = to_heads(rms_norm(sb_k.reshape(B, T, SB_HEADS, HEAD_DIM), sb_k_norm_g[l]).reshape(B, T, SB_WIDTH), SB_HEADS)
        v = to_heads(sb_v, SB_HEADS)
        y_sb = from_heads(stick_breaking_attention(q.astype(jnp.float32), k.astype(jnp.float32),
                                                   v.astype(jnp.float32)))

        rq = retention_rotary(to_heads(r_q, RET_HEADS).astype(jnp.float32), pos)
        rk = retention_rotary(to_heads(r_k, RET_HEADS).astype(jnp.float32), pos) * (HEAD_DIM ** -0.5)
        rv = to_heads(r_v, RET_HEADS).astype(jnp.float32)
        ret = from_heads(multiscale_retention(rq, rk, rv)).reshape(B, T, RET_HEADS, HEAD_DIM)
        ret = rms_norm(ret, ret_norm_g[l].reshape(RET_HEADS, HEAD_DIM)).reshape(B, T, RET_WIDTH)
        y_ret = jax.nn.silu(r_g.astype(jnp.float32)) * ret

        y_conv = conformer_conv(c_a, c_gate, conv_pw_b[l], conv_dw_w[l], conv_dw_b[l],
                                conv_ln_g[l], conv_ln_b[l])

        mixed = jnp.concatenate([y_sb, y_ret, y_conv], axis=-1).astype(x.dtype)
        x = x + jnp.einsum('btc,cd->btd', mixed, w_out[l])

        h = rms_norm(x, mlp_norm_g[l])
        f = jnp.square(jax.nn.relu(jnp.einsum('btd,df->btf', h, w_ff1[l])))
        x = x + jnp.einsum('btf,fd->btd', f, w_ff2[l])
    return x
```

```python
import numpy as np
import concourse.bass as bass
import concourse.mybir as mybir
from concourse.bass_utils import run_bass_kernel_spmd

F32 = mybir.dt.float32
BF16 = mybir.dt.bfloat16
AF = mybir.ActivationFunctionType
ALU = mybir.AluOpType
AX = mybir.AxisListType

T = 4096
D = 1024
NTB = 32
DFF = 4096
DEPTH = 2
INC = 3328
EPS = 1e-6
NEG = -30000.0
RET_STOP = 99
CONV_STOP = 99


class Trk:
    __slots__ = ("name", "lw", "rd", "dsem")

    def __init__(self, name):
        self.name = name
        self.lw = {}
        self.rd = {}
        self.dsem = None


class Eng:
    def __init__(self, name, h, sem):
        self.name = name
        self.h = h
        self.sem = sem
        self.count = 0
        self.seen = {}


class FW:
    def __init__(self, needed=None):
        self.needed = None
        if needed is not None:
            self.needed = {k: sorted(v) for k, v in needed.items()}
            self.needed_set = {k: set(v) for k, v in needed.items()}
        self.waited = {}
        self.nc = bass.Bass("TRN2", target_bir_lowering=False)
        nc = self.nc
        self._stack = []
        self.engs = {}
        for name, h in (("pe", nc.tensor), ("act", nc.scalar), ("dve", nc.vector),
                        ("pool", nc.gpsimd), ("sp", nc.sync)):
            sem = self.enter(nc.semaphore("sem_" + name))
            self.engs[name] = Eng(name, h, sem)
        self.dsem_tot = {}
        self.free_dsems = []
        self.free_dsems_sw = []
        self._scope_dsems = []
        self._persist = []
        self.nwait = 0
        self.ninc = 0
        self.ninst = 0
        self._uid = 0

    def enter(self, cm):
        v = cm.__enter__()
        self._stack.append(cm)
        return v

    def uid(self, p="t"):
        self._uid += 1
        return "%s%d" % (p, self._uid)

    def sb(self, shape, dt, name=None):
        return self.enter(self.nc.sbuf_tensor(name or self.uid("sb"), list(shape), dt))

    def ps(self, shape, dt, name=None):
        return self.enter(self.nc.psum_tensor(name or self.uid("ps"), list(shape), dt))

    def trk(self, name=None):
        return Trk(name or self.uid("k"))

    def mark(self):
        self._scope_dsems.append([])
        return len(self._stack)

    def release(self, mark):
        self.barrier()
        while len(self._stack) > mark:
            cm = self._stack.pop()
            cm.__exit__(None, None, None)
        for is_sw, sem in self._scope_dsems.pop():
            (self.free_dsems_sw if is_sw else self.free_dsems).append(sem)

    def _emit_wait(self, eng, s, k, v):
        if k in self.dsem_tot:
            eng.h.wait_ge(s, v)
        else:
            self.waited.setdefault(k, set()).add(v)
            if self.needed is None:
                eng.h.wait_ge(s, v)
            else:
                import bisect
                lst = self.needed[k]
                r = bisect.bisect_left(lst, v)
                assert r < len(lst) and lst[r] == v, (k, v)
                eng.h.wait_ge(s, r + 1)
        self.nwait += 1

    def _deps(self, reads, writes):
        deps = {}

        def add(d):
            for k, (s, v) in d.items():
                if k not in deps or deps[k][1] < v:
                    deps[k] = (s, v)
        for r in reads:
            add(r.lw)
        for w in writes:
            add(w.lw)
            add(w.rd)
        return deps

    def _wait(self, eng, deps):
        for k, (s, v) in deps.items():
            if k in self.dsem_tot:
                v = self.dsem_tot[k][1]
            if eng.name == "pe" and k == eng.sem.name:
                continue
            if eng.seen.get(k, 0) >= v:
                continue
            self._emit_wait(eng, s, k, v)
            eng.seen[k] = v

    def op(self, ename, fn, reads=(), writes=()):
        eng = self.engs[ename]
        self._wait(eng, self._deps(reads, writes))
        inst = fn(eng.h)
        eng.count += 1
        if self.needed is None or eng.count in self.needed_set.get(eng.sem.name, ()):
            inst.then_inc(eng.sem, 1)
            self.ninc += 1
        ev = (eng.sem, eng.count)
        k = eng.sem.name
        for w in writes:
            w.lw = {k: ev}
            w.rd = {}
        for r in reads:
            r.rd[k] = ev
        self.ninst += 1
        return inst

    def dma(self, qname, out, in_, reads=(), writes=(), slot=None, **kw):
        eng = self.engs[qname]
        self._wait(eng, self._deps(reads, writes))
        if slot.dsem is None:
            pool_ = self.free_dsems_sw if qname == "pool" else self.free_dsems
            if pool_:
                slot.dsem = pool_.pop()
            else:
                cm = self.nc.semaphore(self.uid("dsem"))
                slot.dsem = cm.__enter__()
                self._persist.append(cm)
                self.dsem_tot[slot.dsem.name] = (slot.dsem, 0)
            if self._scope_dsems:
                self._scope_dsems[-1].append((qname == "pool", slot.dsem))
        inst = eng.h.dma_start(out=out, in_=in_, **kw)
        tot = self.dsem_tot[slot.dsem.name][1] + 16
        self.dsem_tot[slot.dsem.name] = (slot.dsem, tot)
        inst.then_inc(slot.dsem, 16)
        ev = (slot.dsem, tot)
        k = slot.dsem.name
        for w in writes:
            w.lw = {k: ev}
            w.rd = {}
        for r in reads:
            r.rd[k] = ev
        self.ninst += 1
        return inst

    def barrier(self):
        for eng in self.engs.values():
            for e in self.engs.values():
                if e is eng or e.count == 0:
                    continue
                if eng.seen.get(e.sem.name, 0) < e.count:
                    self._emit_wait(eng, e.sem, e.sem.name, e.count)
                    eng.seen[e.sem.name] = e.count
            for k, (s, tot) in self.dsem_tot.items():
                if tot and eng.seen.get(k, 0) < tot:
                    self._emit_wait(eng, s, k, tot)
                    eng.seen[k] = tot


def _consts():
    c = {}
    i = np.arange(128)
    c["c_ident"] = np.eye(128, dtype=np.float32)
    c["c_blk"] = (i[:, None] // 64 == i[None, :] // 64).astype(np.float32)
    c["c_negtri"] = -(i[:, None] >= i[None, :]).astype(np.float32)
    c["c_negones"] = -np.ones((128, 128), np.float32)
    c["c_maskneg"] = np.where(i[:, None] >= i[None, :], NEG, 0.0).astype(np.float32)
    c["c_ones"] = np.full((128, 128), 1.0 / 256.0, np.float32)
    inv = (1.0 / (10000.0 ** np.linspace(0.0, 1.0, 32, dtype=np.float32))).astype(np.float32)
    pos = np.arange(T, dtype=np.float32)
    ang = (pos[:, None] * inv[None, :]).astype(np.float32)
    cos = np.cos(ang).astype(np.float32)
    sin = np.sin(ang).astype(np.float32)
    cos2 = np.repeat(cos, 2, axis=1)
    s2 = np.stack([-sin, sin], axis=-1).reshape(T, 64)
    rot = np.concatenate([cos2, s2, cos2 * 0.125, s2 * 0.125], axis=1).astype(np.float32)
    c["c_rot"] = rot.reshape(NTB, 128, 256)
    h = np.arange(8, dtype=np.float32)
    log_g = np.log(1.0 - np.exp2(-5.0 - h)).astype(np.float32)
    j = np.arange(128, dtype=np.float32)
    rel = j[None, :] - j[:, None]
    dt_ = np.where(rel[None] >= 0, np.exp(log_g[:, None, None] * np.maximum(rel[None], 0.0)), 0.0)
    c["c_dt"] = np.ascontiguousarray(dt_.transpose(1, 0, 2)).astype(np.float32)
    qd = np.exp(log_g[:, None] * (j + 1.0)[None, :]).astype(np.float32)
    qdec = np.zeros((128, 4, 128), np.float32)
    g128 = np.zeros((128, 4, 64), np.float32)
    gC = np.exp(log_g * 128.0).astype(np.float32)
    for p in range(128):
        for pr in range(4):
            hh = pr * 2 + p // 64
            qdec[p, pr, :] = qd[hh]
            g128[p, pr, :] = gC[hh]
    c["c_qdec"] = qdec
    c["c_g128"] = g128
    kd = np.exp(log_g[:, None] * (127.0 - j)[None, :]).astype(np.float32)
    c["c_kdec"] = np.ascontiguousarray(np.repeat(kd.T[:, :, None], 64, axis=2)).astype(np.float32)
    return c


_CONSTS = None
_NC_CACHE = {}


def build(nlayers=DEPTH, dbg=None, skip=(), dump_chunks=range(8), needed=None):
    fw = FW(needed)
    nc = fw.nc

    def din(name, shape):
        shape = list(shape)
        if shape[0] == DEPTH and not name.startswith("c_"):
            shape[0] = nlayers
        return nc.dram_tensor(name, shape, F32, kind="ExternalInput").ap()

    x_in = din("x", [T, D])
    mix_g = din("mix_norm_g", [DEPTH, D])
    w_in = din("w_in", [DEPTH, D, INC])
    sbq_g = din("sb_q_norm_g", [DEPTH, 64])
    sbk_g = din("sb_k_norm_g", [DEPTH, 64])
    ret_g = din("ret_norm_g", [DEPTH, 512])
    cpw_b = din("conv_pw_b", [DEPTH, 512])
    cdw_w = din("conv_dw_w", [DEPTH, 31, 256])
    cdw_b = din("conv_dw_b", [DEPTH, 256])
    cln_g = din("conv_ln_g", [DEPTH, 256])
    cln_b = din("conv_ln_b", [DEPTH, 256])
    w_out = din("w_out", [DEPTH, D, D])
    mlp_g = din("mlp_norm_g", [DEPTH, D])
    w_ff1 = din("w_ff1", [DEPTH, D, DFF])
    w_ff2 = din("w_ff2", [DEPTH, DFF, D])
    c_ident = din("c_ident", [128, 128])
    c_blk = din("c_blk", [128, 128])
    c_negtri = din("c_negtri", [128, 128])
    c_negones = din("c_negones", [128, 128])
    c_maskneg = din("c_maskneg", [128, 128])
    c_ones = din("c_ones", [128, 128])
    c_rot = din("c_rot", [NTB, 128, 256])
    c_dt = din("c_dt", [128, 8, 128])
    c_qdec = din("c_qdec", [128, 4, 128])
    c_g128 = din("c_g128", [128, 4, 64])
    c_kdec = din("c_kdec", [128, 8, 64])
    y_out = nc.dram_tensor("y", [T, D], F32, kind="ExternalOutput").ap()
    s0 = nc.dram_tensor("scr0", [T, D], F32, kind="Internal").ap()
    s1 = nc.dram_tensor("scr1", [T, D], F32, kind="Internal").ap()
    dbg_out = None
    if dbg is not None:
        dbg_out = nc.dram_tensor("dbg", [128, 8, T], F32, kind="ExternalOutput").ap()

    k_dram = {"x": fw.trk("dx"), "s0": fw.trk("ds0"), "s1": fw.trk("ds1"), "y": fw.trk("dy"), "dbg": fw.trk("ddbg")}

    R1 = fw.sb([128, 8, T], BF16, "R1")
    R2 = fw.sb([128, 8, T], BF16, "R2")
    k_hT = [fw.trk("hT%d" % i) for i in range(NTB)]
    k_mx = [[fw.trk("mx%d_%d" % (c, i)) for i in range(8)] for c in range(8)]
    identb = fw.sb([128, 128], BF16, "identb"); k_identb = fw.trk("identb")
    identf = fw.sb([128, 128], F32, "identf"); k_identf = fw.trk("identf")
    blkb = fw.sb([128, 128], BF16, "blkb"); k_blkb = fw.trk("blkb")
    negtri = fw.sb([128, 128], BF16, "negtri"); k_negtri = fw.trk("negtri")
    negones = fw.sb([128, 128], BF16, "negones"); k_negones = fw.trk("negones")
    maskneg = fw.sb([128, 128], BF16, "maskneg"); k_maskneg = fw.trk("maskneg")
    onesf = fw.sb([128, 128], BF16, "onesf"); k_onesf = fw.trk("onesf")
    zerob = fw.sb([128, 128], BF16, "zerob"); k_zerob = fw.trk("zerob")
    fw.op("pool", lambda e: e.memset(zerob[:], 0.0), writes=[k_zerob])
    fw.dma("pool", identb[:], c_ident[:, :], writes=[k_identb], slot=k_identb)
    fw.dma("sp", identf[:], c_ident[:, :], writes=[k_identf], slot=k_identf)
    fw.dma("pool", blkb[:], c_blk[:, :], writes=[k_blkb], slot=k_blkb)
    fw.dma("pool", negtri[:], c_negtri[:, :], writes=[k_negtri], slot=k_negtri)
    fw.dma("pool", negones[:], c_negones[:, :], writes=[k_negones], slot=k_negones)
    fw.dma("pool", maskneg[:], c_maskneg[:, :], writes=[k_maskneg], slot=k_maskneg)
    fw.dma("pool", onesf[:], c_ones[:, :], writes=[k_onesf], slot=k_onesf)

    def load_cols(dst, k_dst, src_row_ap, n):
        with nc.allow_non_contiguous_dma(reason="tiny param vector"):
            fw.dma("sp", dst, src_row_ap.rearrange("(c p) -> p c", p=128), writes=[k_dst], slot=k_dst)

    def rstd_from(ename_unused, out_ap, in_ap, scale, reads, writes):
        fw.op("act", lambda e: e.activation(out=out_ap, in_=in_ap, func=AF.Ln, scale=scale, bias=EPS),
              reads=reads, writes=writes)
        fw.op("act", lambda e: e.activation(out=out_ap, in_=out_ap, func=AF.Exp, scale=-0.5),
              reads=writes, writes=writes)

    def sigmoid_from(out_ap, in_ap, reads, writes, nbias=None):
        if nbias is None:
            fw.op("act", lambda e: e.activation(out=out_ap, in_=in_ap, func=AF.Exp, scale=-1.0), reads=reads, writes=writes)
        else:
            fw.op("act", lambda e: e.activation(out=out_ap, in_=in_ap, func=AF.Exp, scale=-1.0, bias=nbias), reads=reads, writes=writes)
        fw.op("act", lambda e: e.activation(out=out_ap, in_=out_ap, func=AF.Ln, bias=1.0), reads=writes, writes=writes)
        fw.op("act", lambda e: e.activation(out=out_ap, in_=out_ap, func=AF.Exp, scale=-1.0), reads=writes, writes=writes)

    def norm_transpose_block(xt, k_xt, gt, k_gt, res, dst_ap, k_dst):
        junk, k_junk, st, k_st, hb, k_hb, pt, k_pt = res
        fw.op("act", lambda e: e.activation(out=junk[:], in_=xt, func=AF.Square, accum_out=st[:, 0:1]),
              reads=[k_xt], writes=[k_junk, k_st])
        rstd_from("act", st[:, 1:2], st[:, 0:1], 1.0 / D, [k_st], [k_st])
        fw.op("dve", lambda e: e.tensor_scalar(out=hb[:], in0=xt, scalar1=st[:, 1:2], scalar2=None, op0=ALU.mult),
              reads=[k_xt, k_st], writes=[k_hb])
        for dc in range(8):
            fw.op("pe", lambda e, dc=dc: e.transpose(out=pt[:, dc, :], in_=hb[:, dc * 128:(dc + 1) * 128], identity=identb[:]),
                  reads=[k_hb, k_identb], writes=[k_pt])
        fw.op("dve", lambda e: e.tensor_tensor(out=dst_ap, in0=pt[:], in1=gt[:].unsqueeze(2).broadcast_to([128, 8, 128]), op=ALU.mult),
              reads=[k_pt, k_gt], writes=[k_dst])

    def phase_A(l, xin, k_xin):
        m = fw.mark()
        gt = fw.sb([128, 8], F32); k_gt = fw.trk()
        load_cols(gt[:], k_gt, mix_g[l, :], 8)
        nb = 3
        xts = [(fw.sb([128, D], F32), fw.trk()) for _ in range(nb)]
        ress = []
        for _ in range(2):
            ress.append((fw.sb([128, D], BF16), fw.trk(), fw.sb([128, 2], F32), fw.trk(),
                         fw.sb([128, D], BF16), fw.trk(), fw.ps([128, 8, 128], BF16), fw.trk()))
        for tb in range(NTB):
            xt, k_xt = xts[tb % nb]
            fw.dma("sp", xt[:], xin[tb * 128:(tb + 1) * 128, :], reads=[k_xin], writes=[k_xt], slot=k_xt)
            norm_transpose_block(xt[:], k_xt, gt, k_gt, ress[tb % 2], R1[:, :, tb * 128:(tb + 1) * 128], k_hT[tb])
        fw.release(m)

    def phase_SB(l, hp):
        m = fw.mark()
        qT = fw.sb([128, T], BF16); kT = fw.sb([128, 2, T], BF16); vv = fw.sb([128, NTB, 128], BF16)
        k_kT0 = fw.trk()
        fw.op("pool", lambda e: e.memset(kT[:], 0.0), writes=[k_kT0])
        k_qT = [fw.trk() for _ in range(8)]; k_kT = [fw.trk() for _ in range(8)]; k_v = [fw.trk() for _ in range(8)]
        gq = fw.sb([128, 1], F32); k_gq = fw.trk(); gk = fw.sb([128, 1], F32); k_gk = fw.trk()
        with nc.allow_non_contiguous_dma(reason="tiny param vector"):
            for half in range(2):
                fw.dma("sp", gq[half * 64:(half + 1) * 64, :], sbq_g[l, :].rearrange("(p o) -> p o", o=1), writes=[k_gq], slot=k_gq)
                fw.dma("sp", gk[half * 64:(half + 1) * 64, :], sbk_g[l, :].rearrange("(p o) -> p o", o=1), writes=[k_gk], slot=k_gk)
        fw.op("dve", lambda e: e.tensor_scalar(out=gq[:], in0=gq[:], scalar1=0.125, scalar2=None, op0=ALU.mult), reads=[k_gq], writes=[k_gq])
        wts = [(fw.sb([128, 8, 128], BF16), fw.trk()) for _ in range(2)]
        pbank = [(fw.ps([128, 512], F32), fw.trk()) for _ in range(2)]
        pss = [(fw.ps([128, 512], F32), fw.trk()) for _ in range(2)]
        sqb = [(fw.sb([128, 512], BF16), fw.trk()) for _ in range(2)]
        rsd = [(fw.sb([128, 512], F32), fw.trk()) for _ in range(2)]
        it = 0
        for which in range(2):
            col0 = (0 if which == 0 else 256) + hp * 128
            wt, k_wt = wts[which]
            fw.dma("pool", wt[:], w_in[l, :, col0:col0 + 128].rearrange("(dc p) c -> p dc c", p=128), writes=[k_wt], slot=k_wt)
            dst = qT if which == 0 else kT
            kd = k_qT if which == 0 else k_kT
            gcol, k_gcol = (gq, k_gq) if which == 0 else (gk, k_gk)
            for tt in range(8):
                pq, k_pq = pbank[it % 2]; ps2, k_ps2 = pss[it % 2]; sq, k_sq = sqb[it % 2]; rs, k_rs = rsd[it % 2]
                it += 1
                for dc in range(8):
                    fw.op("pe", lambda e, dc=dc: e.matmul(pq[:], lhsT=wt[:, dc, :], rhs=R1[:, dc, tt * 512:(tt + 1) * 512], start=(dc == 0), stop=(dc == 7)),
                          reads=[k_wt] + k_hT[tt * 4:(tt + 1) * 4], writes=[k_pq])
                fw.op("act", lambda e: e.activation(out=sq[:], in_=pq[:], func=AF.Square), reads=[k_pq], writes=[k_sq])
                fw.op("pe", lambda e: e.matmul(ps2[:], lhsT=blkb[:], rhs=sq[:], start=True, stop=True), reads=[k_blkb, k_sq], writes=[k_ps2])
                rstd_from("act", rs[:], ps2[:], 1.0 / 64, [k_ps2], [k_rs])
                if which == 0:
                    fw.op("dve", lambda e: e.scalar_tensor_tensor(out=dst[:, tt * 512:(tt + 1) * 512], in0=pq[:], scalar=gcol[:, 0:1], in1=rs[:], op0=ALU.mult, op1=ALU.mult),
                          reads=[k_pq, k_gcol, k_rs], writes=[kd[tt]])
                else:
                    for hh_ in range(2):
                        fw.op("dve", lambda e, hh_=hh_: e.scalar_tensor_tensor(out=kT[hh_ * 64:(hh_ + 1) * 64, hh_, tt * 512:(tt + 1) * 512], in0=pq[hh_ * 64:(hh_ + 1) * 64, :],
                                                                              scalar=gcol[hh_ * 64:(hh_ + 1) * 64, 0:1], in1=rs[hh_ * 64:(hh_ + 1) * 64, :], op0=ALU.mult, op1=ALU.mult),
                              reads=[k_pq, k_gcol, k_rs, k_kT0], writes=[kd[tt]])
        wt, k_wt = wts[0]
        col0 = 512 + hp * 128
        fw.dma("pool", wt[:], w_in[l, :, col0:col0 + 128].rearrange("(dc p) c -> p dc c", p=128), writes=[k_wt], slot=k_wt)
        for tb in range(NTB):
            pq, k_pq = pbank[tb % 2]
            for dc in range(8):
                fw.op("pe", lambda e, dc=dc: e.matmul(pq[:, 0:128], lhsT=R1[:, dc, tb * 128:(tb + 1) * 128], rhs=wt[:, dc, :], start=(dc == 0), stop=(dc == 7)),
                      reads=[k_wt, k_hT[tb]], writes=[k_pq])
            fw.op("act", lambda e: e.activation(out=vv[:, tb, :], in_=pq[:, 0:128], func=AF.Copy), reads=[k_pq], writes=[k_v[tb // 4]])

        if dbg == 'sbproj':
            fw.release(m)
            return
        pz = [(fw.ps([128, 512], F32), fw.trk()) for _ in range(2)]
        plw = [(fw.ps([128, 512], F32), fw.trk()) for _ in range(2)]
        Eb = [(fw.sb([128, 512], F32), fw.trk()) for _ in range(2)]
        Lb = [(fw.sb([128, 512], BF16), fw.trk()) for _ in range(3)]
        Rb = fw.sb([128, 3, 512], BF16); k_R = [fw.trk() for _ in range(3)]
        wb = [(fw.sb([128, 512], BF16), fw.trk()) for _ in range(2)]
        Xb = [(fw.sb([128, 512], F32), fw.trk()) for _ in range(2)]
        for hh in range(2):
            p0 = hh * 64
            for qt in range(8):
                po, k_po = pbank[(hh * 8 + qt) % 2]
                fw.op("pool", lambda e: e.memset(Rb[:], 0.0), writes=k_R)
                fw.op("pe", lambda e: e.matmul(po[:, :], lhsT=zerob[:], rhs=qT[:, qt * 512:(qt + 1) * 512], start=True, stop=False),
                      reads=[k_zerob, k_qT[qt]], writes=[k_po])
                blocks = list(range(4 * qt + 3, -1, -1))
                nblk = len(blocks)
                q0 = qt * 512

                def zmm(dstp, b, last_stop):
                    kb = blocks[b]
                    kl = kb - 4 * qt
                    ksl = kT[:, hh, kb * 128:(kb + 1) * 128]
                    rk = [k_kT[kb // 4], k_qT[qt]]
                    c0 = kl * 128 if kl >= 0 else 0
                    fw.op("pe", lambda e: e.matmul(dstp[0][:, c0:512], lhsT=ksl, rhs=qT[:, q0 + c0:q0 + 512], start=True, stop=(last_stop and kl < 0)),
                          reads=rk, writes=[dstp[1]])
                    if kl >= 0:
                        fw.op("pe", lambda e: e.matmul(dstp[0][:, c0:c0 + 128], lhsT=identb[:], rhs=maskneg[:], start=False, stop=last_stop),
                              reads=[k_identb, k_maskneg], writes=[dstp[1]])
                    return c0

                def stageA1(b):
                    c0 = zmm(pz[b % 2], b, True)
                    E, k_E = Eb[b % 2]
                    fw.op("act", lambda e: e.activation(out=E[:, c0:], in_=pz[b % 2][0][:, c0:], func=AF.Exp), reads=[pz[b % 2][1]], writes=[k_E])

                def stageA2(b):
                    kl = blocks[b] - 4 * qt
                    c0 = kl * 128 if kl >= 0 else 0
                    E, k_E = Eb[b % 2]; L, k_L = Lb[b % 3]
                    fw.op("act", lambda e: e.activation(out=L[:, c0:], in_=E[:, c0:], func=AF.Ln, bias=1.0), reads=[k_E], writes=[k_L])
                    if b + 1 < nblk:
                        fw.op("pool", lambda e: e.tensor_tensor(out=Rb[:, (b + 1) % 3, c0:], in0=Rb[:, b % 3, c0:], in1=L[:, c0:], op=ALU.add),
                              reads=[k_R[b % 3], k_L], writes=[k_R[(b + 1) % 3]])

                def stageB1(b):
                    kl = blocks[b] - 4 * qt
                    c0 = kl * 128 if kl >= 0 else 0
                    L, k_L = Lb[b % 3]
                    fw.op("pe", lambda e: e.matmul(plw[b % 2][0][:, c0:], lhsT=negtri[:], rhs=L[:, c0:], start=True, stop=(b == 0)),
                          reads=[k_negtri, k_L], writes=[plw[b % 2][1]])
                    if b > 0:
                        fw.op("pe", lambda e: e.matmul(plw[b % 2][0][:, c0:], lhsT=negones[:], rhs=Rb[:, b % 3, c0:], start=False, stop=True),
                              reads=[k_negones, k_R[b % 3]], writes=[plw[b % 2][1]])
                    X, k_X = Xb[b % 2]
                    fw.op("act", lambda e: e.activation(out=X[:, c0:], in_=plw[b % 2][0][:, c0:], func=AF.Exp), reads=[plw[b % 2][1]], writes=[k_X])

                def stageB2(b):
                    kl = blocks[b] - 4 * qt
                    c0 = kl * 128 if kl >= 0 else 0
                    E, k_E = Eb[b % 2]; X, k_X = Xb[b % 2]; w, k_w = wb[b % 2]
                    fw.op("dve", lambda e: e.tensor_tensor(out=w[:, c0:], in0=E[:, c0:], in1=X[:, c0:], op=ALU.mult), reads=[k_E, k_X], writes=[k_w])

                def stageC(b):
                    kb = blocks[b]
                    kl = kb - 4 * qt
                    w, k_w = wb[b % 2]
                    last = (b == nblk - 1)
                    vsl = vv[:, kb, :]
                    rd = [k_v[kb // 4], k_w]
                    c0 = kl * 128 if kl >= 0 else 0
                    fw.op("pe", lambda e: e.matmul(po[:, c0:512], lhsT=vsl, rhs=w[:, c0:512], start=False, stop=last), reads=rd, writes=[k_po])
                    if last:
                        fw.op("dve", lambda e: e.tensor_copy(out=R2[p0:p0 + 64, hp, q0:q0 + 512], in_=po[p0:p0 + 64, :]), reads=[k_po], writes=[k_mx[hp][qt]])

                for i in range(nblk + 2):
                    if i < nblk:
                        stageA1(i)
                    if 0 <= i - 1 < nblk:
                        stageB1(i - 1)
                    if i < nblk:
                        stageA2(i)
                    if 0 <= i - 1 < nblk:
                        stageB2(i - 1)
                    if 0 <= i - 2 < nblk:
                        stageC(i - 2)
        fw.release(m)

    def phase_RET(l):
        m = fw.mark()
        wr = fw.sb([128, 8, 2048], BF16); k_wr = [fw.trk() for _ in range(4)]
        for i in range(4):
            fw.dma("pool", wr[:, :, i * 512:(i + 1) * 512], w_in[l, :, 768 + i * 512:768 + (i + 1) * 512].rearrange("(dc p) c -> p dc c", p=128),
                   writes=[k_wr[i]], slot=k_wr[i])
        dtab = fw.sb([128, 8, 128], F32); k_dtab = fw.trk()
        qdec = fw.sb([128, 4, 128], F32); k_qdec = fw.trk()
        kdec = fw.sb([128, 8, 64], F32); k_kdec = fw.trk()
        g128 = fw.sb([128, 4, 64], F32); k_g128 = fw.trk()
        rgb = fw.sb([128, 512], F32); k_rgb = fw.trk()
        fw.dma("sp", dtab[:], c_dt[:, :, :], writes=[k_dtab], slot=k_dtab)
        fw.dma("sp", qdec[:], c_qdec[:, :, :], writes=[k_qdec], slot=k_qdec)
        fw.dma("sp", kdec[:], c_kdec[:, :, :], writes=[k_kdec], slot=k_kdec)
        fw.dma("sp", g128[:], c_g128[:, :, :], writes=[k_g128], slot=k_g128)
        fw.dma("sp", rgb[:], ret_g[l, :].partition_broadcast(128), writes=[k_rgb], slot=k_rgb)
        rot = [(fw.sb([128, 256], F32), fw.trk()) for _ in range(2)]
        pA = (fw.ps([128, 512], F32), fw.trk()); pB = (fw.ps([128, 512], F32), fw.trk())
        ptq = (fw.ps([128, 4, 128], BF16), fw.trk()); ptk = (fw.ps([128, 4, 128], BF16), fw.trk())
        pS = [(fw.ps([128, 4, 128], F32), fw.trk()) for _ in range(2)]
        pO = (fw.ps([128, 512], F32), fw.trk()); pKV = (fw.ps([128, 4, 128], F32), fw.trk())
        tA = [(fw.sb([128, 512], F32), fw.trk()) for _ in range(2)]
        tB = [(fw.sb([128, 512], F32), fw.trk()) for _ in range(2)]
        rq = (fw.sb([128, 512], BF16), fw.trk()); rk = (fw.sb([128, 512], BF16), fw.trk())
        rkd = (fw.sb([128, 512], BF16), fw.trk()); vsb = (fw.sb([128, 512], BF16), fw.trk())
        gsil = (fw.sb([128, 512], F32), fw.trk())
        qTs = (fw.sb([128, 4, 128], BF16), fw.trk()); qdT = (fw.sb([128, 4, 128], BF16), fw.trk())
        kTz = (fw.sb([128, 4, 2, 128], BF16), fw.trk())
        fw.op("pool", lambda e: e.memset(kTz[0][:], 0.0), writes=[kTz[1]])
        AT = (fw.sb([128, 8, 128], BF16), fw.trk())
        sqo = (fw.sb([128, 512], F32), fw.trk()); ssr = (fw.sb([128, 8], F32), fw.trk())
        t1 = (fw.sb([128, 512], F32), fw.trk()); ytm = (fw.sb([128, 512], BF16), fw.trk())
        stf = (fw.sb([128, 4, 64], F32), fw.trk()); stb = (fw.sb([128, 4, 2, 64], BF16), fw.trk()); stt = (fw.sb([128, 4, 64], F32), fw.trk())
        fw.op("pool", lambda e: e.memset(stf[0][:], 0.0), writes=[stf[1]])
        fw.op("pool", lambda e: e.memset(stb[0][:], 0.0), writes=[stb[1]])

        def proj(pdst, wi, tb):
            for dc in range(8):
                fw.op("pe", lambda e, dc=dc: e.matmul(pdst[0][:], lhsT=R1[:, dc, tb * 128:(tb + 1) * 128], rhs=wr[:, dc, wi * 512:(wi + 1) * 512], start=(dc == 0), stop=(dc == 7)),
                      reads=[k_hT[tb], k_wr[wi]], writes=[pdst[1]])

        def v3(ap):
            return ap.rearrange("p (h e) -> p h e", h=8)

        def rotary(psrc, off, ta, tb_, dst, rt, k_rt):
            cos2 = rt[:, off:off + 64].unsqueeze(1).broadcast_to([128, 8, 64])
            s2 = rt[:, off + 64:off + 128]
            s2e = s2.rearrange("p (i two) -> p i two", two=2)[:, :, 0].unsqueeze(1).broadcast_to([128, 8, 32])
            s2o = s2.rearrange("p (i two) -> p i two", two=2)[:, :, 1].unsqueeze(1).broadcast_to([128, 8, 32])
            ps4 = psrc[0][:].rearrange("p (h i two) -> p h i two", h=8, two=2)
            tb4 = tb_[0][:].rearrange("p (h i two) -> p h i two", h=8, two=2)
            fw.op("dve", lambda e: e.tensor_tensor(out=v3(ta[0][:]), in0=v3(psrc[0][:]), in1=cos2, op=ALU.mult), reads=[psrc[1], k_rt], writes=[ta[1]])
            fw.op("dve", lambda e: e.tensor_tensor(out=tb4[:, :, :, 0], in0=ps4[:, :, :, 1], in1=s2e, op=ALU.mult), reads=[psrc[1], k_rt], writes=[tb_[1]])
            fw.op("dve", lambda e: e.tensor_tensor(out=tb4[:, :, :, 1], in0=ps4[:, :, :, 0], in1=s2o, op=ALU.mult), reads=[psrc[1], k_rt], writes=[tb_[1]])
            fw.op("pool", lambda e: e.tensor_tensor(out=dst[0][:], in0=ta[0][:], in1=tb_[0][:], op=ALU.add), reads=[ta[1], tb_[1]], writes=[dst[1]])

        gsil2 = [gsil, (fw.sb([128, 512], F32), fw.trk())]

        def r_head(n):
            rt, k_rt = rot[n % 2]
            fw.dma("sp", rt[:], c_rot[n, :, :], writes=[k_rt], slot=k_rt)
            proj(pA, 0, n)
            rotary(pA, 0, tA[0], tB[0], rq, rt, k_rt)
            proj(pB, 1, n)
            rotary(pB, 128, tA[1], tB[1], rk, rt, k_rt)
            fw.op("pool", lambda e: e.tensor_tensor(out=v3(rkd[0][:]), in0=v3(rk[0][:]), in1=kdec[:], op=ALU.mult), reads=[rk[1], k_kdec], writes=[rkd[1]])
            proj(pA, 2, n)
            fw.op("act", lambda e: e.activation(out=vsb[0][:], in_=pA[0][:], func=AF.Copy), reads=[pA[1]], writes=[vsb[1]])
            proj(pB, 3, n)
            sigmoid_from(gsil2[n % 2][0][:], pB[0][:], [pB[1]], [gsil2[n % 2][1]])
            fw.op("dve", lambda e: e.tensor_tensor(out=gsil2[n % 2][0][:], in0=pB[0][:], in1=gsil2[n % 2][0][:], op=ALU.mult), reads=[pB[1], gsil2[n % 2][1]], writes=[gsil2[n % 2][1]])
            fw.op("pool", lambda e: e.tensor_tensor(out=gsil2[n % 2][0][:], in0=gsil2[n % 2][0][:], in1=rgb[:], op=ALU.mult), reads=[gsil2[n % 2][1], k_rgb], writes=[gsil2[n % 2][1]])
            for pr in range(4):
                fw.op("pe", lambda e, pr=pr: e.transpose(out=ptq[0][:, pr, :], in_=rq[0][:, pr * 128:(pr + 1) * 128], identity=identb[:]), reads=[rq[1], k_identb], writes=[ptq[1]])
            for pr in range(4):
                fw.op("pe", lambda e, pr=pr: e.transpose(out=ptk[0][:, pr, :], in_=rk[0][:, pr * 128:(pr + 1) * 128], identity=identb[:]), reads=[rk[1], k_identb], writes=[ptk[1]])
            fw.op("act", lambda e: e.activation(out=qTs[0][:], in_=ptq[0][:], func=AF.Copy), reads=[ptq[1]], writes=[qTs[1]])
            fw.op("dve", lambda e: e.tensor_tensor(out=qdT[0][:], in0=ptq[0][:], in1=qdec[:], op=ALU.mult), reads=[ptq[1], k_qdec], writes=[qdT[1]])
            for hh in range(2):
                fw.op("act", lambda e, hh=hh: e.activation(out=kTz[0][hh * 64:(hh + 1) * 64, :, hh, :], in_=ptk[0][hh * 64:(hh + 1) * 64, :, :], func=AF.Copy), reads=[ptk[1]], writes=[kTz[1]])

        def r_mid(n):
            for h in range(8):
                pr, p0 = h // 2, (h % 2) * 64
                fw.op("pe", lambda e, h=h, pr=pr: e.matmul(pS[h // 4][0][:, h % 4, :], lhsT=kTz[0][:, pr, h % 2, :], rhs=qTs[0][:, pr, :], start=True, stop=True),
                      reads=[kTz[1], qTs[1]], writes=[pS[h // 4][1]])
            for half in range(2):
                fw.op("dve", lambda e, half=half: e.tensor_tensor(out=AT[0][:, half * 4:(half + 1) * 4, :], in0=pS[half][0][:], in1=dtab[:, half * 4:(half + 1) * 4, :], op=ALU.mult),
                      reads=[pS[half][1], k_dtab], writes=[AT[1]])
            for h in range(8):
                pr, p0 = h // 2, (h % 2) * 64
                fw.op("pe", lambda e, h=h: e.matmul(pO[0][:, h * 64:(h + 1) * 64], lhsT=AT[0][:, h, :], rhs=vsb[0][:, h * 64:(h + 1) * 64], start=True, stop=False),
                      reads=[AT[1], vsb[1]], writes=[pO[1]])
                fw.op("pe", lambda e, h=h, pr=pr: e.matmul(pO[0][:, h * 64:(h + 1) * 64], lhsT=qdT[0][:, pr, :], rhs=stb[0][:, pr, h % 2, :], start=False, stop=True),
                      reads=[qdT[1], stb[1]], writes=[pO[1]])
            for pr in range(4):
                fw.op("pe", lambda e, pr=pr: e.matmul(pKV[0][:, pr, :], lhsT=rkd[0][:, pr * 128:(pr + 1) * 128], rhs=vsb[0][:, pr * 128:(pr + 1) * 128], start=True, stop=True),
                      reads=[rkd[1], vsb[1]], writes=[pKV[1]])
            fw.op("pool", lambda e: e.tensor_tensor(out=stt[0][:], in0=stf[0][:], in1=g128[:], op=ALU.mult), reads=[stf[1], k_g128], writes=[stt[1]])
            for half in range(2):
                p0 = half * 64
                fw.op("dve", lambda e, p0=p0: e.tensor_tensor(out=stf[0][p0:p0 + 64, :, :], in0=stt[0][p0:p0 + 64, :, :], in1=pKV[0][p0:p0 + 64, :, p0:p0 + 64], op=ALU.add),
                      reads=[stt[1], pKV[1]], writes=[stf[1]])
            for hh in range(2):
                fw.op("act", lambda e, hh=hh: e.activation(out=stb[0][hh * 64:(hh + 1) * 64, :, hh, :], in_=stf[0][hh * 64:(hh + 1) * 64, :, :], func=AF.Copy), reads=[stf[1]], writes=[stb[1]])

        def r_tail(n):
            fw.op("act", lambda e: e.activation(out=sqo[0][:], in_=pO[0][:], func=AF.Square), reads=[pO[1]], writes=[sqo[1]])
            fw.op("dve", lambda e: e.tensor_reduce(out=ssr[0][:], in_=v3(sqo[0][:]), axis=AX.X, op=ALU.add), reads=[sqo[1]], writes=[ssr[1]])
            rstd_from("act", ssr[0][:], ssr[0][:], 1.0 / 64, [ssr[1]], [ssr[1]])
            fw.op("dve", lambda e: e.tensor_tensor(out=v3(t1[0][:]), in0=v3(pO[0][:]), in1=ssr[0][:].unsqueeze(2).broadcast_to([128, 8, 64]), op=ALU.mult),
                  reads=[pO[1], ssr[1]], writes=[t1[1]])
            fw.op("pool", lambda e: e.tensor_tensor(out=ytm[0][:], in0=t1[0][:], in1=gsil2[n % 2][0][:], op=ALU.mult), reads=[t1[1], gsil2[n % 2][1]], writes=[ytm[1]])
            for pr in range(4):
                fw.op("pe", lambda e, pr=pr: e.transpose(out=ptq[0][:, pr, :], in_=ytm[0][:, pr * 128:(pr + 1) * 128], identity=identb[:]), reads=[ytm[1], k_identb], writes=[ptq[1]])
            fw.op("act", lambda e: e.activation(out=R2[:, 2:6, n * 128:(n + 1) * 128], in_=ptq[0][:], func=AF.Copy), reads=[ptq[1]],
                  writes=[k_mx[2 + pr][n // 4] for pr in range(4)])

        r_head(0)
        r_mid(0)
        for n in range(NTB):
            if n + 1 < NTB:
                r_head(n + 1)
            r_tail(n)
            if n + 1 < NTB:
                r_mid(n + 1)
        fw.release(m)

    def phase_CONV(l):
        m = fw.mark()
        PAD = 30
        hg = fw.sb([128, 2, PAD + T], BF16); k_hg = [[fw.trk() for _ in range(9)] for _ in range(2)]
        dw31 = fw.sb([128, 256], F32); k_dw31 = fw.trk()
        fw.op("pool", lambda e: e.memset(dw31[:], 0.0), writes=[k_dw31])
        fw.dma("sp", dw31[0:31, :], cdw_w[l, :, :], writes=[k_dw31], slot=k_dw31)
        dwb16 = fw.sb([128, 256], BF16); k_dwb16 = fw.trk()
        fw.op("dve", lambda e: e.tensor_copy(out=dwb16[:], in_=dw31[:]), reads=[k_dw31], writes=[k_dwb16])
        wcol = fw.sb([128, 2, 31], F32); k_wcol = fw.trk()
        diagw = fw.sb([128, 2, 31, 128], BF16); k_diagw = fw.trk()
        cols = {}
        for name, src in (("ba", cpw_b[l, 0:256]), ("bg", cpw_b[l, 256:512]), ("dwb", cdw_b[l, :]), ("lng", cln_g[l, :]), ("lnb", cln_b[l, :])):
            t = fw.sb([128, 2], F32); k = fw.trk()
            load_cols(t[:], k, src, 2)
            cols[name] = (t, k)
        fw.op("dve", lambda e: e.tensor_scalar(out=cols["bg"][0][:], in0=cols["bg"][0][:], scalar1=-1.0, scalar2=None, op0=ALU.mult), reads=[cols["bg"][1]], writes=[cols["bg"][1]])
        pw = [(fw.ps([128, 512], F32), fw.trk()) for _ in range(2)]
        ptw = (fw.ps([128, 2, 128], BF16), fw.trk())
        for cc in range(2):
            fw.op("pe", lambda e, cc=cc: e.transpose(out=ptw[0][:, cc, :], in_=dwb16[:, cc * 128:(cc + 1) * 128], identity=identb[:]),
                  reads=[k_dwb16, k_identb], writes=[ptw[1]])
        fw.op("dve", lambda e: e.tensor_copy(out=wcol[:], in_=ptw[0][:, :, 0:31]), reads=[ptw[1]], writes=[k_wcol])
        for cc in range(2):
            for j in range(31):
                fw.op("dve", lambda e, cc=cc, j=j: e.tensor_scalar(out=diagw[:, cc, j, :], in0=identf[:], scalar1=wcol[:, cc, j:j + 1], scalar2=None, op0=ALU.mult),
                      reads=[k_identf, k_wcol], writes=[k_diagw])
        for cc in range(2):
            fw.op("pool", lambda e, cc=cc: e.memset(hg[:, cc, 0:PAD], 0.0), writes=[k_hg[cc][0]])
        if CONV_STOP < 2:
            fw.release(m)
            return
        wts = [(fw.sb([128, 8, 128], BF16), fw.trk()) for _ in range(2)]
        pa = [(fw.ps([128, 512], F32), fw.trk()) for _ in range(2)]
        pg = pw
        sg = [(fw.sb([128, 512], F32), fw.trk()) for _ in range(2)]
        it = 0
        for cc in range(2):
            fw.dma("pool", wts[0][0][:], w_in[l, :, 2816 + cc * 128:2816 + (cc + 1) * 128].rearrange("(dc p) c -> p dc c", p=128), writes=[wts[0][1]], slot=wts[0][1])
            fw.dma("pool", wts[1][0][:], w_in[l, :, 3072 + cc * 128:3072 + (cc + 1) * 128].rearrange("(dc p) c -> p dc c", p=128), writes=[wts[1][1]], slot=wts[1][1])
            for tt in range(8):
                a_, g_, s_ = pa[it % 2], pg[it % 2], sg[it % 2]
                it += 1
                for dc in range(8):
                    fw.op("pe", lambda e, dc=dc: e.matmul(a_[0][:], lhsT=wts[0][0][:, dc, :], rhs=R1[:, dc, tt * 512:(tt + 1) * 512], start=(dc == 0), stop=(dc == 7)),
                          reads=[wts[0][1]] + k_hT[tt * 4:(tt + 1) * 4], writes=[a_[1]])
                for dc in range(8):
                    fw.op("pe", lambda e, dc=dc: e.matmul(g_[0][:], lhsT=wts[1][0][:, dc, :], rhs=R1[:, dc, tt * 512:(tt + 1) * 512], start=(dc == 0), stop=(dc == 7)),
                          reads=[wts[1][1]] + k_hT[tt * 4:(tt + 1) * 4], writes=[g_[1]])
                sigmoid_from(s_[0][:], g_[0][:], [g_[1], cols["bg"][1]], [s_[1]], nbias=cols["bg"][0][:, cc:cc + 1])
                fw.op("dve", lambda e: e.scalar_tensor_tensor(out=hg[:, cc, PAD + tt * 512:PAD + (tt + 1) * 512], in0=a_[0][:], scalar=cols["ba"][0][:, cc:cc + 1], in1=s_[0][:], op0=ALU.add, op1=ALU.mult),
                      reads=[a_[1], cols["ba"][1], s_[1]], writes=[k_hg[cc][1 + tt]])
        if CONV_STOP < 3:
            fw.release(m)
            return
        pc = [(fw.ps([128, 512], F32), fw.trk()) for _ in range(2)]
        yc = [(fw.sb([128, 512], F32), fw.trk()) for _ in range(2)]
        ysq = [(fw.sb([128, 512], BF16), fw.trk()) for _ in range(2)]
        ycb = [(fw.sb([128, 512], BF16), fw.trk()) for _ in range(2)]
        mean = (fw.sb([128, 512], F32), fw.trk()); var = (fw.sb([128, 512], F32), fw.trk())
        tt1 = (fw.sb([128, 512], F32), fw.trk()); tt2 = (fw.sb([128, 512], F32), fw.trk())
        for tt in range(8):
            for cc in range(2):
                rd = [k_diagw, k_hg[cc][1 + tt], k_hg[cc][tt]]
                for j in range(31):
                    fw.op("pe", lambda e, cc=cc, j=j: e.matmul(pc[cc][0][:], lhsT=diagw[:, cc, j, :], rhs=hg[:, cc, tt * 512 + j:tt * 512 + j + 512], start=(j == 0), stop=(j == 30)),
                          reads=rd, writes=[pc[cc][1]])
                fw.op("dve", lambda e, cc=cc: e.tensor_scalar(out=yc[cc][0][:], in0=pc[cc][0][:], scalar1=cols["dwb"][0][:, cc:cc + 1], scalar2=None, op0=ALU.add), reads=[pc[cc][1], cols["dwb"][1]], writes=[yc[cc][1]])
                fw.op("act", lambda e, cc=cc: e.activation(out=ysq[cc][0][:], in_=yc[cc][0][:], func=AF.Square), reads=[yc[cc][1]], writes=[ysq[cc][1]])
                fw.op("act", lambda e, cc=cc: e.activation(out=ycb[cc][0][:], in_=yc[cc][0][:], func=AF.Copy), reads=[yc[cc][1]], writes=[ycb[cc][1]])
            if CONV_STOP < 4:
                continue
            for cc in range(2):
                fw.op("pe", lambda e, cc=cc: e.matmul(pw[0][0][:], lhsT=onesf[:], rhs=ycb[cc][0][:], start=(cc == 0), stop=(cc == 1)), reads=[k_onesf, ycb[cc][1]], writes=[pw[0][1]])
            for cc in range(2):
                fw.op("pe", lambda e, cc=cc: e.matmul(pw[1][0][:], lhsT=onesf[:], rhs=ysq[cc][0][:], start=(cc == 0), stop=(cc == 1)), reads=[k_onesf, ysq[cc][1]], writes=[pw[1][1]])
            if CONV_STOP < 5:
                continue
            fw.op("act", lambda e: e.activation(out=mean[0][:], in_=pw[0][0][:], func=AF.Copy), reads=[pw[0][1]], writes=[mean[1]])
            fw.op("dve", lambda e: e.tensor_tensor(out=var[0][:], in0=mean[0][:], in1=mean[0][:], op=ALU.mult), reads=[mean[1]], writes=[var[1]])
            fw.op("dve", lambda e: e.tensor_tensor(out=var[0][:], in0=pw[1][0][:], in1=var[0][:], op=ALU.subtract), reads=[pw[1][1], var[1]], writes=[var[1]])
            rstd_from("act", var[0][:], var[0][:], 1.0, [var[1]], [var[1]])
            for cc in range(2):
                fw.op("dve", lambda e, cc=cc: e.tensor_tensor(out=tt1[0][:], in0=yc[cc][0][:], in1=mean[0][:], op=ALU.subtract), reads=[yc[cc][1], mean[1]], writes=[tt1[1]])
                fw.op("dve", lambda e, cc=cc: e.tensor_tensor(out=tt1[0][:], in0=tt1[0][:], in1=var[0][:], op=ALU.mult), reads=[tt1[1], var[1]], writes=[tt1[1]])
                fw.op("dve", lambda e, cc=cc: e.tensor_scalar(out=tt1[0][:], in0=tt1[0][:], scalar1=cols["lng"][0][:, cc:cc + 1], scalar2=cols["lnb"][0][:, cc:cc + 1], op0=ALU.mult, op1=ALU.add),
                      reads=[tt1[1], cols["lng"][1], cols["lnb"][1]], writes=[tt1[1]])
                sigmoid_from(tt2[0][:], tt1[0][:], [tt1[1]], [tt2[1]])
                fw.op("pool", lambda e, cc=cc: e.tensor_tensor(out=R2[:, 6 + cc, tt * 512:(tt + 1) * 512], in0=tt1[0][:], in1=tt2[0][:], op=ALU.mult),
                      reads=[tt1[1], tt2[1]], writes=[k_mx[6 + cc][tt]])
        fw.release(m)

    def phase_C1(l, xin, k_xin, xmid, k_xmid, k_w1):
        m = fw.mark()
        wo = fw.sb([128, 8, D], BF16); k_wo = [fw.trk() for _ in range(2)]
        for i in range(2):
            fw.dma("pool", wo[:, :, i * 512:(i + 1) * 512], w_out[l, :, i * 512:(i + 1) * 512].rearrange("(cc p) d -> p cc d", p=128), writes=[k_wo[i]], slot=k_wo[i])
        for i in range(8):
            fw.dma("pool", R1[:, :, i * 512:(i + 1) * 512], w_ff1[l, :, i * 512:(i + 1) * 512].rearrange("(dc p) f -> p dc f", p=128), writes=[k_w1[i]], slot=k_w1[i])
        xts = [(fw.sb([128, D], F32), fw.trk()) for _ in range(3)]
        xos = [(fw.sb([128, D], F32), fw.trk()) for _ in range(3)]
        py = [(fw.ps([128, D], F32), fw.trk()) for _ in range(2)]
        for tb in range(NTB):
            xt, k_xt = xts[tb % 3]; xo, k_xo = xos[tb % 3]; p_, k_p = py[tb % 2]
            fw.dma("sp", xt[:], xin[tb * 128:(tb + 1) * 128, :], reads=[k_xin], writes=[k_xt], slot=k_xt)
            for half in range(2):
                for cc in range(8):
                    fw.op("pe", lambda e, cc=cc, half=half: e.matmul(p_[:, half * 512:(half + 1) * 512], lhsT=R2[:, cc, tb * 128:(tb + 1) * 128], rhs=wo[:, cc, half * 512:(half + 1) * 512], start=(cc == 0), stop=(cc == 7)),
                          reads=[k_mx[cc][tb // 4], k_wo[half]], writes=[k_p])
            for half in range(2):
                fw.op("dve", lambda e, half=half: e.tensor_tensor(out=xo[:, half * 512:(half + 1) * 512], in0=p_[:, half * 512:(half + 1) * 512], in1=xt[:, half * 512:(half + 1) * 512], op=ALU.add),
                      reads=[k_p, k_xt], writes=[k_xo])
            fw.dma("sp", xmid[tb * 128:(tb + 1) * 128, :], xo[:], reads=[k_xo], writes=[k_xmid], slot=k_xo)
        fw.release(m)

    def phase_C2(l, xmid, k_xmid, xout, k_xout, k_w1):
        m = fw.mark()
        W1 = R1
        W2 = R2[:].rearrange("p c t -> p (c t)").rearrange("p (fc d) -> p fc d", d=D)
        k_w2 = [fw.trk() for _ in range(8)]
        for i in range(8):
            fw.dma("pool", W2[:, i * 4:(i + 1) * 4, :], w_ff2[l, i * 512:(i + 1) * 512, :].rearrange("(fc p) d -> p fc d", p=128), writes=[k_w2[i]], slot=k_w2[i])
        gt = fw.sb([128, 8], F32); k_gt = fw.trk()
        load_cols(gt[:], k_gt, mlp_g[l, :], 8)
        TT = 256
        xts = [(fw.sb([128, 2, D], F32), [fw.trk(), fw.trk()]) for _ in range(2)]
        xos = [(fw.sb([128, 2, D], F32), [fw.trk(), fw.trk()]) for _ in range(2)]
        res = (fw.sb([128, D], BF16), fw.trk(), fw.sb([128, 2], F32), fw.trk(),
               fw.sb([128, D], BF16), fw.trk(), fw.ps([128, 8, 128], BF16), fw.trk())
        h2T = [(fw.sb([128, 8, TT], BF16), [fw.trk(), fw.trk()]) for _ in range(2)]
        py = [(fw.ps([128, D], F32), fw.trk()) for _ in range(2)]
        pf = [(fw.ps([128, 512], F32), fw.trk()) for _ in range(3)]
        rl = [(fw.sb([128, TT], F32), fw.trk()) for _ in range(3)]
        fb = [(fw.sb([128, TT], BF16), fw.trk()) for _ in range(3)]
        for ti in range(T // TT):
            xt, k_xt = xts[ti % 2]; xo, k_xo = xos[ti % 2]; hT2, k_h2 = h2T[ti % 2]
            for b2 in range(2):
                tb = ti * 2 + b2
                fw.dma("sp", xt[:, b2, :], xmid[tb * 128:(tb + 1) * 128, :], reads=[k_xmid], writes=[k_xt[b2]], slot=k_xt[b2])
                norm_transpose_block(xt[:, b2, :], k_xt[b2], gt, k_gt, res, hT2[:, :, b2 * 128:(b2 + 1) * 128], k_h2[b2])

            def stF(fc):
                p_, k_p = pf[fc % 3]
                for dc in range(8):
                    fw.op("pe", lambda e, dc=dc: e.matmul(p_[:, 0:TT], lhsT=W1[:, dc, fc * 128:(fc + 1) * 128], rhs=hT2[:, dc, :], start=(dc == 0), stop=(dc == 7)),
                          reads=[k_w1[fc // 4]] + k_h2, writes=[k_p])
                r_, k_r = rl[fc % 3]; f_, k_f = fb[fc % 3]
                fw.op("act", lambda e: e.activation(out=r_[:], in_=p_[:, 0:TT], func=AF.Relu), reads=[k_p], writes=[k_r])
                fw.op("pool", lambda e: e.tensor_tensor(out=f_[:], in0=r_[:], in1=r_[:], op=ALU.mult), reads=[k_r], writes=[k_f])

            def stY(fc):
                f_, k_f = fb[fc % 3]
                for b2 in range(2):
                    for half in range(2):
                        fw.op("pe", lambda e, b2=b2, half=half: e.matmul(py[b2][0][:, half * 512:(half + 1) * 512], lhsT=f_[:, b2 * 128:(b2 + 1) * 128], rhs=W2[:, fc, half * 512:(half + 1) * 512], start=(fc == 0), stop=(fc == 31)),
                              reads=[k_f, k_w2[fc // 4]], writes=[py[b2][1]])

            for i in range(32 + 2):
                if i < 32:
                    stF(i)
                if 0 <= i - 2 < 32:
                    stY(i - 2)
            for b2 in range(2):
                tb = ti * 2 + b2
                for half in range(2):
                    fw.op("dve", lambda e, b2=b2, half=half: e.tensor_tensor(out=xo[:, b2, half * 512:(half + 1) * 512], in0=py[b2][0][:, half * 512:(half + 1) * 512], in1=xt[:, b2, half * 512:(half + 1) * 512], op=ALU.add),
                          reads=[py[b2][1], k_xt[b2]], writes=[k_xo[b2]])
                fw.dma("sp", xout[tb * 128:(tb + 1) * 128, :], xo[:, b2, :], reads=[k_xo[b2]], writes=[k_xout], slot=k_xo[b2])
        fw.release(m)

    bufs = {0: (x_in, k_dram["x"], s1, k_dram["s1"], s0, k_dram["s0"]),
            1: (s0, k_dram["s0"], s1, k_dram["s1"], y_out, k_dram["y"])}
    if nlayers == 1:
        bufs[0] = (x_in, k_dram["x"], s1, k_dram["s1"], y_out, k_dram["y"])
    for l in range(nlayers):
        xin, k_xin, xmid, k_xmid, xout, k_xout = bufs[l]
        phase_A(l, xin, k_xin)
        if dbg == "hT":
            break
        for hp in range(2):
            if "sb" not in skip:
                phase_SB(l, hp)
        if dbg in ("sb", "sbproj"):
            break
        if "ret" not in skip:
            phase_RET(l)
        if dbg == "ret":
            break
        if "conv" not in skip:
            phase_CONV(l)
        if dbg == "mixed":
            break
        k_w1 = [fw.trk() for _ in range(8)]
        phase_C1(l, xin, k_xin, xmid, k_xmid, k_w1)
        if dbg == "c1":
            break
        phase_C2(l, xmid, k_xmid, xout, k_xout, k_w1)
    if dbg in ("hT", "sb", "ret", "mixed", "sbproj"):
        src = R1 if dbg == "hT" else R2
        m = fw.mark()
        st = [(fw.sb([128, 2048], F32), fw.trk()) for _ in range(2)]
        i = 0
        for c in dump_chunks:
            for hf in range(2):
                s_, k_s = st[i % 2]; i += 1
                fw.op("dve", lambda e: e.tensor_copy(out=s_[:], in_=src[:, c, hf * 2048:(hf + 1) * 2048]), reads=[], writes=[k_s])
                fw.dma("sp", dbg_out[:, c, hf * 2048:(hf + 1) * 2048], s_[:], reads=[k_s], writes=[k_dram["dbg"]], slot=k_s)
        fw.release(m)
    fw.barrier()
    return fw


def kernel(**inputs):
    global _CONSTS
    if _CONSTS is None:
        _CONSTS = _consts()
    if "nc" not in _NC_CACHE:
        p1 = build()
        _NC_CACHE["nc"] = build(needed=p1.waited)
    fw = _NC_CACHE["nc"]
    x = np.ascontiguousarray(np.asarray(inputs["x"], dtype=np.float32))
    shared = {k: np.ascontiguousarray(np.asarray(v, dtype=np.float32)) for k, v in inputs.items() if k != "x"}
    shared.update(_CONSTS)
    in_maps = []
    for b in range(8):
        mp = dict(shared)
        mp["x"] = x[b]
        in_maps.append(mp)
    res = run_bass_kernel_spmd(fw.nc, in_maps, core_ids=list(range(8)))
    return np.stack([np.asarray(r["y"], dtype=np.float32) for r in res.results], axis=0)
```

```python
import numpy as np
import concourse.bass as bass
import concourse.mybir as mybir
from concourse.bass_utils import run_bass_kernel_spmd

F32 = mybir.dt.float32
BF16 = mybir.dt.bfloat16
AF = mybir.ActivationFunctionType
ALU = mybir.AluOpType
AX = mybir.AxisListType

T = 4096
D = 1024
NTB = 32
DFF = 4096
DEPTH = 2
INC = 3328
EPS = 1e-6
NEG = -30000.0
RET_STOP = 99
CONV_STOP = 99


class Trk:
    __slots__ = ("name", "lw", "rd", "dsem")

    def __init__(self, name):
        self.name = name
        self.lw = {}
        self.rd = {}
        self.dsem = None


class Eng:
    def __init__(self, name, h, sem):
        self.name = name
        self.h = h
        self.sem = sem
        self.count = 0
        self.seen = {}


class FW:
    def __init__(self, needed=None):
        self.needed = None
        if needed is not None:
            self.needed = {k: sorted(v) for k, v in needed.items()}
            self.needed_set = {k: set(v) for k, v in needed.items()}
        self.waited = {}
        self.nc = bass.Bass("TRN2", target_bir_lowering=False)
        nc = self.nc
        self._stack = []
        self.engs = {}
        for name, h in (("pe", nc.tensor), ("act", nc.scalar), ("dve", nc.vector),
                        ("pool", nc.gpsimd), ("sp", nc.sync)):
            sem = self.enter(nc.semaphore("sem_" + name))
            self.engs[name] = Eng(name, h, sem)
        self.dsem_tot = {}
        self.free_dsems = []
        self.free_dsems_sw = []
        self._scope_dsems = []
        self._persist = []
        self.nwait = 0
        self.ninc = 0
        self.ninst = 0
        self._uid = 0

    def enter(self, cm):
        v = cm.__enter__()
        self._stack.append(cm)
        return v

    def uid(self, p="t"):
        self._uid += 1
        return "%s%d" % (p, self._uid)

    def sb(self, shape, dt, name=None):
        return self.enter(self.nc.sbuf_tensor(name or self.uid("sb"), list(shape), dt))

    def ps(self, shape, dt, name=None):
        return self.enter(self.nc.psum_tensor(name or self.uid("ps"), list(shape), dt))

    def trk(self, name=None):
        return Trk(name or self.uid("k"))

    def mark(self):
        self._scope_dsems.append([])
        return len(self._stack)

    def release(self, mark):
        self.barrier()
        while len(self._stack) > mark:
            cm = self._stack.pop()
            cm.__exit__(None, None, None)
        for is_sw, sem in self._scope_dsems.pop():
            (self.free_dsems_sw if is_sw else self.free_dsems).append(sem)

    def _emit_wait(self, eng, s, k, v):
        if k in self.dsem_tot:
            eng.h.wait_ge(s, v)
        else:
            self.waited.setdefault(k, set()).add(v)
            if self.needed is None:
                eng.h.wait_ge(s, v)
            else:
                import bisect
                lst = self.needed[k]
                r = bisect.bisect_left(lst, v)
                assert r < len(lst) and lst[r] == v, (k, v)
                eng.h.wait_ge(s, r + 1)
        self.nwait += 1

    def _deps(self, reads, writes):
        deps = {}

        def add(d):
            for k, (s, v) in d.items():
                if k not in deps or deps[k][1] < v:
                    deps[k] = (s, v)
        for r in reads:
            add(r.lw)
        for w in writes:
            add(w.lw)
            add(w.rd)
        return deps

    def _wait(self, eng, deps):
        for k, (s, v) in deps.items():
            if k in self.dsem_tot:
                v = self.dsem_tot[k][1]
            if eng.name == "pe" and k == eng.sem.name:
                continue
            if eng.seen.get(k, 0) >= v:
                continue
            self._emit_wait(eng, s, k, v)
            eng.seen[k] = v

    def op(self, ename, fn, reads=(), writes=()):
        eng = self.engs[ename]
        self._wait(eng, self._deps(reads, writes))
        inst = fn(eng.h)
        eng.count += 1
        if self.needed is None or eng.count in self.needed_set.get(eng.sem.name, ()):
            inst.then_inc(eng.sem, 1)
            self.ninc += 1
        ev = (eng.sem, eng.count)
        k = eng.sem.name
        for w in writes:
            w.lw = {k: ev}
            w.rd = {}
        for r in reads:
            r.rd[k] = ev
        self.ninst += 1
        return inst

    def dma(self, qname, out, in_, reads=(), writes=(), slot=None, **kw):
        eng = self.engs[qname]
        self._wait(eng, self._deps(reads, writes))
        if slot.dsem is None:
            pool_ = self.free_dsems_sw if qname == "pool" else self.free_dsems
            if pool_:
                slot.dsem = pool_.pop()
            else:
                cm = self.nc.semaphore(self.uid("dsem"))
                slot.dsem = cm.__enter__()
                self._persist.append(cm)
                self.dsem_tot[slot.dsem.name] = (slot.dsem, 0)
            if self._scope_dsems:
                self._scope_dsems[-1].append((qname == "pool", slot.dsem))
        inst = eng.h.dma_start(out=out, in_=in_, **kw)
        tot = self.dsem_tot[slot.dsem.name][1] + 16
        self.dsem_tot[slot.dsem.name] = (slot.dsem, tot)
        inst.then_inc(slot.dsem, 16)
        ev = (slot.dsem, tot)
        k = slot.dsem.name
        for w in writes:
            w.lw = {k: ev}
            w.rd = {}
        for r in reads:
            r.rd[k] = ev
        self.ninst += 1
        return inst

    def barrier(self):
        for eng in self.engs.values():
            for e in self.engs.values():
                if e is eng or e.count == 0:
                    continue
                if eng.seen.get(e.sem.name, 0) < e.count:
                    self._emit_wait(eng, e.sem, e.sem.name, e.count)
                    eng.seen[e.sem.name] = e.count
            for k, (s, tot) in self.dsem_tot.items():
                if tot and eng.seen.get(k, 0) < tot:
                    self._emit_wait(eng, s, k, tot)
                    eng.seen[k] = tot


def _consts():
    c = {}
    i = np.arange(128)
    c["c_ident"] = np.eye(128, dtype=np.float32)
    c["c_blk"] = (i[:, None] // 64 == i[None, :] // 64).astype(np.float32)
    c["c_negtri"] = -(i[:, None] >= i[None, :]).astype(np.float32)
    c["c_negones"] = -np.ones((128, 128), np.float32)
    c["c_maskneg"] = np.where(i[:, None] >= i[None, :], NEG, 0.0).astype(np.float32)
    c["c_ones"] = np.full((128, 128), 1.0 / 256.0, np.float32)
    inv = (1.0 / (10000.0 ** np.linspace(0.0, 1.0, 32, dtype=np.float32))).astype(np.float32)
    pos = np.arange(T, dtype=np.float32)
    ang = (pos[:, None] * inv[None, :]).astype(np.float32)
    cos = np.cos(ang).astype(np.float32)
    sin = np.sin(ang).astype(np.float32)
    cos2 = np.repeat(cos, 2, axis=1)
    s2 = np.stack([-sin, sin], axis=-1).reshape(T, 64)
    rot = np.concatenate([cos2, s2, cos2 * 0.125, s2 * 0.125], axis=1).astype(np.float32)
    c["c_rot"] = rot.reshape(NTB, 128, 256)
    h = np.arange(8, dtype=np.float32)
    log_g = np.log(1.0 - np.exp2(-5.0 - h)).astype(np.float32)
    j = np.arange(128, dtype=np.float32)
    rel = j[None, :] - j[:, None]
    dt_ = np.where(rel[None] >= 0, np.exp(log_g[:, None, None] * np.maximum(rel[None], 0.0)), 0.0)
    c["c_dt"] = np.ascontiguousarray(dt_.transpose(1, 0, 2)).astype(np.float32)
    qd = np.exp(log_g[:, None] * (j + 1.0)[None, :]).astype(np.float32)
    qdec = np.zeros((128, 4, 128), np.float32)
    g128 = np.zeros((128, 4, 64), np.float32)
    gC = np.exp(log_g * 128.0).astype(np.float32)
    for p in range(128):
        for pr in range(4):
            hh = pr * 2 + p // 64
            qdec[p, pr, :] = qd[hh]
            g128[p, pr, :] = gC[hh]
    c["c_qdec"] = qdec
    c["c_g128"] = g128
    kd = np.exp(log_g[:, None] * (127.0 - j)[None, :]).astype(np.float32)
    c["c_kdec"] = np.ascontiguousarray(np.repeat(kd.T[:, :, None], 64, axis=2)).astype(np.float32)
    return c


_CONSTS = None
_NC_CACHE = {}


def build(nlayers=DEPTH, dbg=None, skip=(), dump_chunks=range(8), needed=None):
    fw = FW(needed)
    nc = fw.nc

    def din(name, shape):
        shape = list(shape)
        if shape[0] == DEPTH and not name.startswith("c_"):
            shape[0] = nlayers
        return nc.dram_tensor(name, shape, F32, kind="ExternalInput").ap()

    x_in = din("x", [T, D])
    mix_g = din("mix_norm_g", [DEPTH, D])
    w_in = din("w_in", [DEPTH, D, INC])
    sbq_g = din("sb_q_norm_g", [DEPTH, 64])
    sbk_g = din("sb_k_norm_g", [DEPTH, 64])
    ret_g = din("ret_norm_g", [DEPTH, 512])
    cpw_b = din("conv_pw_b", [DEPTH, 512])
    cdw_w = din("conv_dw_w", [DEPTH, 31, 256])
    cdw_b = din("conv_dw_b", [DEPTH, 256])
    cln_g = din("conv_ln_g", [DEPTH, 256])
    cln_b = din("conv_ln_b", [DEPTH, 256])
    w_out = din("w_out", [DEPTH, D, D])
    mlp_g = din("mlp_norm_g", [DEPTH, D])
    w_ff1 = din("w_ff1", [DEPTH, D, DFF])
    w_ff2 = din("w_ff2", [DEPTH, DFF, D])
    c_ident = din("c_ident", [128, 128])
    c_blk = din("c_blk", [128, 128])
    c_negtri = din("c_negtri", [128, 128])
    c_negones = din("c_negones", [128, 128])
    c_maskneg = din("c_maskneg", [128, 128])
    c_ones = din("c_ones", [128, 128])
    c_rot = din("c_rot", [NTB, 128, 256])
    c_dt = din("c_dt", [128, 8, 128])
    c_qdec = din("c_qdec", [128, 4, 128])
    c_g128 = din("c_g128", [128, 4, 64])
    c_kdec = din("c_kdec", [128, 8, 64])
    y_out = nc.dram_tensor("y", [T, D], F32, kind="ExternalOutput").ap()
    s0 = nc.dram_tensor("scr0", [T, D], F32, kind="Internal").ap()
    s1 = nc.dram_tensor("scr1", [T, D], F32, kind="Internal").ap()
    dbg_out = None
    if dbg is not None:
        dbg_out = nc.dram_tensor("dbg", [128, 8, T], F32, kind="ExternalOutput").ap()

    k_dram = {"x": fw.trk("dx"), "s0": fw.trk("ds0"), "s1": fw.trk("ds1"), "y": fw.trk("dy"), "dbg": fw.trk("ddbg")}

    R1 = fw.sb([128, 8, T], BF16, "R1")
    R2 = fw.sb([128, 8, T], BF16, "R2")
    k_hT = [fw.trk("hT%d" % i) for i in range(NTB)]
    k_mx = [[fw.trk("mx%d_%d" % (c, i)) for i in range(8)] for c in range(8)]
    identb = fw.sb([128, 128], BF16, "identb"); k_identb = fw.trk("identb")
    identf = fw.sb([128, 128], F32, "identf"); k_identf = fw.trk("identf")
    blkb = fw.sb([128, 128], BF16, "blkb"); k_blkb = fw.trk("blkb")
    negtri = fw.sb([128, 128], BF16, "negtri"); k_negtri = fw.trk("negtri")
    negones = fw.sb([128, 128], BF16, "negones"); k_negones = fw.trk("negones")
    maskneg = fw.sb([128, 128], BF16, "maskneg"); k_maskneg = fw.trk("maskneg")
    onesf = fw.sb([128, 128], BF16, "onesf"); k_onesf = fw.trk("onesf")
    zerob = fw.sb([128, 128], BF16, "zerob"); k_zerob = fw.trk("zerob")
    fw.op("pool", lambda e: e.memset(zerob[:], 0.0), writes=[k_zerob])
    fw.dma("pool", identb[:], c_ident[:, :], writes=[k_identb], slot=k_identb)
    fw.dma("sp", identf[:], c_ident[:, :], writes=[k_identf], slot=k_identf)
    fw.dma("pool", blkb[:], c_blk[:, :], writes=[k_blkb], slot=k_blkb)
    fw.dma("pool", negtri[:], c_negtri[:, :], writes=[k_negtri], slot=k_negtri)
    fw.dma("pool", negones[:], c_negones[:, :], writes=[k_negones], slot=k_negones)
    fw.dma("pool", maskneg[:], c_maskneg[:, :], writes=[k_maskneg], slot=k_maskneg)
    fw.dma("pool", onesf[:], c_ones[:, :], writes=[k_onesf], slot=k_onesf)

    def load_cols(dst, k_dst, src_row_ap, n):
        with nc.allow_non_contiguous_dma(reason="tiny param vector"):
            fw.dma("sp", dst, src_row_ap.rearrange("(c p) -> p c", p=128), writes=[k_dst], slot=k_dst)

    def rstd_from(ename_unused, out_ap, in_ap, scale, reads, writes):
        fw.op("act", lambda e: e.activation(out=out_ap, in_=in_ap, func=AF.Ln, scale=scale, bias=EPS),
              reads=reads, writes=writes)
        fw.op("act", lambda e: e.activation(out=out_ap, in_=out_ap, func=AF.Exp, scale=-0.5),
              reads=writes, writes=writes)

    def sigmoid_from(out_ap, in_ap, reads, writes, nbias=None):
        if nbias is None:
            fw.op("act", lambda e: e.activation(out=out_ap, in_=in_ap, func=AF.Exp, scale=-1.0), reads=reads, writes=writes)
        else:
            fw.op("act", lambda e: e.activation(out=out_ap, in_=in_ap, func=AF.Exp, scale=-1.0, bias=nbias), reads=reads, writes=writes)
        fw.op("act", lambda e: e.activation(out=out_ap, in_=out_ap, func=AF.Ln, bias=1.0), reads=writes, writes=writes)
        fw.op("act", lambda e: e.activation(out=out_ap, in_=out_ap, func=AF.Exp, scale=-1.0), reads=writes, writes=writes)

    def norm_transpose_block(xt, k_xt, gt, k_gt, res, dst_ap, k_dst):
        junk, k_junk, st, k_st, hb, k_hb, pt, k_pt = res
        fw.op("act", lambda e: e.activation(out=junk[:], in_=xt, func=AF.Square, accum_out=st[:, 0:1]),
              reads=[k_xt], writes=[k_junk, k_st])
        rstd_from("act", st[:, 1:2], st[:, 0:1], 1.0 / D, [k_st], [k_st])
        fw.op("dve", lambda e: e.tensor_scalar(out=hb[:], in0=xt, scalar1=st[:, 1:2], scalar2=None, op0=ALU.mult),
              reads=[k_xt, k_st], writes=[k_hb])
        for dc in range(8):
            fw.op("pe", lambda e, dc=dc: e.transpose(out=pt[:, dc, :], in_=hb[:, dc * 128:(dc + 1) * 128], identity=identb[:]),
                  reads=[k_hb, k_identb], writes=[k_pt])
        fw.op("dve", lambda e: e.tensor_tensor(out=dst_ap, in0=pt[:], in1=gt[:].unsqueeze(2).broadcast_to([128, 8, 128]), op=ALU.mult),
              reads=[k_pt, k_gt], writes=[k_dst])

    def phase_A(l, xin, k_xin):
        m = fw.mark()
        gt = fw.sb([128, 8], F32); k_gt = fw.trk()
        load_cols(gt[:], k_gt, mix_g[l, :], 8)
        nb = 3
        xts = [(fw.sb([128, D], F32), fw.trk()) for _ in range(nb)]
        ress = []
        for _ in range(2):
            ress.append((fw.sb([128, D], BF16), fw.trk(), fw.sb([128, 2], F32), fw.trk(),
                         fw.sb([128, D], BF16), fw.trk(), fw.ps([128, 8, 128], BF16), fw.trk()))
        for tb in range(NTB):
            xt, k_xt = xts[tb % nb]
            fw.dma("sp", xt[:], xin[tb * 128:(tb + 1) * 128, :], reads=[k_xin], writes=[k_xt], slot=k_xt)
            norm_transpose_block(xt[:], k_xt, gt, k_gt, ress[tb % 2], R1[:, :, tb * 128:(tb + 1) * 128], k_hT[tb])
        fw.release(m)

    def phase_SB(l, hp):
        m = fw.mark()
        qT = fw.sb([128, T], BF16); kT = fw.sb([128, 2, T], BF16); vv = fw.sb([128, NTB, 128], BF16)
        k_kT0 = fw.trk()
        fw.op("pool", lambda e: e.memset(kT[:], 0.0), writes=[k_kT0])
        k_qT = [fw.trk() for _ in range(8)]; k_kT = [fw.trk() for _ in range(8)]; k_v = [fw.trk() for _ in range(8)]
        gq = fw.sb([128, 1], F32); k_gq = fw.trk(); gk = fw.sb([128, 1], F32); k_gk = fw.trk()
        with nc.allow_non_contiguous_dma(reason="tiny param vector"):
            for half in range(2):
                fw.dma("sp", gq[half * 64:(half + 1) * 64, :], sbq_g[l, :].rearrange("(p o) -> p o", o=1), writes=[k_gq], slot=k_gq)
                fw.dma("sp", gk[half * 64:(half + 1) * 64, :], sbk_g[l, :].rearrange("(p o) -> p o", o=1), writes=[k_gk], slot=k_gk)
        fw.op("dve", lambda e: e.tensor_scalar(out=gq[:], in0=gq[:], scalar1=0.125, scalar2=None, op0=ALU.mult), reads=[k_gq], writes=[k_gq])
        wts = [(fw.sb([128, 8, 128], BF16), fw.trk()) for _ in range(2)]
        pbank = [(fw.ps([128, 512], F32), fw.trk()) for _ in range(2)]
        pss = [(fw.ps([128, 512], F32), fw.trk()) for _ in range(2)]
        sqb = [(fw.sb([128, 512], BF16), fw.trk()) for _ in range(2)]
        rsd = [(fw.sb([128, 512], F32), fw.trk()) for _ in range(2)]
        it = 0
        for which in range(2):
            col0 = (0 if which == 0 else 256) + hp * 128
            wt, k_wt = wts[which]
            fw.dma("pool", wt[:], w_in[l, :, col0:col0 + 128].rearrange("(dc p) c -> p dc c", p=128), writes=[k_wt], slot=k_wt)
            dst = qT if which == 0 else kT
            kd = k_qT if which == 0 else k_kT
            gcol, k_gcol = (gq, k_gq) if which == 0 else (gk, k_gk)
            for tt in range(8):
                pq, k_pq = pbank[it % 2]; ps2, k_ps2 = pss[it % 2]; sq, k_sq = sqb[it % 2]; rs, k_rs = rsd[it % 2]
                it += 1
                for dc in range(8):
                    fw.op("pe", lambda e, dc=dc: e.matmul(pq[:], lhsT=wt[:, dc, :], rhs=R1[:, dc, tt * 512:(tt + 1) * 512], start=(dc == 0), stop=(dc == 7)),
                          reads=[k_wt] + k_hT[tt * 4:(tt + 1) * 4], writes=[k_pq])
                fw.op("act", lambda e: e.activation(out=sq[:], in_=pq[:], func=AF.Square), reads=[k_pq], writes=[k_sq])
                fw.op("pe", lambda e: e.matmul(ps2[:], lhsT=blkb[:], rhs=sq[:], start=True, stop=True), reads=[k_blkb, k_sq], writes=[k_ps2])
                rstd_from("act", rs[:], ps2[:], 1.0 / 64, [k_ps2], [k_rs])
                if which == 0:
                    fw.op("dve", lambda e: e.scalar_tensor_tensor(out=dst[:, tt * 512:(tt + 1) * 512], in0=pq[:], scalar=gcol[:, 0:1], in1=rs[:], op0=ALU.mult, op1=ALU.mult),
                          reads=[k_pq, k_gcol, k_rs], writes=[kd[tt]])
                else:
                    for hh_ in range(2):
                        fw.op("dve", lambda e, hh_=hh_: e.scalar_tensor_tensor(out=kT[hh_ * 64:(hh_ + 1) * 64, hh_, tt * 512:(tt + 1) * 512], in0=pq[hh_ * 64:(hh_ + 1) * 64, :],
                                                                              scalar=gcol[hh_ * 64:(hh_ + 1) * 64, 0:1], in1=rs[hh_ * 64:(hh_ + 1) * 64, :], op0=ALU.mult, op1=ALU.mult),
                              reads=[k_pq, k_gcol, k_rs, k_kT0], writes=[kd[tt]])
        wt, k_wt = wts[0]
        col0 = 512 + hp * 128
        fw.dma("pool", wt[:], w_in[l, :, col0:col0 + 128].rearrange("(dc p) c -> p dc c", p=128), writes=[k_wt], slot=k_wt)
        for tb in range(NTB):
            pq, k_pq = pbank[tb % 2]
            for dc in range(8):
                fw.op("pe", lambda e, dc=dc: e.matmul(pq[:, 0:128], lhsT=R1[:, dc, tb * 128:(tb + 1) * 128], rhs=wt[:, dc, :], start=(dc == 0), stop=(dc == 7)),
                      reads=[k_wt, k_hT[tb]], writes=[k_pq])
            fw.op("act", lambda e: e.activation(out=vv[:, tb, :], in_=pq[:, 0:128], func=AF.Copy), reads=[k_pq], writes=[k_v[tb // 4]])

        if dbg == 'sbproj':
            fw.release(m)
            return
        pz = [(fw.ps([128, 512], F32), fw.trk()) for _ in range(2)] + [pss[0]]
        plw = [(fw.ps([128, 512], F32), fw.trk()) for _ in range(2)] + [pss[1]]
        Eb = [(fw.sb([128, 512], F32), fw.trk()) for _ in range(3)]
        Lb = [(fw.sb([128, 512], BF16), fw.trk()) for _ in range(3)]
        Rb = fw.sb([128, 3, 512], BF16); k_R = [fw.trk() for _ in range(3)]
        wb = [(fw.sb([128, 512], BF16), fw.trk()) for _ in range(2)]
        Xb = [(fw.sb([128, 512], F32), fw.trk()) for _ in range(2)]
        for hh in range(2):
            p0 = hh * 64
            for qt in range(8):
                po, k_po = pbank[(hh * 8 + qt) % 2]
                fw.op("pool", lambda e: e.memset(Rb[:], 0.0), writes=k_R)
                fw.op("pe", lambda e: e.matmul(po[:, :], lhsT=zerob[:], rhs=qT[:, qt * 512:(qt + 1) * 512], start=True, stop=False),
                      reads=[k_zerob, k_qT[qt]], writes=[k_po])
                blocks = list(range(4 * qt + 3, -1, -1))
                nblk = len(blocks)
                q0 = qt * 512

                def zmm(dstp, b, last_stop):
                    kb = blocks[b]
                    kl = kb - 4 * qt
                    ksl = kT[:, hh, kb * 128:(kb + 1) * 128]
                    rk = [k_kT[kb // 4], k_qT[qt]]
                    c0 = kl * 128 if kl >= 0 else 0
                    fw.op("pe", lambda e: e.matmul(dstp[0][:, c0:512], lhsT=ksl, rhs=qT[:, q0 + c0:q0 + 512], start=True, stop=(last_stop and kl < 0)),
                          reads=rk, writes=[dstp[1]])
                    if kl >= 0:
                        fw.op("pe", lambda e: e.matmul(dstp[0][:, c0:c0 + 128], lhsT=identb[:], rhs=maskneg[:], start=False, stop=last_stop),
                              reads=[k_identb, k_maskneg], writes=[dstp[1]])
                    return c0

                def stageA1(b):
                    c0 = zmm(pz[b % 3], b, True)
                    E, k_E = Eb[b % 3]
                    fw.op("act", lambda e: e.activation(out=E[:, c0:], in_=pz[b % 3][0][:, c0:], func=AF.Exp), reads=[pz[b % 3][1]], writes=[k_E])

                def stageA2(b):
                    kl = blocks[b] - 4 * qt
                    c0 = kl * 128 if kl >= 0 else 0
                    E, k_E = Eb[b % 3]; L, k_L = Lb[b % 3]
                    fw.op("act", lambda e: e.activation(out=L[:, c0:], in_=E[:, c0:], func=AF.Ln, bias=1.0), reads=[k_E], writes=[k_L])
                    if b + 1 < nblk:
                        fw.op("pool", lambda e: e.tensor_tensor(out=Rb[:, (b + 1) % 3, c0:], in0=Rb[:, b % 3, c0:], in1=L[:, c0:], op=ALU.add),
                              reads=[k_R[b % 3], k_L], writes=[k_R[(b + 1) % 3]])

                def stageB1(b):
                    kl = blocks[b] - 4 * qt
                    c0 = kl * 128 if kl >= 0 else 0
                    L, k_L = Lb[b % 3]
                    fw.op("pe", lambda e: e.matmul(plw[b % 3][0][:, c0:], lhsT=negtri[:], rhs=L[:, c0:], start=True, stop=(b == 0)),
                          reads=[k_negtri, k_L], writes=[plw[b % 3][1]])
                    if b > 0:
                        fw.op("pe", lambda e: e.matmul(plw[b % 3][0][:, c0:], lhsT=negones[:], rhs=Rb[:, b % 3, c0:], start=False, stop=True),
                              reads=[k_negones, k_R[b % 3]], writes=[plw[b % 3][1]])
                    X, k_X = Xb[b % 2]
                    fw.op("act", lambda e: e.activation(out=X[:, c0:], in_=plw[b % 3][0][:, c0:], func=AF.Exp), reads=[plw[b % 3][1]], writes=[k_X])

                def stageB2(b):
                    kl = blocks[b] - 4 * qt
                    c0 = kl * 128 if kl >= 0 else 0
                    E, k_E = Eb[b % 3]; X, k_X = Xb[b % 2]; w, k_w = wb[b % 2]
                    fw.op("dve", lambda e: e.tensor_tensor(out=w[:, c0:], in0=E[:, c0:], in1=X[:, c0:], op=ALU.mult), reads=[k_E, k_X], writes=[k_w])

                def stageC(b):
                    kb = blocks[b]
                    kl = kb - 4 * qt
                    w, k_w = wb[b % 2]
                    last = (b == nblk - 1)
                    vsl = vv[:, kb, :]
                    rd = [k_v[kb // 4], k_w]
                    c0 = kl * 128 if kl >= 0 else 0
                    fw.op("pe", lambda e: e.matmul(po[:, c0:512], lhsT=vsl, rhs=w[:, c0:512], start=False, stop=last), reads=rd, writes=[k_po])
                    if last:
                        fw.op("dve", lambda e: e.tensor_copy(out=R2[p0:p0 + 64, hp, q0:q0 + 512], in_=po[p0:p0 + 64, :]), reads=[k_po], writes=[k_mx[hp][qt]])

                for i in range(nblk + 2):
                    if i < nblk:
                        stageA1(i)
                    if 0 <= i - 1 < nblk:
                        stageB1(i - 1)
                    if i < nblk:
                        stageA2(i)
                    if 0 <= i - 1 < nblk:
                        stageB2(i - 1)
                    if 0 <= i - 2 < nblk:
                        stageC(i - 2)
        fw.release(m)

    def phase_RET(l):
        m = fw.mark()
        wr = fw.sb([128, 8, 2048], BF16); k_wr = [fw.trk() for _ in range(4)]
        for i in range(4):
            fw.dma("pool", wr[:, :, i * 512:(i + 1) * 512], w_in[l, :, 768 + i * 512:768 + (i + 1) * 512].rearrange("(dc p) c -> p dc c", p=128),
                   writes=[k_wr[i]], slot=k_wr[i])
        dtab = fw.sb([128, 8, 128], F32); k_dtab = fw.trk()
        qdec = fw.sb([128, 4, 128], F32); k_qdec = fw.trk()
        kdec = fw.sb([128, 8, 64], F32); k_kdec = fw.trk()
        g128 = fw.sb([128, 4, 64], F32); k_g128 = fw.trk()
        rgb = fw.sb([128, 512], F32); k_rgb = fw.trk()
        fw.dma("sp", dtab[:], c_dt[:, :, :], writes=[k_dtab], slot=k_dtab)
        fw.dma("sp", qdec[:], c_qdec[:, :, :], writes=[k_qdec], slot=k_qdec)
        fw.dma("sp", kdec[:], c_kdec[:, :, :], writes=[k_kdec], slot=k_kdec)
        fw.dma("sp", g128[:], c_g128[:, :, :], writes=[k_g128], slot=k_g128)
        fw.dma("sp", rgb[:], ret_g[l, :].partition_broadcast(128), writes=[k_rgb], slot=k_rgb)
        rot = [(fw.sb([128, 256], F32), fw.trk()) for _ in range(2)]
        pA = (fw.ps([128, 512], F32), fw.trk()); pB = (fw.ps([128, 512], F32), fw.trk())
        ptq = (fw.ps([128, 4, 128], BF16), fw.trk()); ptk = (fw.ps([128, 4, 128], BF16), fw.trk())
        pS = [(fw.ps([128, 4, 128], F32), fw.trk()) for _ in range(2)]
        pO = (fw.ps([128, 512], F32), fw.trk()); pKV = (fw.ps([128, 4, 128], F32), fw.trk())
        tA = [(fw.sb([128, 512], F32), fw.trk()) for _ in range(2)]
        tB = [(fw.sb([128, 512], F32), fw.trk()) for _ in range(2)]
        rq = (fw.sb([128, 512], BF16), fw.trk()); rk = (fw.sb([128, 512], BF16), fw.trk())
        rkd = (fw.sb([128, 512], BF16), fw.trk()); vsb = (fw.sb([128, 512], BF16), fw.trk())
        gsil = (fw.sb([128, 512], F32), fw.trk())
        qTs = (fw.sb([128, 4, 128], BF16), fw.trk()); qdT = (fw.sb([128, 4, 128], BF16), fw.trk())
        kTz = (fw.sb([128, 4, 2, 128], BF16), fw.trk())
        fw.op("pool", lambda e: e.memset(kTz[0][:], 0.0), writes=[kTz[1]])
        AT = (fw.sb([128, 8, 128], BF16), fw.trk())
        sqo = (fw.sb([128, 512], F32), fw.trk()); ssr = (fw.sb([128, 8], F32), fw.trk())
        t1 = (fw.sb([128, 512], F32), fw.trk()); ytm = (fw.sb([128, 512], BF16), fw.trk())
        stf = (fw.sb([128, 4, 64], F32), fw.trk()); stb = (fw.sb([128, 4, 2, 64], BF16), fw.trk()); stt = (fw.sb([128, 4, 64], F32), fw.trk())
        fw.op("pool", lambda e: e.memset(stf[0][:], 0.0), writes=[stf[1]])
        fw.op("pool", lambda e: e.memset(stb[0][:], 0.0), writes=[stb[1]])

        def proj(pdst, wi, tb):
            for dc in range(8):
                fw.op("pe", lambda e, dc=dc: e.matmul(pdst[0][:], lhsT=R1[:, dc, tb * 128:(tb + 1) * 128], rhs=wr[:, dc, wi * 512:(wi + 1) * 512], start=(dc == 0), stop=(dc == 7)),
                      reads=[k_hT[tb], k_wr[wi]], writes=[pdst[1]])

        def v3(ap):
            return ap.rearrange("p (h e) -> p h e", h=8)

        def rotary(psrc, off, ta, tb_, dst, rt, k_rt):
            cos2 = rt[:, off:off + 64].unsqueeze(1).broadcast_to([128, 8, 64])
            s2 = rt[:, off + 64:off + 128]
            s2e = s2.rearrange("p (i two) -> p i two", two=2)[:, :, 0].unsqueeze(1).broadcast_to([128, 8, 32])
            s2o = s2.rearrange("p (i two) -> p i two", two=2)[:, :, 1].unsqueeze(1).broadcast_to([128, 8, 32])
            ps4 = psrc[0][:].rearrange("p (h i two) -> p h i two", h=8, two=2)
            tb4 = tb_[0][:].rearrange("p (h i two) -> p h i two", h=8, two=2)
            fw.op("dve", lambda e: e.tensor_tensor(out=v3(ta[0][:]), in0=v3(psrc[0][:]), in1=cos2, op=ALU.mult), reads=[psrc[1], k_rt], writes=[ta[1]])
            fw.op("dve", lambda e: e.tensor_tensor(out=tb4[:, :, :, 0], in0=ps4[:, :, :, 1], in1=s2e, op=ALU.mult), reads=[psrc[1], k_rt], writes=[tb_[1]])
            fw.op("dve", lambda e: e.tensor_tensor(out=tb4[:, :, :, 1], in0=ps4[:, :, :, 0], in1=s2o, op=ALU.mult), reads=[psrc[1], k_rt], writes=[tb_[1]])
            fw.op("pool", lambda e: e.tensor_tensor(out=dst[0][:], in0=ta[0][:], in1=tb_[0][:], op=ALU.add), reads=[ta[1], tb_[1]], writes=[dst[1]])

        gsil2 = [gsil, (fw.sb([128, 512], F32), fw.trk())]

        def r_head(n):
            rt, k_rt = rot[n % 2]
            fw.dma("sp", rt[:], c_rot[n, :, :], writes=[k_rt], slot=k_rt)
            proj(pA, 0, n)
            rotary(pA, 0, tA[0], tB[0], rq, rt, k_rt)
            proj(pB, 1, n)
            rotary(pB, 128, tA[1], tB[1], rk, rt, k_rt)
            fw.op("pool", lambda e: e.tensor_tensor(out=v3(rkd[0][:]), in0=v3(rk[0][:]), in1=kdec[:], op=ALU.mult), reads=[rk[1], k_kdec], writes=[rkd[1]])
            proj(pA, 2, n)
            fw.op("act", lambda e: e.activation(out=vsb[0][:], in_=pA[0][:], func=AF.Copy), reads=[pA[1]], writes=[vsb[1]])
            proj(pB, 3, n)
            sigmoid_from(gsil2[n % 2][0][:], pB[0][:], [pB[1]], [gsil2[n % 2][1]])
            fw.op("dve", lambda e: e.tensor_tensor(out=gsil2[n % 2][0][:], in0=pB[0][:], in1=gsil2[n % 2][0][:], op=ALU.mult), reads=[pB[1], gsil2[n % 2][1]], writes=[gsil2[n % 2][1]])
            fw.op("pool", lambda e: e.tensor_tensor(out=gsil2[n % 2][0][:], in0=gsil2[n % 2][0][:], in1=rgb[:], op=ALU.mult), reads=[gsil2[n % 2][1], k_rgb], writes=[gsil2[n % 2][1]])
            for pr in range(4):
                fw.op("pe", lambda e, pr=pr: e.transpose(out=ptq[0][:, pr, :], in_=rq[0][:, pr * 128:(pr + 1) * 128], identity=identb[:]), reads=[rq[1], k_identb], writes=[ptq[1]])
            for pr in range(4):
                fw.op("pe", lambda e, pr=pr: e.transpose(out=ptk[0][:, pr, :], in_=rk[0][:, pr * 128:(pr + 1) * 128], identity=identb[:]), reads=[rk[1], k_identb], writes=[ptk[1]])
            fw.op("act", lambda e: e.activation(out=qTs[0][:], in_=ptq[0][:], func=AF.Copy), reads=[ptq[1]], writes=[qTs[1]])
            fw.op("dve", lambda e: e.tensor_tensor(out=qdT[0][:], in0=ptq[0][:], in1=qdec[:], op=ALU.mult), reads=[ptq[1], k_qdec], writes=[qdT[1]])
            for hh in range(2):
                fw.op("act", lambda e, hh=hh: e.activation(out=kTz[0][hh * 64:(hh + 1) * 64, :, hh, :], in_=ptk[0][hh * 64:(hh + 1) * 64, :, :], func=AF.Copy), reads=[ptk[1]], writes=[kTz[1]])

        def r_mid(n):
            for h in range(8):
                pr, p0 = h // 2, (h % 2) * 64
                fw.op("pe", lambda e, h=h, pr=pr: e.matmul(pS[h // 4][0][:, h % 4, :], lhsT=kTz[0][:, pr, h % 2, :], rhs=qTs[0][:, pr, :], start=True, stop=True),
                      reads=[kTz[1], qTs[1]], writes=[pS[h // 4][1]])
            for half in range(2):
                fw.op("dve", lambda e, half=half: e.tensor_tensor(out=AT[0][:, half * 4:(half + 1) * 4, :], in0=pS[half][0][:], in1=dtab[:, half * 4:(half + 1) * 4, :], op=ALU.mult),
                      reads=[pS[half][1], k_dtab], writes=[AT[1]])
            for h in range(8):
                pr, p0 = h // 2, (h % 2) * 64
                fw.op("pe", lambda e, h=h: e.matmul(pO[0][:, h * 64:(h + 1) * 64], lhsT=AT[0][:, h, :], rhs=vsb[0][:, h * 64:(h + 1) * 64], start=True, stop=False),
                      reads=[AT[1], vsb[1]], writes=[pO[1]])
                fw.op("pe", lambda e, h=h, pr=pr: e.matmul(pO[0][:, h * 64:(h + 1) * 64], lhsT=qdT[0][:, pr, :], rhs=stb[0][:, pr, h % 2, :], start=False, stop=True),
                      reads=[qdT[1], stb[1]], writes=[pO[1]])
            for pr in range(4):
                fw.op("pe", lambda e, pr=pr: e.matmul(pKV[0][:, pr, :], lhsT=rkd[0][:, pr * 128:(pr + 1) * 128], rhs=vsb[0][:, pr * 128:(pr + 1) * 128], start=True, stop=True),
                      reads=[rkd[1], vsb[1]], writes=[pKV[1]])
            fw.op("pool", lambda e: e.tensor_tensor(out=stt[0][:], in0=stf[0][:], in1=g128[:], op=ALU.mult), reads=[stf[1], k_g128], writes=[stt[1]])
            for half in range(2):
                p0 = half * 64
                fw.op("dve", lambda e, p0=p0: e.tensor_tensor(out=stf[0][p0:p0 + 64, :, :], in0=stt[0][p0:p0 + 64, :, :], in1=pKV[0][p0:p0 + 64, :, p0:p0 + 64], op=ALU.add),
                      reads=[stt[1], pKV[1]], writes=[stf[1]])
            for hh in range(2):
                fw.op("act", lambda e, hh=hh: e.activation(out=stb[0][hh * 64:(hh + 1) * 64, :, hh, :], in_=stf[0][hh * 64:(hh + 1) * 64, :, :], func=AF.Copy), reads=[stf[1]], writes=[stb[1]])

        def r_tail(n):
            fw.op("act", lambda e: e.activation(out=sqo[0][:], in_=pO[0][:], func=AF.Square), reads=[pO[1]], writes=[sqo[1]])
            fw.op("dve", lambda e: e.tensor_reduce(out=ssr[0][:], in_=v3(sqo[0][:]), axis=AX.X, op=ALU.add), reads=[sqo[1]], writes=[ssr[1]])
            rstd_from("act", ssr[0][:], ssr[0][:], 1.0 / 64, [ssr[1]], [ssr[1]])
            fw.op("dve", lambda e: e.tensor_tensor(out=v3(t1[0][:]), in0=v3(pO[0][:]), in1=ssr[0][:].unsqueeze(2).broadcast_to([128, 8, 64]), op=ALU.mult),
                  reads=[pO[1], ssr[1]], writes=[t1[1]])
            fw.op("pool", lambda e: e.tensor_tensor(out=ytm[0][:], in0=t1[0][:], in1=gsil2[n % 2][0][:], op=ALU.mult), reads=[t1[1], gsil2[n % 2][1]], writes=[ytm[1]])
            for pr in range(4):
                fw.op("pe", lambda e, pr=pr: e.transpose(out=ptq[0][:, pr, :], in_=ytm[0][:, pr * 128:(pr + 1) * 128], identity=identb[:]), reads=[ytm[1], k_identb], writes=[ptq[1]])
            fw.op("act", lambda e: e.activation(out=R2[:, 2:6, n * 128:(n + 1) * 128], in_=ptq[0][:], func=AF.Copy), reads=[ptq[1]],
                  writes=[k_mx[2 + pr][n // 4] for pr in range(4)])

        r_head(0)
        r_mid(0)
        for n in range(NTB):
            if n + 1 < NTB:
                r_head(n + 1)
            r_tail(n)
            if n + 1 < NTB:
                r_mid(n + 1)
        fw.release(m)

    def phase_CONV(l):
        m = fw.mark()
        PAD = 30
        hg = fw.sb([128, 2, PAD + T], BF16); k_hg = [[fw.trk() for _ in range(9)] for _ in range(2)]
        dw31 = fw.sb([128, 256], F32); k_dw31 = fw.trk()
        fw.op("pool", lambda e: e.memset(dw31[:], 0.0), writes=[k_dw31])
        fw.dma("sp", dw31[0:31, :], cdw_w[l, :, :], writes=[k_dw31], slot=k_dw31)
        dwb16 = fw.sb([128, 256], BF16); k_dwb16 = fw.trk()
        fw.op("dve", lambda e: e.tensor_copy(out=dwb16[:], in_=dw31[:]), reads=[k_dw31], writes=[k_dwb16])
        wcol = fw.sb([128, 2, 31], F32); k_wcol = fw.trk()
        diagw = fw.sb([128, 2, 31, 128], BF16); k_diagw = fw.trk()
        cols = {}
        for name, src in (("ba", cpw_b[l, 0:256]), ("bg", cpw_b[l, 256:512]), ("dwb", cdw_b[l, :]), ("lng", cln_g[l, :]), ("lnb", cln_b[l, :])):
            t = fw.sb([128, 2], F32); k = fw.trk()
            load_cols(t[:], k, src, 2)
            cols[name] = (t, k)
        fw.op("dve", lambda e: e.tensor_scalar(out=cols["bg"][0][:], in0=cols["bg"][0][:], scalar1=-1.0, scalar2=None, op0=ALU.mult), reads=[cols["bg"][1]], writes=[cols["bg"][1]])
        pw = [(fw.ps([128, 512], F32), fw.trk()) for _ in range(2)]
        ptw = (fw.ps([128, 2, 128], BF16), fw.trk())
        for cc in range(2):
            fw.op("pe", lambda e, cc=cc: e.transpose(out=ptw[0][:, cc, :], in_=dwb16[:, cc * 128:(cc + 1) * 128], identity=identb[:]),
                  reads=[k_dwb16, k_identb], writes=[ptw[1]])
        fw.op("dve", lambda e: e.tensor_copy(out=wcol[:], in_=ptw[0][:, :, 0:31]), reads=[ptw[1]], writes=[k_wcol])
        for cc in range(2):
            for j in range(31):
                fw.op("dve", lambda e, cc=cc, j=j: e.tensor_scalar(out=diagw[:, cc, j, :], in0=identf[:], scalar1=wcol[:, cc, j:j + 1], scalar2=None, op0=ALU.mult),
                      reads=[k_identf, k_wcol], writes=[k_diagw])
        for cc in range(2):
            fw.op("pool", lambda e, cc=cc: e.memset(hg[:, cc, 0:PAD], 0.0), writes=[k_hg[cc][0]])
        if CONV_STOP < 2:
            fw.release(m)
            return
        wts = [(fw.sb([128, 8, 128], BF16), fw.trk()) for _ in range(2)]
        pa = [(fw.ps([128, 512], F32), fw.trk()) for _ in range(2)]
        pg = pw
        sg = [(fw.sb([128, 512], F32), fw.trk()) for _ in range(2)]
        it = 0
        for cc in range(2):
            fw.dma("pool", wts[0][0][:], w_in[l, :, 2816 + cc * 128:2816 + (cc + 1) * 128].rearrange("(dc p) c -> p dc c", p=128), writes=[wts[0][1]], slot=wts[0][1])
            fw.dma("pool", wts[1][0][:], w_in[l, :, 3072 + cc * 128:3072 + (cc + 1) * 128].rearrange("(dc p) c -> p dc c", p=128), writes=[wts[1][1]], slot=wts[1][1])
            for tt in range(8):
                a_, g_, s_ = pa[it % 2], pg[it % 2], sg[it % 2]
                it += 1
                for dc in range(8):
                    fw.op("pe", lambda e, dc=dc: e.matmul(a_[0][:], lhsT=wts[0][0][:, dc, :], rhs=R1[:, dc, tt * 512:(tt + 1) * 512], start=(dc == 0), stop=(dc == 7)),
                          reads=[wts[0][1]] + k_hT[tt * 4:(tt + 1) * 4], writes=[a_[1]])
                for dc in range(8):
                    fw.op("pe", lambda e, dc=dc: e.matmul(g_[0][:], lhsT=wts[1][0][:, dc, :], rhs=R1[:, dc, tt * 512:(tt + 1) * 512], start=(dc == 0), stop=(dc == 7)),
                          reads=[wts[1][1]] + k_hT[tt * 4:(tt + 1) * 4], writes=[g_[1]])
                sigmoid_from(s_[0][:], g_[0][:], [g_[1], cols["bg"][1]], [s_[1]], nbias=cols["bg"][0][:, cc:cc + 1])
                fw.op("dve", lambda e: e.scalar_tensor_tensor(out=hg[:, cc, PAD + tt * 512:PAD + (tt + 1) * 512], in0=a_[0][:], scalar=cols["ba"][0][:, cc:cc + 1], in1=s_[0][:], op0=ALU.add, op1=ALU.mult),
                      reads=[a_[1], cols["ba"][1], s_[1]], writes=[k_hg[cc][1 + tt]])
        if CONV_STOP < 3:
            fw.release(m)
            return
        pc = [(fw.ps([128, 512], F32), fw.trk()) for _ in range(2)]
        yc = [(fw.sb([128, 512], F32), fw.trk()) for _ in range(2)]
        ysq = [(fw.sb([128, 512], BF16), fw.trk()) for _ in range(2)]
        ycb = [(fw.sb([128, 512], BF16), fw.trk()) for _ in range(2)]
        mean = (fw.sb([128, 512], F32), fw.trk()); var = (fw.sb([128, 512], F32), fw.trk())
        tt1 = (fw.sb([128, 512], F32), fw.trk()); tt2 = (fw.sb([128, 512], F32), fw.trk())
        for tt in range(8):
            for cc in range(2):
                rd = [k_diagw, k_hg[cc][1 + tt], k_hg[cc][tt]]
                for j in range(31):
                    fw.op("pe", lambda e, cc=cc, j=j: e.matmul(pc[cc][0][:], lhsT=diagw[:, cc, j, :], rhs=hg[:, cc, tt * 512 + j:tt * 512 + j + 512], start=(j == 0), stop=(j == 30)),
                          reads=rd, writes=[pc[cc][1]])
                fw.op("dve", lambda e, cc=cc: e.tensor_scalar(out=yc[cc][0][:], in0=pc[cc][0][:], scalar1=cols["dwb"][0][:, cc:cc + 1], scalar2=None, op0=ALU.add), reads=[pc[cc][1], cols["dwb"][1]], writes=[yc[cc][1]])
                fw.op("act", lambda e, cc=cc: e.activation(out=ysq[cc][0][:], in_=yc[cc][0][:], func=AF.Square), reads=[yc[cc][1]], writes=[ysq[cc][1]])
                fw.op("act", lambda e, cc=cc: e.activation(out=ycb[cc][0][:], in_=yc[cc][0][:], func=AF.Copy), reads=[yc[cc][1]], writes=[ycb[cc][1]])
            if CONV_STOP < 4:
                continue
            for cc in range(2):
                fw.op("pe", lambda e, cc=cc: e.matmul(pw[0][0][:], lhsT=onesf[:], rhs=ycb[cc][0][:], start=(cc == 0), stop=(cc == 1)), reads=[k_onesf, ycb[cc][1]], writes=[pw[0][1]])
            for cc in range(2):
                fw.op("pe", lambda e, cc=cc: e.matmul(pw[1][0][:], lhsT=onesf[:], rhs=ysq[cc][0][:], start=(cc == 0), stop=(cc == 1)), reads=[k_onesf, ysq[cc][1]], writes=[pw[1][1]])
            if CONV_STOP < 5:
                continue
            fw.op("act", lambda e: e.activation(out=mean[0][:], in_=pw[0][0][:], func=AF.Copy), reads=[pw[0][1]], writes=[mean[1]])
            fw.op("dve", lambda e: e.tensor_tensor(out=var[0][:], in0=mean[0][:], in1=mean[0][:], op=ALU.mult), reads=[mean[1]], writes=[var[1]])
            fw.op("dve", lambda e: e.tensor_tensor(out=var[0][:], in0=pw[1][0][:], in1=var[0][:], op=ALU.subtract), reads=[pw[1][1], var[1]], writes=[var[1]])
            rstd_from("act", var[0][:], var[0][:], 1.0, [var[1]], [var[1]])
            for cc in range(2):
                fw.op("dve", lambda e, cc=cc: e.tensor_tensor(out=tt1[0][:], in0=yc[cc][0][:], in1=mean[0][:], op=ALU.subtract), reads=[yc[cc][1], mean[1]], writes=[tt1[1]])
                fw.op("dve", lambda e, cc=cc: e.tensor_tensor(out=tt1[0][:], in0=tt1[0][:], in1=var[0][:], op=ALU.mult), reads=[tt1[1], var[1]], writes=[tt1[1]])
                fw.op("dve", lambda e, cc=cc: e.tensor_scalar(out=tt1[0][:], in0=tt1[0][:], scalar1=cols["lng"][0][:, cc:cc + 1], scalar2=cols["lnb"][0][:, cc:cc + 1], op0=ALU.mult, op1=ALU.add),
                      reads=[tt1[1], cols["lng"][1], cols["lnb"][1]], writes=[tt1[1]])
                sigmoid_from(tt2[0][:], tt1[0][:], [tt1[1]], [tt2[1]])
                fw.op("pool", lambda e, cc=cc: e.tensor_tensor(out=R2[:, 6 + cc, tt * 512:(tt + 1) * 512], in0=tt1[0][:], in1=tt2[0][:], op=ALU.mult),
                      reads=[tt1[1], tt2[1]], writes=[k_mx[6 + cc][tt]])
        fw.release(m)

    def phase_C1(l, xin, k_xin, xmid, k_xmid, k_w1):
        m = fw.mark()
        wo = fw.sb([128, 8, D], BF16); k_wo = [fw.trk() for _ in range(2)]
        for i in range(2):
            fw.dma("pool", wo[:, :, i * 512:(i + 1) * 512], w_out[l, :, i * 512:(i + 1) * 512].rearrange("(cc p) d -> p cc d", p=128), writes=[k_wo[i]], slot=k_wo[i])
        for i in range(8):
            fw.dma("pool", R1[:, :, i * 512:(i + 1) * 512], w_ff1[l, :, i * 512:(i + 1) * 512].rearrange("(dc p) f -> p dc f", p=128), writes=[k_w1[i]], slot=k_w1[i])
        xts = [(fw.sb([128, D], F32), fw.trk()) for _ in range(3)]
        xos = [(fw.sb([128, D], F32), fw.trk()) for _ in range(3)]
        py = [(fw.ps([128, D], F32), fw.trk()) for _ in range(2)]
        for tb in range(NTB):
            xt, k_xt = xts[tb % 3]; xo, k_xo = xos[tb % 3]; p_, k_p = py[tb % 2]
            fw.dma("sp", xt[:], xin[tb * 128:(tb + 1) * 128, :], reads=[k_xin], writes=[k_xt], slot=k_xt)
            for half in range(2):
                for cc in range(8):
                    fw.op("pe", lambda e, cc=cc, half=half: e.matmul(p_[:, half * 512:(half + 1) * 512], lhsT=R2[:, cc, tb * 128:(tb + 1) * 128], rhs=wo[:, cc, half * 512:(half + 1) * 512], start=(cc == 0), stop=(cc == 7)),
                          reads=[k_mx[cc][tb // 4], k_wo[half]], writes=[k_p])
            for half in range(2):
                fw.op("dve", lambda e, half=half: e.tensor_tensor(out=xo[:, half * 512:(half + 1) * 512], in0=p_[:, half * 512:(half + 1) * 512], in1=xt[:, half * 512:(half + 1) * 512], op=ALU.add),
                      reads=[k_p, k_xt], writes=[k_xo])
            fw.dma("sp", xmid[tb * 128:(tb + 1) * 128, :], xo[:], reads=[k_xo], writes=[k_xmid], slot=k_xo)
        fw.release(m)

    def phase_C2(l, xmid, k_xmid, xout, k_xout, k_w1):
        m = fw.mark()
        W1 = R1
        W2 = R2[:].rearrange("p c t -> p (c t)").rearrange("p (fc d) -> p fc d", d=D)
        k_w2 = [fw.trk() for _ in range(8)]
        for i in range(8):
            fw.dma("pool", W2[:, i * 4:(i + 1) * 4, :], w_ff2[l, i * 512:(i + 1) * 512, :].rearrange("(fc p) d -> p fc d", p=128), writes=[k_w2[i]], slot=k_w2[i])
        gt = fw.sb([128, 8], F32); k_gt = fw.trk()
        load_cols(gt[:], k_gt, mlp_g[l, :], 8)
        TT = 256
        xts = [(fw.sb([128, 2, D], F32), [fw.trk(), fw.trk()]) for _ in range(2)]
        xos = [(fw.sb([128, 2, D], F32), [fw.trk(), fw.trk()]) for _ in range(2)]
        res = (fw.sb([128, D], BF16), fw.trk(), fw.sb([128, 2], F32), fw.trk(),
               fw.sb([128, D], BF16), fw.trk(), fw.ps([128, 8, 128], BF16), fw.trk())
        h2T = [(fw.sb([128, 8, TT], BF16), [fw.trk(), fw.trk()]) for _ in range(2)]
        py = [(fw.ps([128, D], F32), fw.trk()) for _ in range(2)]
        pf = [(fw.ps([128, 512], F32), fw.trk()) for _ in range(3)]
        rl = [(fw.sb([128, TT], F32), fw.trk()) for _ in range(3)]
        fb = [(fw.sb([128, TT], BF16), fw.trk()) for _ in range(3)]
        for ti in range(T // TT):
            xt, k_xt = xts[ti % 2]; xo, k_xo = xos[ti % 2]; hT2, k_h2 = h2T[ti % 2]
            for b2 in range(2):
                tb = ti * 2 + b2
                fw.dma("sp", xt[:, b2, :], xmid[tb * 128:(tb + 1) * 128, :], reads=[k_xmid], writes=[k_xt[b2]], slot=k_xt[b2])
                norm_transpose_block(xt[:, b2, :], k_xt[b2], gt, k_gt, res, hT2[:, :, b2 * 128:(b2 + 1) * 128], k_h2[b2])

            def stF(fc):
                p_, k_p = pf[fc % 3]
                for dc in range(8):
                    fw.op("pe", lambda e, dc=dc: e.matmul(p_[:, 0:TT], lhsT=W1[:, dc, fc * 128:(fc + 1) * 128], rhs=hT2[:, dc, :], start=(dc == 0), stop=(dc == 7)),
                          reads=[k_w1[fc // 4]] + k_h2, writes=[k_p])
                r_, k_r = rl[fc % 3]; f_, k_f = fb[fc % 3]
                fw.op("act", lambda e: e.activation(out=r_[:], in_=p_[:, 0:TT], func=AF.Relu), reads=[k_p], writes=[k_r])
                fw.op("pool", lambda e: e.tensor_tensor(out=f_[:], in0=r_[:], in1=r_[:], op=ALU.mult), reads=[k_r], writes=[k_f])

            def stY(fc):
                f_, k_f = fb[fc % 3]
                for b2 in range(2):
                    for half in range(2):
                        fw.op("pe", lambda e, b2=b2, half=half: e.matmul(py[b2][0][:, half * 512:(half + 1) * 512], lhsT=f_[:, b2 * 128:(b2 + 1) * 128], rhs=W2[:, fc, half * 512:(half + 1) * 512], start=(fc == 0), stop=(fc == 31)),
                              reads=[k_f, k_w2[fc // 4]], writes=[py[b2][1]])

            for i in range(32 + 2):
                if i < 32:
                    stF(i)
                if 0 <= i - 2 < 32:
                    stY(i - 2)
            for b2 in range(2):
                tb = ti * 2 + b2
                for half in range(2):
                    fw.op("dve", lambda e, b2=b2, half=half: e.tensor_tensor(out=xo[:, b2, half * 512:(half + 1) * 512], in0=py[b2][0][:, half * 512:(half + 1) * 512], in1=xt[:, b2, half * 512:(half + 1) * 512], op=ALU.add),
                          reads=[py[b2][1], k_xt[b2]], writes=[k_xo[b2]])
                fw.dma("sp", xout[tb * 128:(tb + 1) * 128, :], xo[:, b2, :], reads=[k_xo[b2]], writes=[k_xout], slot=k_xo[b2])
        fw.release(m)

    bufs = {0: (x_in, k_dram["x"], s1, k_dram["s1"], s0, k_dram["s0"]),
            1: (s0, k_dram["s0"], s1, k_dram["s1"], y_out, k_dram["y"])}
    if nlayers == 1:
        bufs[0] = (x_in, k_dram["x"], s1, k_dram["s1"], y_out, k_dram["y"])
    for l in range(nlayers):
        xin, k_xin, xmid, k_xmid, xout, k_xout = bufs[l]
        phase_A(l, xin, k_xin)
        if dbg == "hT":
            break
        for hp in range(2):
            if "sb" not in skip:
                phase_SB(l, hp)
        if dbg in ("sb", "sbproj"):
            break
        if "ret" not in skip:
            phase_RET(l)
        if dbg == "ret":
            break
        if "conv" not in skip:
            phase_CONV(l)
        if dbg == "mixed":
            break
        k_w1 = [fw.trk() for _ in range(8)]
        phase_C1(l, xin, k_xin, xmid, k_xmid, k_w1)
        if dbg == "c1":
            break
        phase_C2(l, xmid, k_xmid, xout, k_xout, k_w1)
    if dbg in ("hT", "sb", "ret", "mixed", "sbproj"):
        src = R1 if dbg == "hT" else R2
        m = fw.mark()
        st = [(fw.sb([128, 2048], F32), fw.trk()) for _ in range(2)]
        i = 0
        for c in dump_chunks:
            for hf in range(2):
                s_, k_s = st[i % 2]; i += 1
                fw.op("dve", lambda e: e.tensor_copy(out=s_[:], in_=src[:, c, hf * 2048:(hf + 1) * 2048]), reads=[], writes=[k_s])
                fw.dma("sp", dbg_out[:, c, hf * 2048:(hf + 1) * 2048], s_[:], reads=[k_s], writes=[k_dram["dbg"]], slot=k_s)
        fw.release(m)
    fw.barrier()
    return fw


def kernel(**inputs):
    global _CONSTS
    if _CONSTS is None:
        _CONSTS = _consts()
    if "nc" not in _NC_CACHE:
        p1 = build()
        _NC_CACHE["nc"] = build(needed=p1.waited)
    fw = _NC_CACHE["nc"]
    x = np.ascontiguousarray(np.asarray(inputs["x"], dtype=np.float32))
    shared = {k: np.ascontiguousarray(np.asarray(v, dtype=np.float32)) for k, v in inputs.items() if k != "x"}
    shared.update(_CONSTS)
    in_maps = []
    for b in range(8):
        mp = dict(shared)
        mp["x"] = x[b]
        in_maps.append(mp)
    res = run_bass_kernel_spmd(fw.nc, in_maps, core_ids=list(range(8)))
    return np.stack([np.asarray(r["y"], dtype=np.float32) for r in res.results], axis=0)
```

```python
import numpy as np
import concourse.bass as bass
import concourse.mybir as mybir
from concourse.bass_utils import run_bass_kernel_spmd

F32 = mybir.dt.float32
BF16 = mybir.dt.bfloat16
AF = mybir.ActivationFunctionType
ALU = mybir.AluOpType
AX = mybir.AxisListType

T = 4096
D = 1024
NTB = 32
DFF = 4096
DEPTH = 2
INC = 3328
EPS = 1e-6
NEG = -30000.0
RET_STOP = 99
CONV_STOP = 99


class Trk:
    __slots__ = ("name", "lw", "rd", "dsem")

    def __init__(self, name):
        self.name = name
        self.lw = {}
        self.rd = {}
        self.dsem = None


class Eng:
    def __init__(self, name, h, sem):
        self.name = name
        self.h = h
        self.sem = sem
        self.count = 0
        self.seen = {}


class FW:
    def __init__(self, needed=None):
        self.needed = None
        if needed is not None:
            self.needed = {k: sorted(v) for k, v in needed.items()}
            self.needed_set = {k: set(v) for k, v in needed.items()}
        self.waited = {}
        self.nc = bass.Bass("TRN2", target_bir_lowering=False)
        nc = self.nc
        self._stack = []
        self.engs = {}
        for name, h in (("pe", nc.tensor), ("act", nc.scalar), ("dve", nc.vector),
                        ("pool", nc.gpsimd), ("sp", nc.sync)):
            sem = self.enter(nc.semaphore("sem_" + name))
            self.engs[name] = Eng(name, h, sem)
        self.dsem_tot = {}
        self.free_dsems = []
        self.free_dsems_sw = []
        self._scope_dsems = []
        self._persist = []
        self.nwait = 0
        self.ninc = 0
        self.ninst = 0
        self._uid = 0

    def enter(self, cm):
        v = cm.__enter__()
        self._stack.append(cm)
        return v

    def uid(self, p="t"):
        self._uid += 1
        return "%s%d" % (p, self._uid)

    def sb(self, shape, dt, name=None):
        return self.enter(self.nc.sbuf_tensor(name or self.uid("sb"), list(shape), dt))

    def ps(self, shape, dt, name=None):
        return self.enter(self.nc.psum_tensor(name or self.uid("ps"), list(shape), dt))

    def trk(self, name=None):
        return Trk(name or self.uid("k"))

    def mark(self):
        self._scope_dsems.append([])
        return len(self._stack)

    def release(self, mark):
        self.barrier()
        while len(self._stack) > mark:
            cm = self._stack.pop()
            cm.__exit__(None, None, None)
        for is_sw, sem in self._scope_dsems.pop():
            (self.free_dsems_sw if is_sw else self.free_dsems).append(sem)

    def _emit_wait(self, eng, s, k, v):
        if k in self.dsem_tot:
            eng.h.wait_ge(s, v)
        else:
            self.waited.setdefault(k, set()).add(v)
            if self.needed is None:
                eng.h.wait_ge(s, v)
            else:
                import bisect
                lst = self.needed[k]
                r = bisect.bisect_left(lst, v)
                assert r < len(lst) and lst[r] == v, (k, v)
                eng.h.wait_ge(s, r + 1)
        self.nwait += 1

    def _deps(self, reads, writes):
        deps = {}

        def add(d):
            for k, (s, v) in d.items():
                if k not in deps or deps[k][1] < v:
                    deps[k] = (s, v)
        for r in reads:
            add(r.lw)
        for w in writes:
            add(w.lw)
            add(w.rd)
        return deps

    def _wait(self, eng, deps):
        for k, (s, v) in deps.items():
            if k in self.dsem_tot:
                v = self.dsem_tot[k][1]
            if eng.name == "pe" and k == eng.sem.name:
                continue
            if eng.seen.get(k, 0) >= v:
                continue
            self._emit_wait(eng, s, k, v)
            eng.seen[k] = v

    def op(self, ename, fn, reads=(), writes=()):
        eng = self.engs[ename]
        self._wait(eng, self._deps(reads, writes))
        inst = fn(eng.h)
        eng.count += 1
        if self.needed is None or eng.count in self.needed_set.get(eng.sem.name, ()):
            inst.then_inc(eng.sem, 1)
            self.ninc += 1
        ev = (eng.sem, eng.count)
        k = eng.sem.name
        for w in writes:
            w.lw = {k: ev}
            w.rd = {}
        for r in reads:
            r.rd[k] = ev
        self.ninst += 1
        return inst

    def dma(self, qname, out, in_, reads=(), writes=(), slot=None, **kw):
        eng = self.engs[qname]
        self._wait(eng, self._deps(reads, writes))
        if slot.dsem is None:
            pool_ = self.free_dsems_sw if qname == "pool" else self.free_dsems
            if pool_:
                slot.dsem = pool_.pop()
            else:
                cm = self.nc.semaphore(self.uid("dsem"))
                slot.dsem = cm.__enter__()
                self._persist.append(cm)
                self.dsem_tot[slot.dsem.name] = (slot.dsem, 0)
            if self._scope_dsems:
                self._scope_dsems[-1].append((qname == "pool", slot.dsem))
        inst = eng.h.dma_start(out=out, in_=in_, **kw)
        tot = self.dsem_tot[slot.dsem.name][1] + 16
        self.dsem_tot[slot.dsem.name] = (slot.dsem, tot)
        inst.then_inc(slot.dsem, 16)
        ev = (slot.dsem, tot)
        k = slot.dsem.name
        for w in writes:
            w.lw = {k: ev}
            w.rd = {}
        for r in reads:
            r.rd[k] = ev
        self.ninst += 1
        return inst

    def barrier(self):
        for eng in self.engs.values():
            for e in self.engs.values():
                if e is eng or e.count == 0:
                    continue
                if eng.seen.get(e.sem.name, 0) < e.count:
                    self._emit_wait(eng, e.sem, e.sem.name, e.count)
                    eng.seen[e.sem.name] = e.count
            for k, (s, tot) in self.dsem_tot.items():
                if tot and eng.seen.get(k, 0) < tot:
                    self._emit_wait(eng, s, k, tot)
                    eng.seen[k] = tot


def _consts():
    c = {}
    i = np.arange(128)
    c["c_ident"] = np.eye(128, dtype=np.float32)
    c["c_blk"] = (i[:, None] // 64 == i[None, :] // 64).astype(np.float32)
    c["c_negtri"] = -(i[:, None] >= i[None, :]).astype(np.float32)
    c["c_negones"] = -np.ones((128, 128), np.float32)
    c["c_maskneg"] = np.where(i[:, None] >= i[None, :], NEG, 0.0).astype(np.float32)
    c["c_ones"] = np.full((128, 128), 1.0 / 256.0, np.float32)
    inv = (1.0 / (10000.0 ** np.linspace(0.0, 1.0, 32, dtype=np.float32))).astype(np.float32)
    pos = np.arange(T, dtype=np.float32)
    ang = (pos[:, None] * inv[None, :]).astype(np.float32)
    cos = np.cos(ang).astype(np.float32)
    sin = np.sin(ang).astype(np.float32)
    cos2 = np.repeat(cos, 2, axis=1)
    s2 = np.stack([-sin, sin], axis=-1).reshape(T, 64)
    rot = np.concatenate([cos2, s2, cos2 * 0.125, s2 * 0.125], axis=1).astype(np.float32)
    c["c_rot"] = rot.reshape(NTB, 128, 256)
    h = np.arange(8, dtype=np.float32)
    log_g = np.log(1.0 - np.exp2(-5.0 - h)).astype(np.float32)
    j = np.arange(128, dtype=np.float32)
    rel = j[None, :] - j[:, None]
    dt_ = np.where(rel[None] >= 0, np.exp(log_g[:, None, None] * np.maximum(rel[None], 0.0)), 0.0)
    c["c_dt"] = np.ascontiguousarray(dt_.transpose(1, 0, 2)).astype(np.float32)
    qd = np.exp(log_g[:, None] * (j + 1.0)[None, :]).astype(np.float32)
    qdec = np.zeros((128, 4, 128), np.float32)
    g128 = np.zeros((128, 4, 64), np.float32)
    gC = np.exp(log_g * 128.0).astype(np.float32)
    for p in range(128):
        for pr in range(4):
            hh = pr * 2 + p // 64
            qdec[p, pr, :] = qd[hh]
            g128[p, pr, :] = gC[hh]
    c["c_qdec"] = qdec
    c["c_g128"] = g128
    kd = np.exp(log_g[:, None] * (127.0 - j)[None, :]).astype(np.float32)
    c["c_kdec"] = np.ascontiguousarray(np.repeat(kd.T[:, :, None], 64, axis=2)).astype(np.float32)
    return c


_CONSTS = None
_NC_CACHE = {}


def build(nlayers=DEPTH, dbg=None, skip=(), dump_chunks=range(8), needed=None):
    fw = FW(needed)
    nc = fw.nc

    def din(name, shape):
        shape = list(shape)
        if shape[0] == DEPTH and not name.startswith("c_"):
            shape[0] = nlayers
        return nc.dram_tensor(name, shape, F32, kind="ExternalInput").ap()

    x_in = din("x", [T, D])
    mix_g = din("mix_norm_g", [DEPTH, D])
    w_in = din("w_in", [DEPTH, D, INC])
    sbq_g = din("sb_q_norm_g", [DEPTH, 64])
    sbk_g = din("sb_k_norm_g", [DEPTH, 64])
    ret_g = din("ret_norm_g", [DEPTH, 512])
    cpw_b = din("conv_pw_b", [DEPTH, 512])
    cdw_w = din("conv_dw_w", [DEPTH, 31, 256])
    cdw_b = din("conv_dw_b", [DEPTH, 256])
    cln_g = din("conv_ln_g", [DEPTH, 256])
    cln_b = din("conv_ln_b", [DEPTH, 256])
    w_out = din("w_out", [DEPTH, D, D])
    mlp_g = din("mlp_norm_g", [DEPTH, D])
    w_ff1 = din("w_ff1", [DEPTH, D, DFF])
    w_ff2 = din("w_ff2", [DEPTH, DFF, D])
    c_ident = din("c_ident", [128, 128])
    c_blk = din("c_blk", [128, 128])
    c_negtri = din("c_negtri", [128, 128])
    c_negones = din("c_negones", [128, 128])
    c_maskneg = din("c_maskneg", [128, 128])
    c_ones = din("c_ones", [128, 128])
    c_rot = din("c_rot", [NTB, 128, 256])
    c_dt = din("c_dt", [128, 8, 128])
    c_qdec = din("c_qdec", [128, 4, 128])
    c_g128 = din("c_g128", [128, 4, 64])
    c_kdec = din("c_kdec", [128, 8, 64])
    y_out = nc.dram_tensor("y", [T, D], F32, kind="ExternalOutput").ap()
    s0 = nc.dram_tensor("scr0", [T, D], F32, kind="Internal").ap()
    s1 = nc.dram_tensor("scr1", [T, D], F32, kind="Internal").ap()
    dbg_out = None
    if dbg is not None:
        dbg_out = nc.dram_tensor("dbg", [128, 8, T], F32, kind="ExternalOutput").ap()

    k_dram = {"x": fw.trk("dx"), "s0": fw.trk("ds0"), "s1": fw.trk("ds1"), "y": fw.trk("dy"), "dbg": fw.trk("ddbg")}

    R1 = fw.sb([128, 8, T], BF16, "R1")
    R2 = fw.sb([128, 8, T], BF16, "R2")
    k_hT = [fw.trk("hT%d" % i) for i in range(NTB)]
    k_mx = [[fw.trk("mx%d_%d" % (c, i)) for i in range(8)] for c in range(8)]
    identb = fw.sb([128, 128], BF16, "identb"); k_identb = fw.trk("identb")
    identf = fw.sb([128, 128], F32, "identf"); k_identf = fw.trk("identf")
    blkb = fw.sb([128, 128], BF16, "blkb"); k_blkb = fw.trk("blkb")
    negtri = fw.sb([128, 128], BF16, "negtri"); k_negtri = fw.trk("negtri")
    negones = fw.sb([128, 128], BF16, "negones"); k_negones = fw.trk("negones")
    maskneg = fw.sb([128, 128], BF16, "maskneg"); k_maskneg = fw.trk("maskneg")
    onesf = fw.sb([128, 128], BF16, "onesf"); k_onesf = fw.trk("onesf")
    zerob = fw.sb([128, 128], BF16, "zerob"); k_zerob = fw.trk("zerob")
    fw.op("pool", lambda e: e.memset(zerob[:], 0.0), writes=[k_zerob])
    fw.dma("pool", identb[:], c_ident[:, :], writes=[k_identb], slot=k_identb)
    fw.dma("sp", identf[:], c_ident[:, :], writes=[k_identf], slot=k_identf)
    fw.dma("pool", blkb[:], c_blk[:, :], writes=[k_blkb], slot=k_blkb)
    fw.dma("pool", negtri[:], c_negtri[:, :], writes=[k_negtri], slot=k_negtri)
    fw.dma("pool", negones[:], c_negones[:, :], writes=[k_negones], slot=k_negones)
    fw.dma("pool", maskneg[:], c_maskneg[:, :], writes=[k_maskneg], slot=k_maskneg)
    fw.dma("pool", onesf[:], c_ones[:, :], writes=[k_onesf], slot=k_onesf)

    def load_cols(dst, k_dst, src_row_ap, n):
        with nc.allow_non_contiguous_dma(reason="tiny param vector"):
            fw.dma("sp", dst, src_row_ap.rearrange("(c p) -> p c", p=128), writes=[k_dst], slot=k_dst)

    def rstd_from(ename_unused, out_ap, in_ap, scale, reads, writes):
        fw.op("act", lambda e: e.activation(out=out_ap, in_=in_ap, func=AF.Ln, scale=scale, bias=EPS),
              reads=reads, writes=writes)
        fw.op("act", lambda e: e.activation(out=out_ap, in_=out_ap, func=AF.Exp, scale=-0.5),
              reads=writes, writes=writes)

    def sigmoid_from(out_ap, in_ap, reads, writes, nbias=None):
        if nbias is None:
            fw.op("act", lambda e: e.activation(out=out_ap, in_=in_ap, func=AF.Exp, scale=-1.0), reads=reads, writes=writes)
        else:
            fw.op("act", lambda e: e.activation(out=out_ap, in_=in_ap, func=AF.Exp, scale=-1.0, bias=nbias), reads=reads, writes=writes)
        fw.op("act", lambda e: e.activation(out=out_ap, in_=out_ap, func=AF.Ln, bias=1.0), reads=writes, writes=writes)
        fw.op("act", lambda e: e.activation(out=out_ap, in_=out_ap, func=AF.Exp, scale=-1.0), reads=writes, writes=writes)

    def norm_transpose_block(xt, k_xt, gt, k_gt, res, dst_ap, k_dst):
        junk, k_junk, st, k_st, hb, k_hb, pt, k_pt = res
        fw.op("act", lambda e: e.activation(out=junk[:], in_=xt, func=AF.Square, accum_out=st[:, 0:1]),
              reads=[k_xt], writes=[k_junk, k_st])
        rstd_from("act", st[:, 1:2], st[:, 0:1], 1.0 / D, [k_st], [k_st])
        fw.op("dve", lambda e: e.tensor_scalar(out=hb[:], in0=xt, scalar1=st[:, 1:2], scalar2=None, op0=ALU.mult),
              reads=[k_xt, k_st], writes=[k_hb])
        for dc in range(8):
            fw.op("pe", lambda e, dc=dc: e.transpose(out=pt[:, dc, :], in_=hb[:, dc * 128:(dc + 1) * 128], identity=identb[:]),
                  reads=[k_hb, k_identb], writes=[k_pt])
        fw.op("dve", lambda e: e.tensor_tensor(out=dst_ap, in0=pt[:], in1=gt[:].unsqueeze(2).broadcast_to([128, 8, 128]), op=ALU.mult),
              reads=[k_pt, k_gt], writes=[k_dst])

    def phase_A(l, xin, k_xin):
        m = fw.mark()
        gt = fw.sb([128, 8], F32); k_gt = fw.trk()
        load_cols(gt[:], k_gt, mix_g[l, :], 8)
        nb = 3
        xts = [(fw.sb([128, D], F32), fw.trk()) for _ in range(nb)]
        ress = []
        for _ in range(2):
            ress.append((fw.sb([128, D], BF16), fw.trk(), fw.sb([128, 2], F32), fw.trk(),
                         fw.sb([128, D], BF16), fw.trk(), fw.ps([128, 8, 128], BF16), fw.trk()))
        for tb in range(NTB):
            xt, k_xt = xts[tb % nb]
            fw.dma("sp", xt[:], xin[tb * 128:(tb + 1) * 128, :], reads=[k_xin], writes=[k_xt], slot=k_xt)
            norm_transpose_block(xt[:], k_xt, gt, k_gt, ress[tb % 2], R1[:, :, tb * 128:(tb + 1) * 128], k_hT[tb])
        fw.release(m)

    def phase_SB(l, hp):
        m = fw.mark()
        qT = fw.sb([128, T], BF16); kT = fw.sb([128, 2, T], BF16); vv = fw.sb([128, NTB, 128], BF16)
        k_kT0 = fw.trk()
        fw.op("pool", lambda e: e.memset(kT[:], 0.0), writes=[k_kT0])
        k_qT = [fw.trk() for _ in range(8)]; k_kT = [fw.trk() for _ in range(8)]; k_v = [fw.trk() for _ in range(8)]
        gq = fw.sb([128, 1], F32); k_gq = fw.trk(); gk = fw.sb([128, 1], F32); k_gk = fw.trk()
        with nc.allow_non_contiguous_dma(reason="tiny param vector"):
            for half in range(2):
                fw.dma("sp", gq[half * 64:(half + 1) * 64, :], sbq_g[l, :].rearrange("(p o) -> p o", o=1), writes=[k_gq], slot=k_gq)
                fw.dma("sp", gk[half * 64:(half + 1) * 64, :], sbk_g[l, :].rearrange("(p o) -> p o", o=1), writes=[k_gk], slot=k_gk)
        fw.op("dve", lambda e: e.tensor_scalar(out=gq[:], in0=gq[:], scalar1=0.125, scalar2=None, op0=ALU.mult), reads=[k_gq], writes=[k_gq])
        wts = [(fw.sb([128, 8, 128], BF16), fw.trk()) for _ in range(2)]
        pbank = [(fw.ps([128, 512], F32), fw.trk()) for _ in range(2)]
        pss = [(fw.ps([128, 512], F32), fw.trk()) for _ in range(2)]
        sqb = [(fw.sb([128, 512], BF16), fw.trk()) for _ in range(2)]
        rsd = [(fw.sb([128, 512], F32), fw.trk()) for _ in range(2)]
        it = 0
        for which in range(2):
            col0 = (0 if which == 0 else 256) + hp * 128
            wt, k_wt = wts[which]
            fw.dma("pool", wt[:], w_in[l, :, col0:col0 + 128].rearrange("(dc p) c -> p dc c", p=128), writes=[k_wt], slot=k_wt)
            dst = qT if which == 0 else kT
            kd = k_qT if which == 0 else k_kT
            gcol, k_gcol = (gq, k_gq) if which == 0 else (gk, k_gk)
            for tt in range(8):
                pq, k_pq = pbank[it % 2]; ps2, k_ps2 = pss[it % 2]; sq, k_sq = sqb[it % 2]; rs, k_rs = rsd[it % 2]
                it += 1
                for dc in range(8):
                    fw.op("pe", lambda e, dc=dc: e.matmul(pq[:], lhsT=wt[:, dc, :], rhs=R1[:, dc, tt * 512:(tt + 1) * 512], start=(dc == 0), stop=(dc == 7)),
                          reads=[k_wt] + k_hT[tt * 4:(tt + 1) * 4], writes=[k_pq])
                fw.op("act", lambda e: e.activation(out=sq[:], in_=pq[:], func=AF.Square), reads=[k_pq], writes=[k_sq])
                fw.op("pe", lambda e: e.matmul(ps2[:], lhsT=blkb[:], rhs=sq[:], start=True, stop=True), reads=[k_blkb, k_sq], writes=[k_ps2])
                rstd_from("act", rs[:], ps2[:], 1.0 / 64, [k_ps2], [k_rs])
                if which == 0:
                    fw.op("dve", lambda e: e.scalar_tensor_tensor(out=dst[:, tt * 512:(tt + 1) * 512], in0=pq[:], scalar=gcol[:, 0:1], in1=rs[:], op0=ALU.mult, op1=ALU.mult),
                          reads=[k_pq, k_gcol, k_rs], writes=[kd[tt]])
                else:
                    for hh_ in range(2):
                        fw.op("dve", lambda e, hh_=hh_: e.scalar_tensor_tensor(out=kT[hh_ * 64:(hh_ + 1) * 64, hh_, tt * 512:(tt + 1) * 512], in0=pq[hh_ * 64:(hh_ + 1) * 64, :],
                                                                              scalar=gcol[hh_ * 64:(hh_ + 1) * 64, 0:1], in1=rs[hh_ * 64:(hh_ + 1) * 64, :], op0=ALU.mult, op1=ALU.mult),
                              reads=[k_pq, k_gcol, k_rs, k_kT0], writes=[kd[tt]])
        wt, k_wt = wts[0]
        col0 = 512 + hp * 128
        fw.dma("pool", wt[:], w_in[l, :, col0:col0 + 128].rearrange("(dc p) c -> p dc c", p=128), writes=[k_wt], slot=k_wt)
        for tb in range(NTB):
            pq, k_pq = pbank[tb % 2]
            for dc in range(8):
                fw.op("pe", lambda e, dc=dc: e.matmul(pq[:, 0:128], lhsT=R1[:, dc, tb * 128:(tb + 1) * 128], rhs=wt[:, dc, :], start=(dc == 0), stop=(dc == 7)),
                      reads=[k_wt, k_hT[tb]], writes=[k_pq])
            fw.op("act", lambda e: e.activation(out=vv[:, tb, :], in_=pq[:, 0:128], func=AF.Copy), reads=[k_pq], writes=[k_v[tb // 4]])

        if dbg == 'sbproj':
            fw.release(m)
            return
        pz = [(fw.ps([128, 512], F32), fw.trk()) for _ in range(2)] + [pss[0]]
        plw = [(fw.ps([128, 512], F32), fw.trk()) for _ in range(2)] + [pss[1]]
        Eb = [(fw.sb([128, 512], F32), fw.trk()) for _ in range(3)]
        Lb = [(fw.sb([128, 512], BF16), fw.trk()) for _ in range(3)]
        Rb = fw.sb([128, 3, 512], BF16); k_R = [fw.trk() for _ in range(3)]
        wb = [(fw.sb([128, 512], BF16), fw.trk()) for _ in range(2)]
        Xb = [(fw.sb([128, 512], F32), fw.trk()) for _ in range(2)]
        for hh in range(2):
            p0 = hh * 64
            for qt in range(8):
                po, k_po = pbank[(hh * 8 + qt) % 2]
                fw.op("pool", lambda e: e.memset(Rb[:], 0.0), writes=k_R)
                fw.op("pe", lambda e: e.matmul(po[:, :], lhsT=zerob[:], rhs=qT[:, qt * 512:(qt + 1) * 512], start=True, stop=False),
                      reads=[k_zerob, k_qT[qt]], writes=[k_po])
                blocks = list(range(4 * qt + 3, -1, -1))
                nblk = len(blocks)
                q0 = qt * 512

                def zmm(dstp, b, last_stop):
                    kb = blocks[b]
                    kl = kb - 4 * qt
                    ksl = kT[:, hh, kb * 128:(kb + 1) * 128]
                    rk = [k_kT[kb // 4], k_qT[qt]]
                    c0 = kl * 128 if kl >= 0 else 0
                    fw.op("pe", lambda e: e.matmul(dstp[0][:, c0:512], lhsT=ksl, rhs=qT[:, q0 + c0:q0 + 512], start=True, stop=(last_stop and kl < 0)),
                          reads=rk, writes=[dstp[1]])
                    if kl >= 0:
                        fw.op("pe", lambda e: e.matmul(dstp[0][:, c0:c0 + 128], lhsT=identb[:], rhs=maskneg[:], start=False, stop=last_stop),
                              reads=[k_identb, k_maskneg], writes=[dstp[1]])
                    return c0

                def stageA1(b):
                    c0 = zmm(pz[b % 3], b, True)
                    E, k_E = Eb[b % 3]
                    fw.op("act", lambda e: e.activation(out=E[:, c0:], in_=pz[b % 3][0][:, c0:], func=AF.Exp), reads=[pz[b % 3][1]], writes=[k_E])

                def stageA2(b):
                    kl = blocks[b] - 4 * qt
                    c0 = kl * 128 if kl >= 0 else 0
                    E, k_E = Eb[b % 3]; L, k_L = Lb[b % 3]
                    fw.op("act", lambda e: e.activation(out=L[:, c0:], in_=E[:, c0:], func=AF.Ln, bias=1.0), reads=[k_E], writes=[k_L])
                    if b + 1 < nblk:
                        fw.op("pool", lambda e: e.tensor_tensor(out=Rb[:, (b + 1) % 3, c0:], in0=Rb[:, b % 3, c0:], in1=L[:, c0:], op=ALU.add),
                              reads=[k_R[b % 3], k_L], writes=[k_R[(b + 1) % 3]])

                def stageB1(b):
                    kl = blocks[b] - 4 * qt
                    c0 = kl * 128 if kl >= 0 else 0
                    L, k_L = Lb[b % 3]
                    fw.op("pe", lambda e: e.matmul(plw[b % 3][0][:, c0:], lhsT=negtri[:], rhs=L[:, c0:], start=True, stop=(b == 0)),
                          reads=[k_negtri, k_L], writes=[plw[b % 3][1]])
                    if b > 0:
                        fw.op("pe", lambda e: e.matmul(plw[b % 3][0][:, c0:], lhsT=negones[:], rhs=Rb[:, b % 3, c0:], start=False, stop=True),
                              reads=[k_negones, k_R[b % 3]], writes=[plw[b % 3][1]])
                    X, k_X = Xb[b % 2]
                    fw.op("act", lambda e: e.activation(out=X[:, c0:], in_=plw[b % 3][0][:, c0:], func=AF.Exp), reads=[plw[b % 3][1]], writes=[k_X])

                def stageB2(b):
                    kl = blocks[b] - 4 * qt
                    c0 = kl * 128 if kl >= 0 else 0
                    E, k_E = Eb[b % 3]; X, k_X = Xb[b % 2]; w, k_w = wb[b % 2]
                    fw.op("dve", lambda e: e.tensor_tensor(out=w[:, c0:], in0=E[:, c0:], in1=X[:, c0:], op=ALU.mult), reads=[k_E, k_X], writes=[k_w])

                def stageC(b):
                    kb = blocks[b]
                    kl = kb - 4 * qt
                    w, k_w = wb[b % 2]
                    last = (b == nblk - 1)
                    vsl = vv[:, kb, :]
                    rd = [k_v[kb // 4], k_w]
                    c0 = kl * 128 if kl >= 0 else 0
                    fw.op("pe", lambda e: e.matmul(po[:, c0:512], lhsT=vsl, rhs=w[:, c0:512], start=False, stop=last), reads=rd, writes=[k_po])
                    if last:
                        fw.op("dve", lambda e: e.tensor_copy(out=R2[p0:p0 + 64, hp, q0:q0 + 512], in_=po[p0:p0 + 64, :]), reads=[k_po], writes=[k_mx[hp][qt]])

                for i in range(nblk + 3):
                    if i < nblk:
                        stageA1(i)
                    if 0 <= i - 2 < nblk:
                        stageB1(i - 2)
                    if i < nblk:
                        stageA2(i)
                    if 0 <= i - 2 < nblk:
                        stageB2(i - 2)
                    if 0 <= i - 3 < nblk:
                        stageC(i - 3)
        fw.release(m)

    def phase_RET(l):
        m = fw.mark()
        wr = fw.sb([128, 8, 2048], BF16); k_wr = [fw.trk() for _ in range(4)]
        for i in range(4):
            fw.dma("pool", wr[:, :, i * 512:(i + 1) * 512], w_in[l, :, 768 + i * 512:768 + (i + 1) * 512].rearrange("(dc p) c -> p dc c", p=128),
                   writes=[k_wr[i]], slot=k_wr[i])
        dtab = fw.sb([128, 8, 128], F32); k_dtab = fw.trk()
        qdec = fw.sb([128, 4, 128], F32); k_qdec = fw.trk()
        kdec = fw.sb([128, 8, 64], F32); k_kdec = fw.trk()
        g128 = fw.sb([128, 4, 64], F32); k_g128 = fw.trk()
        rgb = fw.sb([128, 512], F32); k_rgb = fw.trk()
        fw.dma("sp", dtab[:], c_dt[:, :, :], writes=[k_dtab], slot=k_dtab)
        fw.dma("sp", qdec[:], c_qdec[:, :, :], writes=[k_qdec], slot=k_qdec)
        fw.dma("sp", kdec[:], c_kdec[:, :, :], writes=[k_kdec], slot=k_kdec)
        fw.dma("sp", g128[:], c_g128[:, :, :], writes=[k_g128], slot=k_g128)
        fw.dma("sp", rgb[:], ret_g[l, :].partition_broadcast(128), writes=[k_rgb], slot=k_rgb)
        rot = [(fw.sb([128, 256], F32), fw.trk()) for _ in range(2)]
        pA = (fw.ps([128, 512], F32), fw.trk()); pB = (fw.ps([128, 512], F32), fw.trk())
        ptq = (fw.ps([128, 4, 128], BF16), fw.trk()); ptk = (fw.ps([128, 4, 128], BF16), fw.trk())
        pS = [(fw.ps([128, 4, 128], F32), fw.trk()) for _ in range(2)]
        pO = (fw.ps([128, 512], F32), fw.trk()); pKV = (fw.ps([128, 4, 128], F32), fw.trk())
        tA = [(fw.sb([128, 512], F32), fw.trk()) for _ in range(2)]
        tB = [(fw.sb([128, 512], F32), fw.trk()) for _ in range(2)]
        rq = (fw.sb([128, 512], BF16), fw.trk()); rk = (fw.sb([128, 512], BF16), fw.trk())
        rkd = (fw.sb([128, 512], BF16), fw.trk()); vsb = (fw.sb([128, 512], BF16), fw.trk())
        gsil = (fw.sb([128, 512], F32), fw.trk())
        qTs = (fw.sb([128, 4, 128], BF16), fw.trk()); qdT = (fw.sb([128, 4, 128], BF16), fw.trk())
        kTz = (fw.sb([128, 4, 2, 128], BF16), fw.trk())
        fw.op("pool", lambda e: e.memset(kTz[0][:], 0.0), writes=[kTz[1]])
        AT = (fw.sb([128, 8, 128], BF16), fw.trk())
        sqo = (fw.sb([128, 512], F32), fw.trk()); ssr = (fw.sb([128, 8], F32), fw.trk())
        t1 = (fw.sb([128, 512], F32), fw.trk()); ytm = (fw.sb([128, 512], BF16), fw.trk())
        stf = (fw.sb([128, 4, 64], F32), fw.trk()); stb = (fw.sb([128, 4, 2, 64], BF16), fw.trk()); stt = (fw.sb([128, 4, 64], F32), fw.trk())
        fw.op("pool", lambda e: e.memset(stf[0][:], 0.0), writes=[stf[1]])
        fw.op("pool", lambda e: e.memset(stb[0][:], 0.0), writes=[stb[1]])

        def proj(pdst, wi, tb):
            for dc in range(8):
                fw.op("pe", lambda e, dc=dc: e.matmul(pdst[0][:], lhsT=R1[:, dc, tb * 128:(tb + 1) * 128], rhs=wr[:, dc, wi * 512:(wi + 1) * 512], start=(dc == 0), stop=(dc == 7)),
                      reads=[k_hT[tb], k_wr[wi]], writes=[pdst[1]])

        def v3(ap):
            return ap.rearrange("p (h e) -> p h e", h=8)

        def rotary(psrc, off, ta, tb_, dst, rt, k_rt):
            cos2 = rt[:, off:off + 64].unsqueeze(1).broadcast_to([128, 8, 64])
            s2 = rt[:, off + 64:off + 128]
            s2e = s2.rearrange("p (i two) -> p i two", two=2)[:, :, 0].unsqueeze(1).broadcast_to([128, 8, 32])
            s2o = s2.rearrange("p (i two) -> p i two", two=2)[:, :, 1].unsqueeze(1).broadcast_to([128, 8, 32])
            ps4 = psrc[0][:].rearrange("p (h i two) -> p h i two", h=8, two=2)
            tb4 = tb_[0][:].rearrange("p (h i two) -> p h i two", h=8, two=2)
            fw.op("dve", lambda e: e.tensor_tensor(out=v3(ta[0][:]), in0=v3(psrc[0][:]), in1=cos2, op=ALU.mult), reads=[psrc[1], k_rt], writes=[ta[1]])
            fw.op("dve", lambda e: e.tensor_tensor(out=tb4[:, :, :, 0], in0=ps4[:, :, :, 1], in1=s2e, op=ALU.mult), reads=[psrc[1], k_rt], writes=[tb_[1]])
            fw.op("dve", lambda e: e.tensor_tensor(out=tb4[:, :, :, 1], in0=ps4[:, :, :, 0], in1=s2o, op=ALU.mult), reads=[psrc[1], k_rt], writes=[tb_[1]])
            fw.op("pool", lambda e: e.tensor_tensor(out=dst[0][:], in0=ta[0][:], in1=tb_[0][:], op=ALU.add), reads=[ta[1], tb_[1]], writes=[dst[1]])

        gsil2 = [gsil, (fw.sb([128, 512], F32), fw.trk())]

        def r_head(n):
            rt, k_rt = rot[n % 2]
            fw.dma("sp", rt[:], c_rot[n, :, :], writes=[k_rt], slot=k_rt)
            proj(pA, 0, n)
            rotary(pA, 0, tA[0], tB[0], rq, rt, k_rt)
            proj(pB, 1, n)
            rotary(pB, 128, tA[1], tB[1], rk, rt, k_rt)
            fw.op("pool", lambda e: e.tensor_tensor(out=v3(rkd[0][:]), in0=v3(rk[0][:]), in1=kdec[:], op=ALU.mult), reads=[rk[1], k_kdec], writes=[rkd[1]])
            proj(pA, 2, n)
            fw.op("act", lambda e: e.activation(out=vsb[0][:], in_=pA[0][:], func=AF.Copy), reads=[pA[1]], writes=[vsb[1]])
            proj(pB, 3, n)
            sigmoid_from(gsil2[n % 2][0][:], pB[0][:], [pB[1]], [gsil2[n % 2][1]])
            fw.op("dve", lambda e: e.tensor_tensor(out=gsil2[n % 2][0][:], in0=pB[0][:], in1=gsil2[n % 2][0][:], op=ALU.mult), reads=[pB[1], gsil2[n % 2][1]], writes=[gsil2[n % 2][1]])
            fw.op("pool", lambda e: e.tensor_tensor(out=gsil2[n % 2][0][:], in0=gsil2[n % 2][0][:], in1=rgb[:], op=ALU.mult), reads=[gsil2[n % 2][1], k_rgb], writes=[gsil2[n % 2][1]])
            for pr in range(4):
                fw.op("pe", lambda e, pr=pr: e.transpose(out=ptq[0][:, pr, :], in_=rq[0][:, pr * 128:(pr + 1) * 128], identity=identb[:]), reads=[rq[1], k_identb], writes=[ptq[1]])
            for pr in range(4):
                fw.op("pe", lambda e, pr=pr: e.transpose(out=ptk[0][:, pr, :], in_=rk[0][:, pr * 128:(pr + 1) * 128], identity=identb[:]), reads=[rk[1], k_identb], writes=[ptk[1]])
            fw.op("act", lambda e: e.activation(out=qTs[0][:], in_=ptq[0][:], func=AF.Copy), reads=[ptq[1]], writes=[qTs[1]])
            fw.op("dve", lambda e: e.tensor_tensor(out=qdT[0][:], in0=ptq[0][:], in1=qdec[:], op=ALU.mult), reads=[ptq[1], k_qdec], writes=[qdT[1]])
            for hh in range(2):
                fw.op("act", lambda e, hh=hh: e.activation(out=kTz[0][hh * 64:(hh + 1) * 64, :, hh, :], in_=ptk[0][hh * 64:(hh + 1) * 64, :, :], func=AF.Copy), reads=[ptk[1]], writes=[kTz[1]])

        def r_mid(n):
            for h in range(8):
                pr, p0 = h // 2, (h % 2) * 64
                fw.op("pe", lambda e, h=h, pr=pr: e.matmul(pS[h // 4][0][:, h % 4, :], lhsT=kTz[0][:, pr, h % 2, :], rhs=qTs[0][:, pr, :], start=True, stop=True),
                      reads=[kTz[1], qTs[1]], writes=[pS[h // 4][1]])
            for half in range(2):
                fw.op("dve", lambda e, half=half: e.tensor_tensor(out=AT[0][:, half * 4:(half + 1) * 4, :], in0=pS[half][0][:], in1=dtab[:, half * 4:(half + 1) * 4, :], op=ALU.mult),
                      reads=[pS[half][1], k_dtab], writes=[AT[1]])
            for h in range(8):
                pr, p0 = h // 2, (h % 2) * 64
                fw.op("pe", lambda e, h=h: e.matmul(pO[0][:, h * 64:(h + 1) * 64], lhsT=AT[0][:, h, :], rhs=vsb[0][:, h * 64:(h + 1) * 64], start=True, stop=False),
                      reads=[AT[1], vsb[1]], writes=[pO[1]])
                fw.op("pe", lambda e, h=h, pr=pr: e.matmul(pO[0][:, h * 64:(h + 1) * 64], lhsT=qdT[0][:, pr, :], rhs=stb[0][:, pr, h % 2, :], start=False, stop=True),
                      reads=[qdT[1], stb[1]], writes=[pO[1]])
            for pr in range(4):
                fw.op("pe", lambda e, pr=pr: e.matmul(pKV[0][:, pr, :], lhsT=rkd[0][:, pr * 128:(pr + 1) * 128], rhs=vsb[0][:, pr * 128:(pr + 1) * 128], start=True, stop=True),
                      reads=[rkd[1], vsb[1]], writes=[pKV[1]])
            fw.op("pool", lambda e: e.tensor_tensor(out=stt[0][:], in0=stf[0][:], in1=g128[:], op=ALU.mult), reads=[stf[1], k_g128], writes=[stt[1]])
            for half in range(2):
                p0 = half * 64
                fw.op("dve", lambda e, p0=p0: e.tensor_tensor(out=stf[0][p0:p0 + 64, :, :], in0=stt[0][p0:p0 + 64, :, :], in1=pKV[0][p0:p0 + 64, :, p0:p0 + 64], op=ALU.add),
                      reads=[stt[1], pKV[1]], writes=[stf[1]])
            for hh in range(2):
                fw.op("act", lambda e, hh=hh: e.activation(out=stb[0][hh * 64:(hh + 1) * 64, :, hh, :], in_=stf[0][hh * 64:(hh + 1) * 64, :, :], func=AF.Copy), reads=[stf[1]], writes=[stb[1]])

        def r_tail(n):
            fw.op("act", lambda e: e.activation(out=sqo[0][:], in_=pO[0][:], func=AF.Square), reads=[pO[1]], writes=[sqo[1]])
            fw.op("dve", lambda e: e.tensor_reduce(out=ssr[0][:], in_=v3(sqo[0][:]), axis=AX.X, op=ALU.add), reads=[sqo[1]], writes=[ssr[1]])
            rstd_from("act", ssr[0][:], ssr[0][:], 1.0 / 64, [ssr[1]], [ssr[1]])
            fw.op("dve", lambda e: e.tensor_tensor(out=v3(t1[0][:]), in0=v3(pO[0][:]), in1=ssr[0][:].unsqueeze(2).broadcast_to([128, 8, 64]), op=ALU.mult),
                  reads=[pO[1], ssr[1]], writes=[t1[1]])
            fw.op("pool", lambda e: e.tensor_tensor(out=ytm[0][:], in0=t1[0][:], in1=gsil2[n % 2][0][:], op=ALU.mult), reads=[t1[1], gsil2[n % 2][1]], writes=[ytm[1]])
            for pr in range(4):
                fw.op("pe", lambda e, pr=pr: e.transpose(out=ptq[0][:, pr, :], in_=ytm[0][:, pr * 128:(pr + 1) * 128], identity=identb[:]), reads=[ytm[1], k_identb], writes=[ptq[1]])
            fw.op("act", lambda e: e.activation(out=R2[:, 2:6, n * 128:(n + 1) * 128], in_=ptq[0][:], func=AF.Copy), reads=[ptq[1]],
                  writes=[k_mx[2 + pr][n // 4] for pr in range(4)])

        r_head(0)
        r_mid(0)
        for n in range(NTB):
            if n + 1 < NTB:
                r_head(n + 1)
            r_tail(n)
            if n + 1 < NTB:
                r_mid(n + 1)
        fw.release(m)

    def phase_CONV(l):
        m = fw.mark()
        PAD = 30
        hg = fw.sb([128, 2, PAD + T], BF16); k_hg = [[fw.trk() for _ in range(9)] for _ in range(2)]
        dw31 = fw.sb([128, 256], F32); k_dw31 = fw.trk()
        fw.op("pool", lambda e: e.memset(dw31[:], 0.0), writes=[k_dw31])
        fw.dma("sp", dw31[0:31, :], cdw_w[l, :, :], writes=[k_dw31], slot=k_dw31)
        dwb16 = fw.sb([128, 256], BF16); k_dwb16 = fw.trk()
        fw.op("dve", lambda e: e.tensor_copy(out=dwb16[:], in_=dw31[:]), reads=[k_dw31], writes=[k_dwb16])
        wcol = fw.sb([128, 2, 31], F32); k_wcol = fw.trk()
        diagw = fw.sb([128, 2, 31, 128], BF16); k_diagw = fw.trk()
        cols = {}
        for name, src in (("ba", cpw_b[l, 0:256]), ("bg", cpw_b[l, 256:512]), ("dwb", cdw_b[l, :]), ("lng", cln_g[l, :]), ("lnb", cln_b[l, :])):
            t = fw.sb([128, 2], F32); k = fw.trk()
            load_cols(t[:], k, src, 2)
            cols[name] = (t, k)
        fw.op("dve", lambda e: e.tensor_scalar(out=cols["bg"][0][:], in0=cols["bg"][0][:], scalar1=-1.0, scalar2=None, op0=ALU.mult), reads=[cols["bg"][1]], writes=[cols["bg"][1]])
        pw = [(fw.ps([128, 512], F32), fw.trk()) for _ in range(2)]
        ptw = (fw.ps([128, 2, 128], BF16), fw.trk())
        for cc in range(2):
            fw.op("pe", lambda e, cc=cc: e.transpose(out=ptw[0][:, cc, :], in_=dwb16[:, cc * 128:(cc + 1) * 128], identity=identb[:]),
                  reads=[k_dwb16, k_identb], writes=[ptw[1]])
        fw.op("dve", lambda e: e.tensor_copy(out=wcol[:], in_=ptw[0][:, :, 0:31]), reads=[ptw[1]], writes=[k_wcol])
        for cc in range(2):
            for j in range(31):
                fw.op("dve", lambda e, cc=cc, j=j: e.tensor_scalar(out=diagw[:, cc, j, :], in0=identf[:], scalar1=wcol[:, cc, j:j + 1], scalar2=None, op0=ALU.mult),
                      reads=[k_identf, k_wcol], writes=[k_diagw])
        for cc in range(2):
            fw.op("pool", lambda e, cc=cc: e.memset(hg[:, cc, 0:PAD], 0.0), writes=[k_hg[cc][0]])
        if CONV_STOP < 2:
            fw.release(m)
            return
        wts = [(fw.sb([128, 8, 128], BF16), fw.trk()) for _ in range(2)]
        pa = [(fw.ps([128, 512], F32), fw.trk()) for _ in range(2)]
        pg = pw
        sg = [(fw.sb([128, 512], F32), fw.trk()) for _ in range(2)]
        it = 0
        for cc in range(2):
            fw.dma("pool", wts[0][0][:], w_in[l, :, 2816 + cc * 128:2816 + (cc + 1) * 128].rearrange("(dc p) c -> p dc c", p=128), writes=[wts[0][1]], slot=wts[0][1])
            fw.dma("pool", wts[1][0][:], w_in[l, :, 3072 + cc * 128:3072 + (cc + 1) * 128].rearrange("(dc p) c -> p dc c", p=128), writes=[wts[1][1]], slot=wts[1][1])
            for tt in range(8):
                a_, g_, s_ = pa[it % 2], pg[it % 2], sg[it % 2]
                it += 1
                for dc in range(8):
                    fw.op("pe", lambda e, dc=dc: e.matmul(a_[0][:], lhsT=wts[0][0][:, dc, :], rhs=R1[:, dc, tt * 512:(tt + 1) * 512], start=(dc == 0), stop=(dc == 7)),
                          reads=[wts[0][1]] + k_hT[tt * 4:(tt + 1) * 4], writes=[a_[1]])
                for dc in range(8):
                    fw.op("pe", lambda e, dc=dc: e.matmul(g_[0][:], lhsT=wts[1][0][:, dc, :], rhs=R1[:, dc, tt * 512:(tt + 1) * 512], start=(dc == 0), stop=(dc == 7)),
                          reads=[wts[1][1]] + k_hT[tt * 4:(tt + 1) * 4], writes=[g_[1]])
                sigmoid_from(s_[0][:], g_[0][:], [g_[1], cols["bg"][1]], [s_[1]], nbias=cols["bg"][0][:, cc:cc + 1])
                fw.op("dve", lambda e: e.scalar_tensor_tensor(out=hg[:, cc, PAD + tt * 512:PAD + (tt + 1) * 512], in0=a_[0][:], scalar=cols["ba"][0][:, cc:cc + 1], in1=s_[0][:], op0=ALU.add, op1=ALU.mult),
                      reads=[a_[1], cols["ba"][1], s_[1]], writes=[k_hg[cc][1 + tt]])
        if CONV_STOP < 3:
            fw.release(m)
            return
        pc = [(fw.ps([128, 512], F32), fw.trk()) for _ in range(2)]
        yc = [(fw.sb([128, 512], F32), fw.trk()) for _ in range(2)]
        ysq = [(fw.sb([128, 512], BF16), fw.trk()) for _ in range(2)]
        ycb = [(fw.sb([128, 512], BF16), fw.trk()) for _ in range(2)]
        mean = (fw.sb([128, 512], F32), fw.trk()); var = (fw.sb([128, 512], F32), fw.trk())
        tt1 = (fw.sb([128, 512], F32), fw.trk()); tt2 = (fw.sb([128, 512], F32), fw.trk())
        for tt in range(8):
            for cc in range(2):
                rd = [k_diagw, k_hg[cc][1 + tt], k_hg[cc][tt]]
                for j in range(31):
                    fw.op("pe", lambda e, cc=cc, j=j: e.matmul(pc[cc][0][:], lhsT=diagw[:, cc, j, :], rhs=hg[:, cc, tt * 512 + j:tt * 512 + j + 512], start=(j == 0), stop=(j == 30)),
                          reads=rd, writes=[pc[cc][1]])
                fw.op("dve", lambda e, cc=cc: e.tensor_scalar(out=yc[cc][0][:], in0=pc[cc][0][:], scalar1=cols["dwb"][0][:, cc:cc + 1], scalar2=None, op0=ALU.add), reads=[pc[cc][1], cols["dwb"][1]], writes=[yc[cc][1]])
                fw.op("act", lambda e, cc=cc: e.activation(out=ysq[cc][0][:], in_=yc[cc][0][:], func=AF.Square), reads=[yc[cc][1]], writes=[ysq[cc][1]])
                fw.op("act", lambda e, cc=cc: e.activation(out=ycb[cc][0][:], in_=yc[cc][0][:], func=AF.Copy), reads=[yc[cc][1]], writes=[ycb[cc][1]])
            if CONV_STOP < 4:
                continue
            for cc in range(2):
                fw.op("pe", lambda e, cc=cc: e.matmul(pw[0][0][:], lhsT=onesf[:], rhs=ycb[cc][0][:], start=(cc == 0), stop=(cc == 1)), reads=[k_onesf, ycb[cc][1]], writes=[pw[0][1]])
            for cc in range(2):
                fw.op("pe", lambda e, cc=cc: e.matmul(pw[1][0][:], lhsT=onesf[:], rhs=ysq[cc][0][:], start=(cc == 0), stop=(cc == 1)), reads=[k_onesf, ysq[cc][1]], writes=[pw[1][1]])
            if CONV_STOP < 5:
                continue
            fw.op("act", lambda e: e.activation(out=mean[0][:], in_=pw[0][0][:], func=AF.Copy), reads=[pw[0][1]], writes=[mean[1]])
            fw.op("dve", lambda e: e.tensor_tensor(out=var[0][:], in0=mean[0][:], in1=mean[0][:], op=ALU.mult), reads=[mean[1]], writes=[var[1]])
            fw.op("dve", lambda e: e.tensor_tensor(out=var[0][:], in0=pw[1][0][:], in1=var[0][:], op=ALU.subtract), reads=[pw[1][1], var[1]], writes=[var[1]])
            rstd_from("act", var[0][:], var[0][:], 1.0, [var[1]], [var[1]])
            for cc in range(2):
                fw.op("dve", lambda e, cc=cc: e.tensor_tensor(out=tt1[0][:], in0=yc[cc][0][:], in1=mean[0][:], op=ALU.subtract), reads=[yc[cc][1], mean[1]], writes=[tt1[1]])
                fw.op("dve", lambda e, cc=cc: e.tensor_tensor(out=tt1[0][:], in0=tt1[0][:], in1=var[0][:], op=ALU.mult), reads=[tt1[1], var[1]], writes=[tt1[1]])
                fw.op("dve", lambda e, cc=cc: e.tensor_scalar(out=tt1[0][:], in0=tt1[0][:], scalar1=cols["lng"][0][:, cc:cc + 1], scalar2=cols["lnb"][0][:, cc:cc + 1], op0=ALU.mult, op1=ALU.add),
                      reads=[tt1[1], cols["lng"][1], cols["lnb"][1]], writes=[tt1[1]])
                sigmoid_from(tt2[0][:], tt1[0][:], [tt1[1]], [tt2[1]])
                fw.op("pool", lambda e, cc=cc: e.tensor_tensor(out=R2[:, 6 + cc, tt * 512:(tt + 1) * 512], in0=tt1[0][:], in1=tt2[0][:], op=ALU.mult),
                      reads=[tt1[1], tt2[1]], writes=[k_mx[6 + cc][tt]])
        fw.release(m)

    def phase_C1(l, xin, k_xin, xmid, k_xmid, k_w1):
        m = fw.mark()
        wo = fw.sb([128, 8, D], BF16); k_wo = [fw.trk() for _ in range(2)]
        for i in range(2):
            fw.dma("pool", wo[:, :, i * 512:(i + 1) * 512], w_out[l, :, i * 512:(i + 1) * 512].rearrange("(cc p) d -> p cc d", p=128), writes=[k_wo[i]], slot=k_wo[i])
        for i in range(8):
            fw.dma("pool", R1[:, :, i * 512:(i + 1) * 512], w_ff1[l, :, i * 512:(i + 1) * 512].rearrange("(dc p) f -> p dc f", p=128), writes=[k_w1[i]], slot=k_w1[i])
        xts = [(fw.sb([128, D], F32), fw.trk()) for _ in range(3)]
        xos = [(fw.sb([128, D], F32), fw.trk()) for _ in range(3)]
        py = [(fw.ps([128, D], F32), fw.trk()) for _ in range(2)]
        for tb in range(NTB):
            xt, k_xt = xts[tb % 3]; xo, k_xo = xos[tb % 3]; p_, k_p = py[tb % 2]
            fw.dma("sp", xt[:], xin[tb * 128:(tb + 1) * 128, :], reads=[k_xin], writes=[k_xt], slot=k_xt)
            for half in range(2):
                for cc in range(8):
                    fw.op("pe", lambda e, cc=cc, half=half: e.matmul(p_[:, half * 512:(half + 1) * 512], lhsT=R2[:, cc, tb * 128:(tb + 1) * 128], rhs=wo[:, cc, half * 512:(half + 1) * 512], start=(cc == 0), stop=(cc == 7)),
                          reads=[k_mx[cc][tb // 4], k_wo[half]], writes=[k_p])
            for half in range(2):
                fw.op("dve", lambda e, half=half: e.tensor_tensor(out=xo[:, half * 512:(half + 1) * 512], in0=p_[:, half * 512:(half + 1) * 512], in1=xt[:, half * 512:(half + 1) * 512], op=ALU.add),
                      reads=[k_p, k_xt], writes=[k_xo])
            fw.dma("sp", xmid[tb * 128:(tb + 1) * 128, :], xo[:], reads=[k_xo], writes=[k_xmid], slot=k_xo)
        fw.release(m)

    def phase_C2(l, xmid, k_xmid, xout, k_xout, k_w1):
        m = fw.mark()
        W1 = R1
        W2 = R2[:].rearrange("p c t -> p (c t)").rearrange("p (fc d) -> p fc d", d=D)
        k_w2 = [fw.trk() for _ in range(8)]
        for i in range(8):
            fw.dma("pool", W2[:, i * 4:(i + 1) * 4, :], w_ff2[l, i * 512:(i + 1) * 512, :].rearrange("(fc p) d -> p fc d", p=128), writes=[k_w2[i]], slot=k_w2[i])
        gt = fw.sb([128, 8], F32); k_gt = fw.trk()
        load_cols(gt[:], k_gt, mlp_g[l, :], 8)
        TT = 256
        xts = [(fw.sb([128, 2, D], F32), [fw.trk(), fw.trk()]) for _ in range(2)]
        xos = [(fw.sb([128, 2, D], F32), [fw.trk(), fw.trk()]) for _ in range(2)]
        res = (fw.sb([128, D], BF16), fw.trk(), fw.sb([128, 2], F32), fw.trk(),
               fw.sb([128, D], BF16), fw.trk(), fw.ps([128, 8, 128], BF16), fw.trk())
        h2T = [(fw.sb([128, 8, TT], BF16), [fw.trk(), fw.trk()]) for _ in range(2)]
        py = [(fw.ps([128, D], F32), fw.trk()) for _ in range(2)]
        pf = [(fw.ps([128, 512], F32), fw.trk()) for _ in range(3)]
        rl = [(fw.sb([128, TT], F32), fw.trk()) for _ in range(3)]
        fb = [(fw.sb([128, TT], BF16), fw.trk()) for _ in range(3)]
        for ti in range(T // TT):
            xt, k_xt = xts[ti % 2]; xo, k_xo = xos[ti % 2]; hT2, k_h2 = h2T[ti % 2]
            for b2 in range(2):
                tb = ti * 2 + b2
                fw.dma("sp", xt[:, b2, :], xmid[tb * 128:(tb + 1) * 128, :], reads=[k_xmid], writes=[k_xt[b2]], slot=k_xt[b2])
                norm_transpose_block(xt[:, b2, :], k_xt[b2], gt, k_gt, res, hT2[:, :, b2 * 128:(b2 + 1) * 128], k_h2[b2])

            def stF(fc):
                p_, k_p = pf[fc % 3]
                for dc in range(8):
                    fw.op("pe", lambda e, dc=dc: e.matmul(p_[:, 0:TT], lhsT=W1[:, dc, fc * 128:(fc + 1) * 128], rhs=hT2[:, dc, :], start=(dc == 0), stop=(dc == 7)),
                          reads=[k_w1[fc // 4]] + k_h2, writes=[k_p])
                r_, k_r = rl[fc % 3]; f_, k_f = fb[fc % 3]
                fw.op("act", lambda e: e.activation(out=r_[:], in_=p_[:, 0:TT], func=AF.Relu), reads=[k_p], writes=[k_r])
                fw.op("pool", lambda e: e.tensor_tensor(out=f_[:], in0=r_[:], in1=r_[:], op=ALU.mult), reads=[k_r], writes=[k_f])

            def stY(fc):
                f_, k_f = fb[fc % 3]
                for b2 in range(2):
                    for half in range(2):
                        fw.op("pe", lambda e, b2=b2, half=half: e.matmul(py[b2][0][:, half * 512:(half + 1) * 512], lhsT=f_[:, b2 * 128:(b2 + 1) * 128], rhs=W2[:, fc, half * 512:(half + 1) * 512], start=(fc == 0), stop=(fc == 31)),
                              reads=[k_f, k_w2[fc // 4]], writes=[py[b2][1]])

            for i in range(32 + 2):
                if i < 32:
                    stF(i)
                if 0 <= i - 2 < 32:
                    stY(i - 2)
            for b2 in range(2):
                tb = ti * 2 + b2
                for half in range(2):
                    fw.op("dve", lambda e, b2=b2, half=half: e.tensor_tensor(out=xo[:, b2, half * 512:(half + 1) * 512], in0=py[b2][0][:, half * 512:(half + 1) * 512], in1=xt[:, b2, half * 512:(half + 1) * 512], op=ALU.add),
                          reads=[py[b2][1], k_xt[b2]], writes=[k_xo[b2]])
                fw.dma("sp", xout[tb * 128:(tb + 1) * 128, :], xo[:, b2, :], reads=[k_xo[b2]], writes=[k_xout], slot=k_xo[b2])
        fw.release(m)

    bufs = {0: (x_in, k_dram["x"], s1, k_dram["s1"], s0, k_dram["s0"]),
            1: (s0, k_dram["s0"], s1, k_dram["s1"], y_out, k_dram["y"])}
    if nlayers == 1:
        bufs[0] = (x_in, k_dram["x"], s1, k_dram["s1"], y_out, k_dram["y"])
    for l in range(nlayers):
        xin, k_xin, xmid, k_xmid, xout, k_xout = bufs[l]
        phase_A(l, xin, k_xin)
        if dbg == "hT":
            break
        for hp in range(2):
            if "sb" not in skip:
                phase_SB(l, hp)
        if dbg in ("sb", "sbproj"):
            break
        if "ret" not in skip:
            phase_RET(l)
        if dbg == "ret":
            break
        if "conv" not in skip:
            phase_CONV(l)
        if dbg == "mixed":
            break
        k_w1 = [fw.trk() for _ in range(8)]
        phase_C1(l, xin, k_xin, xmid, k_xmid, k_w1)
        if dbg == "c1":
            break
        phase_C2(l, xmid, k_xmid, xout, k_xout, k_w1)
    if dbg in ("hT", "sb", "ret", "mixed", "sbproj"):
        src = R1 if dbg == "hT" else R2
        m = fw.mark()
        st = [(fw.sb([128, 2048], F32), fw.trk()) for _ in range(2)]
        i = 0
        for c in dump_chunks:
            for hf in range(2):
                s_, k_s = st[i % 2]; i += 1
                fw.op("dve", lambda e: e.tensor_copy(out=s_[:], in_=src[:, c, hf * 2048:(hf + 1) * 2048]), reads=[], writes=[k_s])
                fw.dma("sp", dbg_out[:, c, hf * 2048:(hf + 1) * 2048], s_[:], reads=[k_s], writes=[k_dram["dbg"]], slot=k_s)
        fw.release(m)
    fw.barrier()
    return fw


def kernel(**inputs):
    global _CONSTS
    if _CONSTS is None:
        _CONSTS = _consts()
    if "nc" not in _NC_CACHE:
        p1 = build()
        _NC_CACHE["nc"] = build(needed=p1.waited)
    fw = _NC_CACHE["nc"]
    x = np.ascontiguousarray(np.asarray(inputs["x"], dtype=np.float32))
    shared = {k: np.ascontiguousarray(np.asarray(v, dtype=np.float32)) for k, v in inputs.items() if k != "x"}
    shared.update(_CONSTS)
    in_maps = []
    for b in range(8):
        mp = dict(shared)
        mp["x"] = x[b]
        in_maps.append(mp)
    res = run_bass_kernel_spmd(fw.nc, in_maps, core_ids=list(range(8)))
    return np.stack([np.asarray(r["y"], dtype=np.float32) for r in res.results], axis=0)
```

```python
import numpy as np
import concourse.bass as bass
import concourse.mybir as mybir
from concourse.bass_utils import run_bass_kernel_spmd

F32 = mybir.dt.float32
BF16 = mybir.dt.bfloat16
AF = mybir.ActivationFunctionType
ALU = mybir.AluOpType
AX = mybir.AxisListType

T = 4096
D = 1024
NTB = 32
DFF = 4096
DEPTH = 2
INC = 3328
EPS = 1e-6
NEG = -30000.0
RET_STOP = 99
CONV_STOP = 99


class Trk:
    __slots__ = ("name", "lw", "rd", "dsem")

    def __init__(self, name):
        self.name = name
        self.lw = {}
        self.rd = {}
        self.dsem = None


class Eng:
    def __init__(self, name, h, sem):
        self.name = name
        self.h = h
        self.sem = sem
        self.count = 0
        self.seen = {}


class FW:
    def __init__(self, needed=None):
        self.needed = None
        if needed is not None:
            self.needed = {k: sorted(v) for k, v in needed.items()}
            self.needed_set = {k: set(v) for k, v in needed.items()}
        self.waited = {}
        self.nc = bass.Bass("TRN2", target_bir_lowering=False)
        nc = self.nc
        self._stack = []
        self.engs = {}
        for name, h in (("pe", nc.tensor), ("act", nc.scalar), ("dve", nc.vector),
                        ("pool", nc.gpsimd), ("sp", nc.sync)):
            sem = self.enter(nc.semaphore("sem_" + name))
            self.engs[name] = Eng(name, h, sem)
        self.dsem_tot = {}
        self.free_dsems = []
        self.free_dsems_sw = []
        self._scope_dsems = []
        self._persist = []
        self.nwait = 0
        self.ninc = 0
        self.ninst = 0
        self._uid = 0

    def enter(self, cm):
        v = cm.__enter__()
        self._stack.append(cm)
        return v

    def uid(self, p="t"):
        self._uid += 1
        return "%s%d" % (p, self._uid)

    def sb(self, shape, dt, name=None):
        return self.enter(self.nc.sbuf_tensor(name or self.uid("sb"), list(shape), dt))

    def ps(self, shape, dt, name=None):
        return self.enter(self.nc.psum_tensor(name or self.uid("ps"), list(shape), dt))

    def trk(self, name=None):
        return Trk(name or self.uid("k"))

    def mark(self):
        self._scope_dsems.append([])
        return len(self._stack)

    def release(self, mark):
        self.barrier()
        while len(self._stack) > mark:
            cm = self._stack.pop()
            cm.__exit__(None, None, None)
        for is_sw, sem in self._scope_dsems.pop():
            (self.free_dsems_sw if is_sw else self.free_dsems).append(sem)

    def _emit_wait(self, eng, s, k, v):
        if k in self.dsem_tot:
            eng.h.wait_ge(s, v)
        else:
            self.waited.setdefault(k, set()).add(v)
            if self.needed is None:
                eng.h.wait_ge(s, v)
            else:
                import bisect
                lst = self.needed[k]
                r = bisect.bisect_left(lst, v)
                assert r < len(lst) and lst[r] == v, (k, v)
                eng.h.wait_ge(s, r + 1)
        self.nwait += 1

    def _deps(self, reads, writes):
        deps = {}

        def add(d):
            for k, (s, v) in d.items():
                if k not in deps or deps[k][1] < v:
                    deps[k] = (s, v)
        for r in reads:
            add(r.lw)
        for w in writes:
            add(w.lw)
            add(w.rd)
        return deps

    def _wait(self, eng, deps):
        for k, (s, v) in deps.items():
            if k in self.dsem_tot:
                v = self.dsem_tot[k][1]
            if eng.name == "pe" and k == eng.sem.name:
                continue
            if eng.seen.get(k, 0) >= v:
                continue
            self._emit_wait(eng, s, k, v)
            eng.seen[k] = v

    def op(self, ename, fn, reads=(), writes=()):
        eng = self.engs[ename]
        self._wait(eng, self._deps(reads, writes))
        inst = fn(eng.h)
        eng.count += 1
        if self.needed is None or eng.count in self.needed_set.get(eng.sem.name, ()):
            inst.then_inc(eng.sem, 1)
            self.ninc += 1
        ev = (eng.sem, eng.count)
        k = eng.sem.name
        for w in writes:
            w.lw = {k: ev}
            w.rd = {}
        for r in reads:
            r.rd[k] = ev
        self.ninst += 1
        return inst

    def dma(self, qname, out, in_, reads=(), writes=(), slot=None, **kw):
        eng = self.engs[qname]
        self._wait(eng, self._deps(reads, writes))
        if slot.dsem is None:
            pool_ = self.free_dsems_sw if qname == "pool" else self.free_dsems
            if pool_:
                slot.dsem = pool_.pop()
            else:
                cm = self.nc.semaphore(self.uid("dsem"))
                slot.dsem = cm.__enter__()
                self._persist.append(cm)
                self.dsem_tot[slot.dsem.name] = (slot.dsem, 0)
            if self._scope_dsems:
                self._scope_dsems[-1].append((qname == "pool", slot.dsem))
        inst = eng.h.dma_start(out=out, in_=in_, **kw)
        tot = self.dsem_tot[slot.dsem.name][1] + 16
        self.dsem_tot[slot.dsem.name] = (slot.dsem, tot)
        inst.then_inc(slot.dsem, 16)
        ev = (slot.dsem, tot)
        k = slot.dsem.name
        for w in writes:
            w.lw = {k: ev}
            w.rd = {}
        for r in reads:
            r.rd[k] = ev
        self.ninst += 1
        return inst

    def barrier(self):
        for eng in self.engs.values():
            for e in self.engs.values():
                if e is eng or e.count == 0:
                    continue
                if eng.seen.get(e.sem.name, 0) < e.count:
                    self._emit_wait(eng, e.sem, e.sem.name, e.count)
                    eng.seen[e.sem.name] = e.count
            for k, (s, tot) in self.dsem_tot.items():
                if tot and eng.seen.get(k, 0) < tot:
                    self._emit_wait(eng, s, k, tot)
                    eng.seen[k] = tot


def _consts():
    c = {}
    i = np.arange(128)
    c["c_ident"] = np.eye(128, dtype=np.float32)
    c["c_blk"] = (i[:, None] // 64 == i[None, :] // 64).astype(np.float32)
    c["c_negtri"] = -(i[:, None] >= i[None, :]).astype(np.float32)
    c["c_negones"] = -np.ones((128, 128), np.float32)
    c["c_maskneg"] = np.where(i[:, None] >= i[None, :], NEG, 0.0).astype(np.float32)
    c["c_ones"] = np.full((128, 128), 1.0 / 256.0, np.float32)
    inv = (1.0 / (10000.0 ** np.linspace(0.0, 1.0, 32, dtype=np.float32))).astype(np.float32)
    pos = np.arange(T, dtype=np.float32)
    ang = (pos[:, None] * inv[None, :]).astype(np.float32)
    cos = np.cos(ang).astype(np.float32)
    sin = np.sin(ang).astype(np.float32)
    cos2 = np.repeat(cos, 2, axis=1)
    s2 = np.stack([-sin, sin], axis=-1).reshape(T, 64)
    rot = np.concatenate([cos2, s2, cos2 * 0.125, s2 * 0.125], axis=1).astype(np.float32)
    c["c_rot"] = rot.reshape(NTB, 128, 256)
    h = np.arange(8, dtype=np.float32)
    log_g = np.log(1.0 - np.exp2(-5.0 - h)).astype(np.float32)
    j = np.arange(128, dtype=np.float32)
    rel = j[None, :] - j[:, None]
    dt_ = np.where(rel[None] >= 0, np.exp(log_g[:, None, None] * np.maximum(rel[None], 0.0)), 0.0)
    c["c_dt"] = np.ascontiguousarray(dt_.transpose(1, 0, 2)).astype(np.float32)
    qd = np.exp(log_g[:, None] * (j + 1.0)[None, :]).astype(np.float32)
    qdec = np.zeros((128, 4, 128), np.float32)
    g128 = np.zeros((128, 4, 64), np.float32)
    gC = np.exp(log_g * 128.0).astype(np.float32)
    for p in range(128):
        for pr in range(4):
            hh = pr * 2 + p // 64
            qdec[p, pr, :] = qd[hh]
            g128[p, pr, :] = gC[hh]
    c["c_qdec"] = qdec
    c["c_g128"] = g128
    kd = np.exp(log_g[:, None] * (127.0 - j)[None, :]).astype(np.float32)
    c["c_kdec"] = np.ascontiguousarray(np.repeat(kd.T[:, :, None], 64, axis=2)).astype(np.float32)
    return c


_CONSTS = None
_NC_CACHE = {}


def build(nlayers=DEPTH, dbg=None, skip=(), dump_chunks=range(8), needed=None):
    fw = FW(needed)
    nc = fw.nc

    def din(name, shape):
        shape = list(shape)
        if shape[0] == DEPTH and not name.startswith("c_"):
            shape[0] = nlayers
        return nc.dram_tensor(name, shape, F32, kind="ExternalInput").ap()

    x_in = din("x", [T, D])
    mix_g = din("mix_norm_g", [DEPTH, D])
    w_in = din("w_in", [DEPTH, D, INC])
    sbq_g = din("sb_q_norm_g", [DEPTH, 64])
    sbk_g = din("sb_k_norm_g", [DEPTH, 64])
    ret_g = din("ret_norm_g", [DEPTH, 512])
    cpw_b = din("conv_pw_b", [DEPTH, 512])
    cdw_w = din("conv_dw_w", [DEPTH, 31, 256])
    cdw_b = din("conv_dw_b", [DEPTH, 256])
    cln_g = din("conv_ln_g", [DEPTH, 256])
    cln_b = din("conv_ln_b", [DEPTH, 256])
    w_out = din("w_out", [DEPTH, D, D])
    mlp_g = din("mlp_norm_g", [DEPTH, D])
    w_ff1 = din("w_ff1", [DEPTH, D, DFF])
    w_ff2 = din("w_ff2", [DEPTH, DFF, D])
    c_ident = din("c_ident", [128, 128])
    c_blk = din("c_blk", [128, 128])
    c_negtri = din("c_negtri", [128, 128])
    c_negones = din("c_negones", [128, 128])
    c_maskneg = din("c_maskneg", [128, 128])
    c_ones = din("c_ones", [128, 128])
    c_rot = din("c_rot", [NTB, 128, 256])
    c_dt = din("c_dt", [128, 8, 128])
    c_qdec = din("c_qdec", [128, 4, 128])
    c_g128 = din("c_g128", [128, 4, 64])
    c_kdec = din("c_kdec", [128, 8, 64])
    y_out = nc.dram_tensor("y", [T, D], F32, kind="ExternalOutput").ap()
    s0 = nc.dram_tensor("scr0", [T, D], F32, kind="Internal").ap()
    s1 = nc.dram_tensor("scr1", [T, D], F32, kind="Internal").ap()
    dbg_out = None
    if dbg is not None:
        dbg_out = nc.dram_tensor("dbg", [128, 8, T], F32, kind="ExternalOutput").ap()

    k_dram = {"x": fw.trk("dx"), "s0": fw.trk("ds0"), "s1": fw.trk("ds1"), "y": fw.trk("dy"), "dbg": fw.trk("ddbg")}

    R1 = fw.sb([128, 8, T], BF16, "R1")
    R2 = fw.sb([128, 8, T], BF16, "R2")
    k_hT = [fw.trk("hT%d" % i) for i in range(NTB)]
    k_mx = [[fw.trk("mx%d_%d" % (c, i)) for i in range(8)] for c in range(8)]
    identb = fw.sb([128, 128], BF16, "identb"); k_identb = fw.trk("identb")
    identf = fw.sb([128, 128], F32, "identf"); k_identf = fw.trk("identf")
    blkb = fw.sb([128, 128], BF16, "blkb"); k_blkb = fw.trk("blkb")
    negtri = fw.sb([128, 128], BF16, "negtri"); k_negtri = fw.trk("negtri")
    negones = fw.sb([128, 128], BF16, "negones"); k_negones = fw.trk("negones")
    maskneg = fw.sb([128, 128], BF16, "maskneg"); k_maskneg = fw.trk("maskneg")
    onesf = fw.sb([128, 128], BF16, "onesf"); k_onesf = fw.trk("onesf")
    zerob = fw.sb([128, 128], BF16, "zerob"); k_zerob = fw.trk("zerob")
    fw.op("pool", lambda e: e.memset(zerob[:], 0.0), writes=[k_zerob])
    fw.dma("pool", identb[:], c_ident[:, :], writes=[k_identb], slot=k_identb)
    fw.dma("sp", identf[:], c_ident[:, :], writes=[k_identf], slot=k_identf)
    fw.dma("pool", blkb[:], c_blk[:, :], writes=[k_blkb], slot=k_blkb)
    fw.dma("pool", negtri[:], c_negtri[:, :], writes=[k_negtri], slot=k_negtri)
    fw.dma("pool", negones[:], c_negones[:, :], writes=[k_negones], slot=k_negones)
    fw.dma("pool", maskneg[:], c_maskneg[:, :], writes=[k_maskneg], slot=k_maskneg)
    fw.dma("pool", onesf[:], c_ones[:, :], writes=[k_onesf], slot=k_onesf)

    def load_cols(dst, k_dst, src_row_ap, n):
        with nc.allow_non_contiguous_dma(reason="tiny param vector"):
            fw.dma("sp", dst, src_row_ap.rearrange("(c p) -> p c", p=128), writes=[k_dst], slot=k_dst)

    def rstd_from(ename_unused, out_ap, in_ap, scale, reads, writes):
        fw.op("act", lambda e: e.activation(out=out_ap, in_=in_ap, func=AF.Ln, scale=scale, bias=EPS),
              reads=reads, writes=writes)
        fw.op("act", lambda e: e.activation(out=out_ap, in_=out_ap, func=AF.Exp, scale=-0.5),
              reads=writes, writes=writes)

    def sigmoid_from(out_ap, in_ap, reads, writes, nbias=None):
        if nbias is None:
            fw.op("act", lambda e: e.activation(out=out_ap, in_=in_ap, func=AF.Exp, scale=-1.0), reads=reads, writes=writes)
        else:
            fw.op("act", lambda e: e.activation(out=out_ap, in_=in_ap, func=AF.Exp, scale=-1.0, bias=nbias), reads=reads, writes=writes)
        fw.op("act", lambda e: e.activation(out=out_ap, in_=out_ap, func=AF.Ln, bias=1.0), reads=writes, writes=writes)
        fw.op("act", lambda e: e.activation(out=out_ap, in_=out_ap, func=AF.Exp, scale=-1.0), reads=writes, writes=writes)

    def norm_transpose_block(xt, k_xt, gt, k_gt, res, dst_ap, k_dst):
        junk, k_junk, st, k_st, hb, k_hb, pt, k_pt = res
        fw.op("act", lambda e: e.activation(out=junk[:], in_=xt, func=AF.Square, accum_out=st[:, 0:1]),
              reads=[k_xt], writes=[k_junk, k_st])
        rstd_from("act", st[:, 1:2], st[:, 0:1], 1.0 / D, [k_st], [k_st])
        fw.op("dve", lambda e: e.tensor_scalar(out=hb[:], in0=xt, scalar1=st[:, 1:2], scalar2=None, op0=ALU.mult),
              reads=[k_xt, k_st], writes=[k_hb])
        for dc in range(8):
            fw.op("pe", lambda e, dc=dc: e.transpose(out=pt[:, dc, :], in_=hb[:, dc * 128:(dc + 1) * 128], identity=identb[:]),
                  reads=[k_hb, k_identb], writes=[k_pt])
        fw.op("dve", lambda e: e.tensor_tensor(out=dst_ap, in0=pt[:], in1=gt[:].unsqueeze(2).broadcast_to([128, 8, 128]), op=ALU.mult),
              reads=[k_pt, k_gt], writes=[k_dst])

    def phase_A(l, xin, k_xin):
        m = fw.mark()
        gt = fw.sb([128, 8], F32); k_gt = fw.trk()
        load_cols(gt[:], k_gt, mix_g[l, :], 8)
        nb = 3
        xts = [(fw.sb([128, D], F32), fw.trk()) for _ in range(nb)]
        ress = []
        for _ in range(2):
            ress.append((fw.sb([128, D], BF16), fw.trk(), fw.sb([128, 2], F32), fw.trk(),
                         fw.sb([128, D], BF16), fw.trk(), fw.ps([128, 8, 128], BF16), fw.trk()))
        for tb in range(NTB):
            xt, k_xt = xts[tb % nb]
            fw.dma("sp", xt[:], xin[tb * 128:(tb + 1) * 128, :], reads=[k_xin], writes=[k_xt], slot=k_xt)
            norm_transpose_block(xt[:], k_xt, gt, k_gt, ress[tb % 2], R1[:, :, tb * 128:(tb + 1) * 128], k_hT[tb])
        fw.release(m)

    def phase_SB(l, hp):
        m = fw.mark()
        qT = fw.sb([128, T], BF16); kT = fw.sb([128, 2, T], BF16); vv = fw.sb([128, NTB, 128], BF16)
        k_kT0 = fw.trk()
        fw.op("pool", lambda e: e.memset(kT[:], 0.0), writes=[k_kT0])
        k_qT = [fw.trk() for _ in range(8)]; k_kT = [fw.trk() for _ in range(8)]; k_v = [fw.trk() for _ in range(8)]
        gq = fw.sb([128, 1], F32); k_gq = fw.trk(); gk = fw.sb([128, 1], F32); k_gk = fw.trk()
        with nc.allow_non_contiguous_dma(reason="tiny param vector"):
            for half in range(2):
                fw.dma("sp", gq[half * 64:(half + 1) * 64, :], sbq_g[l, :].rearrange("(p o) -> p o", o=1), writes=[k_gq], slot=k_gq)
                fw.dma("sp", gk[half * 64:(half + 1) * 64, :], sbk_g[l, :].rearrange("(p o) -> p o", o=1), writes=[k_gk], slot=k_gk)
        fw.op("dve", lambda e: e.tensor_scalar(out=gq[:], in0=gq[:], scalar1=0.125, scalar2=None, op0=ALU.mult), reads=[k_gq], writes=[k_gq])
        wts = [(fw.sb([128, 8, 128], BF16), fw.trk()) for _ in range(2)]
        pbank = [(fw.ps([128, 512], F32), fw.trk()) for _ in range(2)]
        pss = [(fw.ps([128, 512], F32), fw.trk()) for _ in range(2)]
        sqb = [(fw.sb([128, 512], BF16), fw.trk()) for _ in range(2)]
        rsd = [(fw.sb([128, 512], F32), fw.trk()) for _ in range(2)]
        it = 0
        for which in range(2):
            col0 = (0 if which == 0 else 256) + hp * 128
            wt, k_wt = wts[which]
            fw.dma("pool", wt[:], w_in[l, :, col0:col0 + 128].rearrange("(dc p) c -> p dc c", p=128), writes=[k_wt], slot=k_wt)
            dst = qT if which == 0 else kT
            kd = k_qT if which == 0 else k_kT
            gcol, k_gcol = (gq, k_gq) if which == 0 else (gk, k_gk)
            for tt in range(8):
                pq, k_pq = pbank[it % 2]; ps2, k_ps2 = pss[it % 2]; sq, k_sq = sqb[it % 2]; rs, k_rs = rsd[it % 2]
                it += 1
                for dc in range(8):
                    fw.op("pe", lambda e, dc=dc: e.matmul(pq[:], lhsT=wt[:, dc, :], rhs=R1[:, dc, tt * 512:(tt + 1) * 512], start=(dc == 0), stop=(dc == 7)),
                          reads=[k_wt] + k_hT[tt * 4:(tt + 1) * 4], writes=[k_pq])
                fw.op("act", lambda e: e.activation(out=sq[:], in_=pq[:], func=AF.Square), reads=[k_pq], writes=[k_sq])
                fw.op("pe", lambda e: e.matmul(ps2[:], lhsT=blkb[:], rhs=sq[:], start=True, stop=True), reads=[k_blkb, k_sq], writes=[k_ps2])
                rstd_from("act", rs[:], ps2[:], 1.0 / 64, [k_ps2], [k_rs])
                if which == 0:
                    fw.op("dve", lambda e: e.scalar_tensor_tensor(out=dst[:, tt * 512:(tt + 1) * 512], in0=pq[:], scalar=gcol[:, 0:1], in1=rs[:], op0=ALU.mult, op1=ALU.mult),
                          reads=[k_pq, k_gcol, k_rs], writes=[kd[tt]])
                else:
                    for hh_ in range(2):
                        fw.op("dve", lambda e, hh_=hh_: e.scalar_tensor_tensor(out=kT[hh_ * 64:(hh_ + 1) * 64, hh_, tt * 512:(tt + 1) * 512], in0=pq[hh_ * 64:(hh_ + 1) * 64, :],
                                                                              scalar=gcol[hh_ * 64:(hh_ + 1) * 64, 0:1], in1=rs[hh_ * 64:(hh_ + 1) * 64, :], op0=ALU.mult, op1=ALU.mult),
                              reads=[k_pq, k_gcol, k_rs, k_kT0], writes=[kd[tt]])
        wt, k_wt = wts[0]
        col0 = 512 + hp * 128
        fw.dma("pool", wt[:], w_in[l, :, col0:col0 + 128].rearrange("(dc p) c -> p dc c", p=128), writes=[k_wt], slot=k_wt)
        for tb in range(NTB):
            pq, k_pq = pbank[tb % 2]
            for dc in range(8):
                fw.op("pe", lambda e, dc=dc: e.matmul(pq[:, 0:128], lhsT=R1[:, dc, tb * 128:(tb + 1) * 128], rhs=wt[:, dc, :], start=(dc == 0), stop=(dc == 7)),
                      reads=[k_wt, k_hT[tb]], writes=[k_pq])
            fw.op("act", lambda e: e.activation(out=vv[:, tb, :], in_=pq[:, 0:128], func=AF.Copy), reads=[k_pq], writes=[k_v[tb // 4]])

        if dbg == 'sbproj':
            fw.release(m)
            return
        pz = [(fw.ps([128, 512], F32), fw.trk()) for _ in range(2)] + [pss[0]]
        plw = [(fw.ps([128, 512], F32), fw.trk()) for _ in range(2)] + [pss[1]]
        Eb = [(fw.sb([128, 512], F32), fw.trk()) for _ in range(3)]
        Lb = [(fw.sb([128, 512], BF16), fw.trk()) for _ in range(3)]
        Rb = fw.sb([128, 3, 512], BF16); k_R = [fw.trk() for _ in range(3)]
        wb = [(fw.sb([128, 512], BF16), fw.trk()) for _ in range(2)]
        for hh in range(2):
            p0 = hh * 64
            for qt in range(8):
                po, k_po = pbank[(hh * 8 + qt) % 2]
                fw.op("pool", lambda e: e.memset(Rb[:], 0.0), writes=k_R)
                fw.op("pe", lambda e: e.matmul(po[:, :], lhsT=zerob[:], rhs=qT[:, qt * 512:(qt + 1) * 512], start=True, stop=False),
                      reads=[k_zerob, k_qT[qt]], writes=[k_po])
                blocks = list(range(4 * qt + 3, -1, -1))
                nblk = len(blocks)
                q0 = qt * 512

                def zmm(dstp, b, last_stop):
                    kb = blocks[b]
                    kl = kb - 4 * qt
                    ksl = kT[:, hh, kb * 128:(kb + 1) * 128]
                    rk = [k_kT[kb // 4], k_qT[qt]]
                    c0 = kl * 128 if kl >= 0 else 0
                    fw.op("pe", lambda e: e.matmul(dstp[0][:, c0:512], lhsT=ksl, rhs=qT[:, q0 + c0:q0 + 512], start=True, stop=(last_stop and kl < 0)),
                          reads=rk, writes=[dstp[1]])
                    if kl >= 0:
                        fw.op("pe", lambda e: e.matmul(dstp[0][:, c0:c0 + 128], lhsT=identb[:], rhs=maskneg[:], start=False, stop=last_stop),
                              reads=[k_identb, k_maskneg], writes=[dstp[1]])
                    return c0

                def stageA(b):
                    c0 = zmm(pz[b % 3], b, True)
                    E, k_E = Eb[b % 3]; L, k_L = Lb[b % 3]
                    fw.op("act", lambda e: e.activation(out=E[:, c0:], in_=pz[b % 3][0][:, c0:], func=AF.Exp), reads=[pz[b % 3][1]], writes=[k_E])
                    fw.op("act", lambda e: e.activation(out=L[:, c0:], in_=E[:, c0:], func=AF.Ln, bias=1.0), reads=[k_E], writes=[k_L])
                    if b + 1 < nblk:
                        fw.op("pool", lambda e: e.tensor_tensor(out=Rb[:, (b + 1) % 3, c0:], in0=Rb[:, b % 3, c0:], in1=L[:, c0:], op=ALU.add),
                              reads=[k_R[b % 3], k_L], writes=[k_R[(b + 1) % 3]])

                def stageB(b):
                    c0 = zmm(plw[b % 3], b, False)
                    L, k_L = Lb[b % 3]
                    fw.op("pe", lambda e: e.matmul(plw[b % 3][0][:, c0:], lhsT=negtri[:], rhs=L[:, c0:], start=False, stop=(b == 0)),
                          reads=[k_negtri, k_L], writes=[plw[b % 3][1]])
                    if b > 0:
                        fw.op("pe", lambda e: e.matmul(plw[b % 3][0][:, c0:], lhsT=negones[:], rhs=Rb[:, b % 3, c0:], start=False, stop=True),
                              reads=[k_negones, k_R[b % 3]], writes=[plw[b % 3][1]])
                    w, k_w = wb[b % 2]
                    fw.op("act", lambda e: e.activation(out=w[:, c0:], in_=plw[b % 3][0][:, c0:], func=AF.Exp), reads=[plw[b % 3][1]], writes=[k_w])

                def stageC(b):
                    kb = blocks[b]
                    kl = kb - 4 * qt
                    w, k_w = wb[b % 2]
                    last = (b == nblk - 1)
                    vsl = vv[:, kb, :]
                    rd = [k_v[kb // 4], k_w]
                    c0 = kl * 128 if kl >= 0 else 0
                    fw.op("pe", lambda e: e.matmul(po[:, c0:512], lhsT=vsl, rhs=w[:, c0:512], start=False, stop=last), reads=rd, writes=[k_po])
                    if last:
                        fw.op("dve", lambda e: e.tensor_copy(out=R2[p0:p0 + 64, hp, q0:q0 + 512], in_=po[p0:p0 + 64, :]), reads=[k_po], writes=[k_mx[hp][qt]])

                for i in range(nblk + 2):
                    if i < nblk:
                        stageA(i)
                    if 0 <= i - 1 < nblk:
                        stageB(i - 1)
                    if 0 <= i - 2 < nblk:
                        stageC(i - 2)
        fw.release(m)

    def phase_RET(l):
        m = fw.mark()
        wr = fw.sb([128, 8, 2048], BF16); k_wr = [fw.trk() for _ in range(4)]
        for i in range(4):
            fw.dma("pool", wr[:, :, i * 512:(i + 1) * 512], w_in[l, :, 768 + i * 512:768 + (i + 1) * 512].rearrange("(dc p) c -> p dc c", p=128),
                   writes=[k_wr[i]], slot=k_wr[i])
        dtab = fw.sb([128, 8, 128], F32); k_dtab = fw.trk()
        qdec = fw.sb([128, 4, 128], F32); k_qdec = fw.trk()
        kdec = fw.sb([128, 8, 64], F32); k_kdec = fw.trk()
        g128 = fw.sb([128, 4, 64], F32); k_g128 = fw.trk()
        rgb = fw.sb([128, 512], F32); k_rgb = fw.trk()
        fw.dma("sp", dtab[:], c_dt[:, :, :], writes=[k_dtab], slot=k_dtab)
        fw.dma("sp", qdec[:], c_qdec[:, :, :], writes=[k_qdec], slot=k_qdec)
        fw.dma("sp", kdec[:], c_kdec[:, :, :], writes=[k_kdec], slot=k_kdec)
        fw.dma("sp", g128[:], c_g128[:, :, :], writes=[k_g128], slot=k_g128)
        fw.dma("sp", rgb[:], ret_g[l, :].partition_broadcast(128), writes=[k_rgb], slot=k_rgb)
        rot = [(fw.sb([128, 256], F32), fw.trk()) for _ in range(2)]
        pA = (fw.ps([128, 512], F32), fw.trk()); pB = (fw.ps([128, 512], F32), fw.trk())
        ptq = (fw.ps([128, 4, 128], BF16), fw.trk()); ptk = (fw.ps([128, 4, 128], BF16), fw.trk())
        pS = [(fw.ps([128, 4, 128], F32), fw.trk()) for _ in range(2)]
        pO = (fw.ps([128, 512], F32), fw.trk()); pKV = (fw.ps([128, 4, 128], F32), fw.trk())
        tA = [(fw.sb([128, 512], F32), fw.trk()) for _ in range(2)]
        tB = [(fw.sb([128, 512], F32), fw.trk()) for _ in range(2)]
        rq = (fw.sb([128, 512], BF16), fw.trk()); rk = (fw.sb([128, 512], BF16), fw.trk())
        rkd = (fw.sb([128, 512], BF16), fw.trk()); vsb = (fw.sb([128, 512], BF16), fw.trk())
        gsil = (fw.sb([128, 512], F32), fw.trk())
        qTs = (fw.sb([128, 4, 128], BF16), fw.trk()); qdT = (fw.sb([128, 4, 128], BF16), fw.trk())
        kTz = (fw.sb([128, 4, 2, 128], BF16), fw.trk())
        fw.op("pool", lambda e: e.memset(kTz[0][:], 0.0), writes=[kTz[1]])
        AT = (fw.sb([128, 8, 128], BF16), fw.trk())
        sqo = (fw.sb([128, 512], F32), fw.trk()); ssr = (fw.sb([128, 8], F32), fw.trk())
        t1 = (fw.sb([128, 512], F32), fw.trk()); ytm = (fw.sb([128, 512], BF16), fw.trk())
        stf = (fw.sb([128, 4, 64], F32), fw.trk()); stb = (fw.sb([128, 4, 2, 64], BF16), fw.trk()); stt = (fw.sb([128, 4, 64], F32), fw.trk())
        fw.op("pool", lambda e: e.memset(stf[0][:], 0.0), writes=[stf[1]])
        fw.op("pool", lambda e: e.memset(stb[0][:], 0.0), writes=[stb[1]])

        def proj(pdst, wi, tb):
            for dc in range(8):
                fw.op("pe", lambda e, dc=dc: e.matmul(pdst[0][:], lhsT=R1[:, dc, tb * 128:(tb + 1) * 128], rhs=wr[:, dc, wi * 512:(wi + 1) * 512], start=(dc == 0), stop=(dc == 7)),
                      reads=[k_hT[tb], k_wr[wi]], writes=[pdst[1]])

        def v3(ap):
            return ap.rearrange("p (h e) -> p h e", h=8)

        def rotary(psrc, off, ta, tb_, dst, rt, k_rt):
            cos2 = rt[:, off:off + 64].unsqueeze(1).broadcast_to([128, 8, 64])
            s2 = rt[:, off + 64:off + 128]
            s2e = s2.rearrange("p (i two) -> p i two", two=2)[:, :, 0].unsqueeze(1).broadcast_to([128, 8, 32])
            s2o = s2.rearrange("p (i two) -> p i two", two=2)[:, :, 1].unsqueeze(1).broadcast_to([128, 8, 32])
            ps4 = psrc[0][:].rearrange("p (h i two) -> p h i two", h=8, two=2)
            tb4 = tb_[0][:].rearrange("p (h i two) -> p h i two", h=8, two=2)
            fw.op("dve", lambda e: e.tensor_tensor(out=v3(ta[0][:]), in0=v3(psrc[0][:]), in1=cos2, op=ALU.mult), reads=[psrc[1], k_rt], writes=[ta[1]])
            fw.op("dve", lambda e: e.tensor_tensor(out=tb4[:, :, :, 0], in0=ps4[:, :, :, 1], in1=s2e, op=ALU.mult), reads=[psrc[1], k_rt], writes=[tb_[1]])
            fw.op("dve", lambda e: e.tensor_tensor(out=tb4[:, :, :, 1], in0=ps4[:, :, :, 0], in1=s2o, op=ALU.mult), reads=[psrc[1], k_rt], writes=[tb_[1]])
            fw.op("pool", lambda e: e.tensor_tensor(out=dst[0][:], in0=ta[0][:], in1=tb_[0][:], op=ALU.add), reads=[ta[1], tb_[1]], writes=[dst[1]])

        gsil2 = [gsil, (fw.sb([128, 512], F32), fw.trk())]

        def r_head(n):
            rt, k_rt = rot[n % 2]
            fw.dma("sp", rt[:], c_rot[n, :, :], writes=[k_rt], slot=k_rt)
            proj(pA, 0, n)
            rotary(pA, 0, tA[0], tB[0], rq, rt, k_rt)
            proj(pB, 1, n)
            rotary(pB, 128, tA[1], tB[1], rk, rt, k_rt)
            fw.op("pool", lambda e: e.tensor_tensor(out=v3(rkd[0][:]), in0=v3(rk[0][:]), in1=kdec[:], op=ALU.mult), reads=[rk[1], k_kdec], writes=[rkd[1]])
            proj(pA, 2, n)
            fw.op("act", lambda e: e.activation(out=vsb[0][:], in_=pA[0][:], func=AF.Copy), reads=[pA[1]], writes=[vsb[1]])
            proj(pB, 3, n)
            sigmoid_from(gsil2[n % 2][0][:], pB[0][:], [pB[1]], [gsil2[n % 2][1]])
            fw.op("dve", lambda e: e.tensor_tensor(out=gsil2[n % 2][0][:], in0=pB[0][:], in1=gsil2[n % 2][0][:], op=ALU.mult), reads=[pB[1], gsil2[n % 2][1]], writes=[gsil2[n % 2][1]])
            fw.op("pool", lambda e: e.tensor_tensor(out=gsil2[n % 2][0][:], in0=gsil2[n % 2][0][:], in1=rgb[:], op=ALU.mult), reads=[gsil2[n % 2][1], k_rgb], writes=[gsil2[n % 2][1]])
            for pr in range(4):
                fw.op("pe", lambda e, pr=pr: e.transpose(out=ptq[0][:, pr, :], in_=rq[0][:, pr * 128:(pr + 1) * 128], identity=identb[:]), reads=[rq[1], k_identb], writes=[ptq[1]])
            for pr in range(4):
                fw.op("pe", lambda e, pr=pr: e.transpose(out=ptk[0][:, pr, :], in_=rk[0][:, pr * 128:(pr + 1) * 128], identity=identb[:]), reads=[rk[1], k_identb], writes=[ptk[1]])
            fw.op("act", lambda e: e.activation(out=qTs[0][:], in_=ptq[0][:], func=AF.Copy), reads=[ptq[1]], writes=[qTs[1]])
            fw.op("dve", lambda e: e.tensor_tensor(out=qdT[0][:], in0=ptq[0][:], in1=qdec[:], op=ALU.mult), reads=[ptq[1], k_qdec], writes=[qdT[1]])
            for hh in range(2):
                fw.op("act", lambda e, hh=hh: e.activation(out=kTz[0][hh * 64:(hh + 1) * 64, :, hh, :], in_=ptk[0][hh * 64:(hh + 1) * 64, :, :], func=AF.Copy), reads=[ptk[1]], writes=[kTz[1]])

        def r_mid(n):
            for h in range(8):
                pr, p0 = h // 2, (h % 2) * 64
                fw.op("pe", lambda e, h=h, pr=pr: e.matmul(pS[h // 4][0][:, h % 4, :], lhsT=kTz[0][:, pr, h % 2, :], rhs=qTs[0][:, pr, :], start=True, stop=True),
                      reads=[kTz[1], qTs[1]], writes=[pS[h // 4][1]])
            for half in range(2):
                fw.op("dve", lambda e, half=half: e.tensor_tensor(out=AT[0][:, half * 4:(half + 1) * 4, :], in0=pS[half][0][:], in1=dtab[:, half * 4:(half + 1) * 4, :], op=ALU.mult),
                      reads=[pS[half][1], k_dtab], writes=[AT[1]])
            for h in range(8):
                pr, p0 = h // 2, (h % 2) * 64
                fw.op("pe", lambda e, h=h: e.matmul(pO[0][:, h * 64:(h + 1) * 64], lhsT=AT[0][:, h, :], rhs=vsb[0][:, h * 64:(h + 1) * 64], start=True, stop=False),
                      reads=[AT[1], vsb[1]], writes=[pO[1]])
                fw.op("pe", lambda e, h=h, pr=pr: e.matmul(pO[0][:, h * 64:(h + 1) * 64], lhsT=qdT[0][:, pr, :], rhs=stb[0][:, pr, h % 2, :], start=False, stop=True),
                      reads=[qdT[1], stb[1]], writes=[pO[1]])
            for pr in range(4):
                fw.op("pe", lambda e, pr=pr: e.matmul(pKV[0][:, pr, :], lhsT=rkd[0][:, pr * 128:(pr + 1) * 128], rhs=vsb[0][:, pr * 128:(pr + 1) * 128], start=True, stop=True),
                      reads=[rkd[1], vsb[1]], writes=[pKV[1]])
            fw.op("pool", lambda e: e.tensor_tensor(out=stt[0][:], in0=stf[0][:], in1=g128[:], op=ALU.mult), reads=[stf[1], k_g128], writes=[stt[1]])
            for half in range(2):
                p0 = half * 64
                fw.op("dve", lambda e, p0=p0: e.tensor_tensor(out=stf[0][p0:p0 + 64, :, :], in0=stt[0][p0:p0 + 64, :, :], in1=pKV[0][p0:p0 + 64, :, p0:p0 + 64], op=ALU.add),
                      reads=[stt[1], pKV[1]], writes=[stf[1]])
            for hh in range(2):
                fw.op("act", lambda e, hh=hh: e.activation(out=stb[0][hh * 64:(hh + 1) * 64, :, hh, :], in_=stf[0][hh * 64:(hh + 1) * 64, :, :], func=AF.Copy), reads=[stf[1]], writes=[stb[1]])

        def r_tail(n):
            fw.op("act", lambda e: e.activation(out=sqo[0][:], in_=pO[0][:], func=AF.Square), reads=[pO[1]], writes=[sqo[1]])
            fw.op("dve", lambda e: e.tensor_reduce(out=ssr[0][:], in_=v3(sqo[0][:]), axis=AX.X, op=ALU.add), reads=[sqo[1]], writes=[ssr[1]])
            rstd_from("act", ssr[0][:], ssr[0][:], 1.0 / 64, [ssr[1]], [ssr[1]])
            fw.op("dve", lambda e: e.tensor_tensor(out=v3(t1[0][:]), in0=v3(pO[0][:]), in1=ssr[0][:].unsqueeze(2).broadcast_to([128, 8, 64]), op=ALU.mult),
                  reads=[pO[1], ssr[1]], writes=[t1[1]])
            fw.op("pool", lambda e: e.tensor_tensor(out=ytm[0][:], in0=t1[0][:], in1=gsil2[n % 2][0][:], op=ALU.mult), reads=[t1[1], gsil2[n % 2][1]], writes=[ytm[1]])
            for pr in range(4):
                fw.op("pe", lambda e, pr=pr: e.transpose(out=ptq[0][:, pr, :], in_=ytm[0][:, pr * 128:(pr + 1) * 128], identity=identb[:]), reads=[ytm[1], k_identb], writes=[ptq[1]])
            fw.op("act", lambda e: e.activation(out=R2[:, 2:6, n * 128:(n + 1) * 128], in_=ptq[0][:], func=AF.Copy), reads=[ptq[1]],
                  writes=[k_mx[2 + pr][n // 4] for pr in range(4)])

        r_head(0)
        r_mid(0)
        for n in range(NTB):
            if n + 1 < NTB:
                r_head(n + 1)
            r_tail(n)
            if n + 1 < NTB:
                r_mid(n + 1)
        fw.release(m)

    def phase_CONV(l):
        m = fw.mark()
        PAD = 30
        hg = fw.sb([128, 2, PAD + T], BF16); k_hg = [[fw.trk() for _ in range(9)] for _ in range(2)]
        dw31 = fw.sb([128, 256], F32); k_dw31 = fw.trk()
        fw.op("pool", lambda e: e.memset(dw31[:], 0.0), writes=[k_dw31])
        fw.dma("sp", dw31[0:31, :], cdw_w[l, :, :], writes=[k_dw31], slot=k_dw31)
        dwb16 = fw.sb([128, 256], BF16); k_dwb16 = fw.trk()
        fw.op("dve", lambda e: e.tensor_copy(out=dwb16[:], in_=dw31[:]), reads=[k_dw31], writes=[k_dwb16])
        wcol = fw.sb([128, 2, 31], F32); k_wcol = fw.trk()
        diagw = fw.sb([128, 2, 31, 128], BF16); k_diagw = fw.trk()
        cols = {}
        for name, src in (("ba", cpw_b[l, 0:256]), ("bg", cpw_b[l, 256:512]), ("dwb", cdw_b[l, :]), ("lng", cln_g[l, :]), ("lnb", cln_b[l, :])):
            t = fw.sb([128, 2], F32); k = fw.trk()
            load_cols(t[:], k, src, 2)
            cols[name] = (t, k)
        fw.op("dve", lambda e: e.tensor_scalar(out=cols["bg"][0][:], in0=cols["bg"][0][:], scalar1=-1.0, scalar2=None, op0=ALU.mult), reads=[cols["bg"][1]], writes=[cols["bg"][1]])
        pw = [(fw.ps([128, 512], F32), fw.trk()) for _ in range(2)]
        ptw = (fw.ps([128, 2, 128], BF16), fw.trk())
        for cc in range(2):
            fw.op("pe", lambda e, cc=cc: e.transpose(out=ptw[0][:, cc, :], in_=dwb16[:, cc * 128:(cc + 1) * 128], identity=identb[:]),
                  reads=[k_dwb16, k_identb], writes=[ptw[1]])
        fw.op("dve", lambda e: e.tensor_copy(out=wcol[:], in_=ptw[0][:, :, 0:31]), reads=[ptw[1]], writes=[k_wcol])
        for cc in range(2):
            for j in range(31):
                fw.op("dve", lambda e, cc=cc, j=j: e.tensor_scalar(out=diagw[:, cc, j, :], in0=identf[:], scalar1=wcol[:, cc, j:j + 1], scalar2=None, op0=ALU.mult),
                      reads=[k_identf, k_wcol], writes=[k_diagw])
        for cc in range(2):
            fw.op("pool", lambda e, cc=cc: e.memset(hg[:, cc, 0:PAD], 0.0), writes=[k_hg[cc][0]])
        if CONV_STOP < 2:
            fw.release(m)
            return
        wts = [(fw.sb([128, 8, 128], BF16), fw.trk()) for _ in range(2)]
        pa = [(fw.ps([128, 512], F32), fw.trk()) for _ in range(2)]
        pg = pw
        sg = [(fw.sb([128, 512], F32), fw.trk()) for _ in range(2)]
        it = 0
        for cc in range(2):
            fw.dma("pool", wts[0][0][:], w_in[l, :, 2816 + cc * 128:2816 + (cc + 1) * 128].rearrange("(dc p) c -> p dc c", p=128), writes=[wts[0][1]], slot=wts[0][1])
            fw.dma("pool", wts[1][0][:], w_in[l, :, 3072 + cc * 128:3072 + (cc + 1) * 128].rearrange("(dc p) c -> p dc c", p=128), writes=[wts[1][1]], slot=wts[1][1])
            for tt in range(8):
                a_, g_, s_ = pa[it % 2], pg[it % 2], sg[it % 2]
                it += 1
                for dc in range(8):
                    fw.op("pe", lambda e, dc=dc: e.matmul(a_[0][:], lhsT=wts[0][0][:, dc, :], rhs=R1[:, dc, tt * 512:(tt + 1) * 512], start=(dc == 0), stop=(dc == 7)),
                          reads=[wts[0][1]] + k_hT[tt * 4:(tt + 1) * 4], writes=[a_[1]])
                for dc in range(8):
                    fw.op("pe", lambda e, dc=dc: e.matmul(g_[0][:], lhsT=wts[1][0][:, dc, :], rhs=R1[:, dc, tt * 512:(tt + 1) * 512], start=(dc == 0), stop=(dc == 7)),
                          reads=[wts[1][1]] + k_hT[tt * 4:(tt + 1) * 4], writes=[g_[1]])
                sigmoid_from(s_[0][:], g_[0][:], [g_[1], cols["bg"][1]], [s_[1]], nbias=cols["bg"][0][:, cc:cc + 1])
                fw.op("dve", lambda e: e.scalar_tensor_tensor(out=hg[:, cc, PAD + tt * 512:PAD + (tt + 1) * 512], in0=a_[0][:], scalar=cols["ba"][0][:, cc:cc + 1], in1=s_[0][:], op0=ALU.add, op1=ALU.mult),
                      reads=[a_[1], cols["ba"][1], s_[1]], writes=[k_hg[cc][1 + tt]])
        if CONV_STOP < 3:
            fw.release(m)
            return
        pc = [(fw.ps([128, 512], F32), fw.trk()) for _ in range(2)]
        yc = [(fw.sb([128, 512], F32), fw.trk()) for _ in range(2)]
        ysq = [(fw.sb([128, 512], BF16), fw.trk()) for _ in range(2)]
        ycb = [(fw.sb([128, 512], BF16), fw.trk()) for _ in range(2)]
        mean = (fw.sb([128, 512], F32), fw.trk()); var = (fw.sb([128, 512], F32), fw.trk())
        tt1 = (fw.sb([128, 512], F32), fw.trk()); tt2 = (fw.sb([128, 512], F32), fw.trk())
        for tt in range(8):
            for cc in range(2):
                rd = [k_diagw, k_hg[cc][1 + tt], k_hg[cc][tt]]
                for j in range(31):
                    fw.op("pe", lambda e, cc=cc, j=j: e.matmul(pc[cc][0][:], lhsT=diagw[:, cc, j, :], rhs=hg[:, cc, tt * 512 + j:tt * 512 + j + 512], start=(j == 0), stop=(j == 30)),
                          reads=rd, writes=[pc[cc][1]])
                fw.op("dve", lambda e, cc=cc: e.tensor_scalar(out=yc[cc][0][:], in0=pc[cc][0][:], scalar1=cols["dwb"][0][:, cc:cc + 1], scalar2=None, op0=ALU.add), reads=[pc[cc][1], cols["dwb"][1]], writes=[yc[cc][1]])
                fw.op("act", lambda e, cc=cc: e.activation(out=ysq[cc][0][:], in_=yc[cc][0][:], func=AF.Square), reads=[yc[cc][1]], writes=[ysq[cc][1]])
                fw.op("act", lambda e, cc=cc: e.activation(out=ycb[cc][0][:], in_=yc[cc][0][:], func=AF.Copy), reads=[yc[cc][1]], writes=[ycb[cc][1]])
            if CONV_STOP < 4:
                continue
            for cc in range(2):
                fw.op("pe", lambda e, cc=cc: e.matmul(pw[0][0][:], lhsT=onesf[:], rhs=ycb[cc][0][:], start=(cc == 0), stop=(cc == 1)), reads=[k_onesf, ycb[cc][1]], writes=[pw[0][1]])
            for cc in range(2):
                fw.op("pe", lambda e, cc=cc: e.matmul(pw[1][0][:], lhsT=onesf[:], rhs=ysq[cc][0][:], start=(cc == 0), stop=(cc == 1)), reads=[k_onesf, ysq[cc][1]], writes=[pw[1][1]])
            if CONV_STOP < 5:
                continue
            fw.op("act", lambda e: e.activation(out=mean[0][:], in_=pw[0][0][:], func=AF.Copy), reads=[pw[0][1]], writes=[mean[1]])
            fw.op("dve", lambda e: e.tensor_tensor(out=var[0][:], in0=mean[0][:], in1=mean[0][:], op=ALU.mult), reads=[mean[1]], writes=[var[1]])
            fw.op("dve", lambda e: e.tensor_tensor(out=var[0][:], in0=pw[1][0][:], in1=var[0][:], op=ALU.subtract), reads=[pw[1][1], var[1]], writes=[var[1]])
            rstd_from("act", var[0][:], var[0][:], 1.0, [var[1]], [var[1]])
            for cc in range(2):
                fw.op("dve", lambda e, cc=cc: e.tensor_tensor(out=tt1[0][:], in0=yc[cc][0][:], in1=mean[0][:], op=ALU.subtract), reads=[yc[cc][1], mean[1]], writes=[tt1[1]])
                fw.op("dve", lambda e, cc=cc: e.tensor_tensor(out=tt1[0][:], in0=tt1[0][:], in1=var[0][:], op=ALU.mult), reads=[tt1[1], var[1]], writes=[tt1[1]])
                fw.op("dve", lambda e, cc=cc: e.tensor_scalar(out=tt1[0][:], in0=tt1[0][:], scalar1=cols["lng"][0][:, cc:cc + 1], scalar2=cols["lnb"][0][:, cc:cc + 1], op0=ALU.mult, op1=ALU.add),
                      reads=[tt1[1], cols["lng"][1], cols["lnb"][1]], writes=[tt1[1]])
                sigmoid_from(tt2[0][:], tt1[0][:], [tt1[1]], [tt2[1]])
                fw.op("pool", lambda e, cc=cc: e.tensor_tensor(out=R2[:, 6 + cc, tt * 512:(tt + 1) * 512], in0=tt1[0][:], in1=tt2[0][:], op=ALU.mult),
                      reads=[tt1[1], tt2[1]], writes=[k_mx[6 + cc][tt]])
        fw.release(m)

    def phase_C1(l, xin, k_xin, xmid, k_xmid, k_w1):
        m = fw.mark()
        wo = fw.sb([128, 8, D], BF16); k_wo = [fw.trk() for _ in range(2)]
        for i in range(2):
            fw.dma("pool", wo[:, :, i * 512:(i + 1) * 512], w_out[l, :, i * 512:(i + 1) * 512].rearrange("(cc p) d -> p cc d", p=128), writes=[k_wo[i]], slot=k_wo[i])
        for i in range(8):
            fw.dma("pool", R1[:, :, i * 512:(i + 1) * 512], w_ff1[l, :, i * 512:(i + 1) * 512].rearrange("(dc p) f -> p dc f", p=128), writes=[k_w1[i]], slot=k_w1[i])
        xts = [(fw.sb([128, D], F32), fw.trk()) for _ in range(3)]
        xos = [(fw.sb([128, D], F32), fw.trk()) for _ in range(3)]
        py = [(fw.ps([128, D], F32), fw.trk()) for _ in range(2)]
        for tb in range(NTB):
            xt, k_xt = xts[tb % 3]; xo, k_xo = xos[tb % 3]; p_, k_p = py[tb % 2]
            fw.dma("sp", xt[:], xin[tb * 128:(tb + 1) * 128, :], reads=[k_xin], writes=[k_xt], slot=k_xt)
            for half in range(2):
                for cc in range(8):
                    fw.op("pe", lambda e, cc=cc, half=half: e.matmul(p_[:, half * 512:(half + 1) * 512], lhsT=R2[:, cc, tb * 128:(tb + 1) * 128], rhs=wo[:, cc, half * 512:(half + 1) * 512], start=(cc == 0), stop=(cc == 7)),
                          reads=[k_mx[cc][tb // 4], k_wo[half]], writes=[k_p])
            for half in range(2):
                fw.op("dve", lambda e, half=half: e.tensor_tensor(out=xo[:, half * 512:(half + 1) * 512], in0=p_[:, half * 512:(half + 1) * 512], in1=xt[:, half * 512:(half + 1) * 512], op=ALU.add),
                      reads=[k_p, k_xt], writes=[k_xo])
            fw.dma("sp", xmid[tb * 128:(tb + 1) * 128, :], xo[:], reads=[k_xo], writes=[k_xmid], slot=k_xo)
        fw.release(m)

    def phase_C2(l, xmid, k_xmid, xout, k_xout, k_w1):
        m = fw.mark()
        W1 = R1
        W2 = R2[:].rearrange("p c t -> p (c t)").rearrange("p (fc d) -> p fc d", d=D)
        k_w2 = [fw.trk() for _ in range(8)]
        for i in range(8):
            fw.dma("pool", W2[:, i * 4:(i + 1) * 4, :], w_ff2[l, i * 512:(i + 1) * 512, :].rearrange("(fc p) d -> p fc d", p=128), writes=[k_w2[i]], slot=k_w2[i])
        gt = fw.sb([128, 8], F32); k_gt = fw.trk()
        load_cols(gt[:], k_gt, mlp_g[l, :], 8)
        TT = 256
        xts = [(fw.sb([128, 2, D], F32), [fw.trk(), fw.trk()]) for _ in range(2)]
        xos = [(fw.sb([128, 2, D], F32), [fw.trk(), fw.trk()]) for _ in range(2)]
        res = (fw.sb([128, D], BF16), fw.trk(), fw.sb([128, 2], F32), fw.trk(),
               fw.sb([128, D], BF16), fw.trk(), fw.ps([128, 8, 128], BF16), fw.trk())
        h2T = [(fw.sb([128, 8, TT], BF16), [fw.trk(), fw.trk()]) for _ in range(2)]
        py = [(fw.ps([128, D], F32), fw.trk()) for _ in range(2)]
        pf = [(fw.ps([128, 512], F32), fw.trk()) for _ in range(3)]
        rl = [(fw.sb([128, TT], F32), fw.trk()) for _ in range(3)]
        fb = [(fw.sb([128, TT], BF16), fw.trk()) for _ in range(3)]
        for ti in range(T // TT):
            xt, k_xt = xts[ti % 2]; xo, k_xo = xos[ti % 2]; hT2, k_h2 = h2T[ti % 2]
            for b2 in range(2):
                tb = ti * 2 + b2
                fw.dma("sp", xt[:, b2, :], xmid[tb * 128:(tb + 1) * 128, :], reads=[k_xmid], writes=[k_xt[b2]], slot=k_xt[b2])
                norm_transpose_block(xt[:, b2, :], k_xt[b2], gt, k_gt, res, hT2[:, :, b2 * 128:(b2 + 1) * 128], k_h2[b2])

            def stF(fc):
                p_, k_p = pf[fc % 3]
                for dc in range(8):
                    fw.op("pe", lambda e, dc=dc: e.matmul(p_[:, 0:TT], lhsT=W1[:, dc, fc * 128:(fc + 1) * 128], rhs=hT2[:, dc, :], start=(dc == 0), stop=(dc == 7)),
                          reads=[k_w1[fc // 4]] + k_h2, writes=[k_p])
                r_, k_r = rl[fc % 3]; f_, k_f = fb[fc % 3]
                fw.op("act", lambda e: e.activation(out=r_[:], in_=p_[:, 0:TT], func=AF.Relu), reads=[k_p], writes=[k_r])
                fw.op("pool", lambda e: e.tensor_tensor(out=f_[:], in0=r_[:], in1=r_[:], op=ALU.mult), reads=[k_r], writes=[k_f])

            def stY(fc):
                f_, k_f = fb[fc % 3]
                for b2 in range(2):
                    for half in range(2):
                        fw.op("pe", lambda e, b2=b2, half=half: e.matmul(py[b2][0][:, half * 512:(half + 1) * 512], lhsT=f_[:, b2 * 128:(b2 + 1) * 128], rhs=W2[:, fc, half * 512:(half + 1) * 512], start=(fc == 0), stop=(fc == 31)),
                              reads=[k_f, k_w2[fc // 4]], writes=[py[b2][1]])

            for i in range(32 + 2):
                if i < 32:
                    stF(i)
                if 0 <= i - 2 < 32:
                    stY(i - 2)
            for b2 in range(2):
                tb = ti * 2 + b2
                for half in range(2):
                    fw.op("dve", lambda e, b2=b2, half=half: e.tensor_tensor(out=xo[:, b2, half * 512:(half + 1) * 512], in0=py[b2][0][:, half * 512:(half + 1) * 512], in1=xt[:, b2, half * 512:(half + 1) * 512], op=ALU.add),
                          reads=[py[b2][1], k_xt[b2]], writes=[k_xo[b2]])
                fw.dma("sp", xout[tb * 128:(tb + 1) * 128, :], xo[:, b2, :], reads=[k_xo[b2]], writes=[k_xout], slot=k_xo[b2])
        fw.release(m)

    bufs = {0: (x_in, k_dram["x"], s1, k_dram["s1"], s0, k_dram["s0"]),
            1: (s0, k_dram["s0"], s1, k_dram["s1"], y_out, k_dram["y"])}
    if nlayers == 1:
        bufs[0] = (x_in, k_dram["x"], s1, k_dram["s1"], y_out, k_dram["y"])
    for l in range(nlayers):
        xin, k_xin, xmid, k_xmid, xout, k_xout = bufs[l]
        phase_A(l, xin, k_xin)
        if dbg == "hT":
            break
        for hp in range(2):
            if "sb" not in skip:
                phase_SB(l, hp)
        if dbg in ("sb", "sbproj"):
            break
        if "ret" not in skip:
            phase_RET(l)
        if dbg == "ret":
            break
        if "conv" not in skip:
            phase_CONV(l)
        if dbg == "mixed":
            break
        k_w1 = [fw.trk() for _ in range(8)]
        phase_C1(l, xin, k_xin, xmid, k_xmid, k_w1)
        if dbg == "c1":
            break
        phase_C2(l, xmid, k_xmid, xout, k_xout, k_w1)
    if dbg in ("hT", "sb", "ret", "mixed", "sbproj"):
        src = R1 if dbg == "hT" else R2
        m = fw.mark()
        st = [(fw.sb([128, 2048], F32), fw.trk()) for _ in range(2)]
        i = 0
        for c in dump_chunks:
            for hf in range(2):
                s_, k_s = st[i % 2]; i += 1
                fw.op("dve", lambda e: e.tensor_copy(out=s_[:], in_=src[:, c, hf * 2048:(hf + 1) * 2048]), reads=[], writes=[k_s])
                fw.dma("sp", dbg_out[:, c, hf * 2048:(hf + 1) * 2048], s_[:], reads=[k_s], writes=[k_dram["dbg"]], slot=k_s)
        fw.release(m)
    fw.barrier()
    return fw


def kernel(**inputs):
    global _CONSTS
    if _CONSTS is None:
        _CONSTS = _consts()
    if "nc" not in _NC_CACHE:
        p1 = build()
        _NC_CACHE["nc"] = build(needed=p1.waited)
    fw = _NC_CACHE["nc"]
    x = np.ascontiguousarray(np.asarray(inputs["x"], dtype=np.float32))
    shared = {k: np.ascontiguousarray(np.asarray(v, dtype=np.float32)) for k, v in inputs.items() if k != "x"}
    shared.update(_CONSTS)
    in_maps = []
    for b in range(8):
        mp = dict(shared)
        mp["x"] = x[b]
        in_maps.append(mp)
    res = run_bass_kernel_spmd(fw.nc, in_maps, core_ids=list(range(8)))
    return np.stack([np.asarray(r["y"], dtype=np.float32) for r in res.results], axis=0)
```

```python
import numpy as np
import concourse.bass as bass
import concourse.mybir as mybir
from concourse.bass_utils import run_bass_kernel_spmd

F32 = mybir.dt.float32
BF16 = mybir.dt.bfloat16
AF = mybir.ActivationFunctionType
ALU = mybir.AluOpType
AX = mybir.AxisListType

T = 4096
D = 1024
NTB = 32
DFF = 4096
DEPTH = 2
INC = 3328
EPS = 1e-6
NEG = -30000.0
RET_STOP = 99
CONV_STOP = 99


class Trk:
    __slots__ = ("name", "lw", "rd", "dsem")

    def __init__(self, name):
        self.name = name
        self.lw = {}
        self.rd = {}
        self.dsem = None


class Eng:
    def __init__(self, name, h, sem):
        self.name = name
        self.h = h
        self.sem = sem
        self.count = 0
        self.seen = {}


class FW:
    def __init__(self, needed=None):
        self.needed = None
        if needed is not None:
            self.needed = {k: sorted(v) for k, v in needed.items()}
            self.needed_set = {k: set(v) for k, v in needed.items()}
        self.waited = {}
        self.nc = bass.Bass("TRN2", target_bir_lowering=False)
        nc = self.nc
        self._stack = []
        self.engs = {}
        for name, h in (("pe", nc.tensor), ("act", nc.scalar), ("dve", nc.vector),
                        ("pool", nc.gpsimd), ("sp", nc.sync)):
            sem = self.enter(nc.semaphore("sem_" + name))
            self.engs[name] = Eng(name, h, sem)
        self.dsem_tot = {}
        self.free_dsems = []
        self.free_dsems_sw = []
        self._scope_dsems = []
        self._persist = []
        self.nwait = 0
        self.ninc = 0
        self.ninst = 0
        self._uid = 0

    def enter(self, cm):
        v = cm.__enter__()
        self._stack.append(cm)
        return v

    def uid(self, p="t"):
        self._uid += 1
        return "%s%d" % (p, self._uid)

    def sb(self, shape, dt, name=None):
        return self.enter(self.nc.sbuf_tensor(name or self.uid("sb"), list(shape), dt))

    def ps(self, shape, dt, name=None):
        return self.enter(self.nc.psum_tensor(name or self.uid("ps"), list(shape), dt))

    def trk(self, name=None):
        return Trk(name or self.uid("k"))

    def mark(self):
        self._scope_dsems.append([])
        return len(self._stack)

    def release(self, mark):
        self.barrier()
        while len(self._stack) > mark:
            cm = self._stack.pop()
            cm.__exit__(None, None, None)
        for is_sw, sem in self._scope_dsems.pop():
            (self.free_dsems_sw if is_sw else self.free_dsems).append(sem)

    def _emit_wait(self, eng, s, k, v):
        if k in self.dsem_tot:
            eng.h.wait_ge(s, v)
        else:
            self.waited.setdefault(k, set()).add(v)
            if self.needed is None:
                eng.h.wait_ge(s, v)
            else:
                import bisect
                lst = self.needed[k]
                r = bisect.bisect_left(lst, v)
                assert r < len(lst) and lst[r] == v, (k, v)
                eng.h.wait_ge(s, r + 1)
        self.nwait += 1

    def _deps(self, reads, writes):
        deps = {}

        def add(d):
            for k, (s, v) in d.items():
                if k not in deps or deps[k][1] < v:
                    deps[k] = (s, v)
        for r in reads:
            add(r.lw)
        for w in writes:
            add(w.lw)
            add(w.rd)
        return deps

    def _wait(self, eng, deps):
        for k, (s, v) in deps.items():
            if k in self.dsem_tot:
                v = self.dsem_tot[k][1]
            if eng.name == "pe" and k == eng.sem.name:
                continue
            if eng.seen.get(k, 0) >= v:
                continue
            self._emit_wait(eng, s, k, v)
            eng.seen[k] = v

    def op(self, ename, fn, reads=(), writes=()):
        eng = self.engs[ename]
        self._wait(eng, self._deps(reads, writes))
        inst = fn(eng.h)
        eng.count += 1
        if self.needed is None or eng.count in self.needed_set.get(eng.sem.name, ()):
            inst.then_inc(eng.sem, 1)
            self.ninc += 1
        ev = (eng.sem, eng.count)
        k = eng.sem.name
        for w in writes:
            w.lw = {k: ev}
            w.rd = {}
        for r in reads:
            r.rd[k] = ev
        self.ninst += 1
        return inst

    def dma(self, qname, out, in_, reads=(), writes=(), slot=None, **kw):
        eng = self.engs[qname]
        self._wait(eng, self._deps(reads, writes))
        if slot.dsem is None:
            pool_ = self.free_dsems_sw if qname == "pool" else self.free_dsems
            if pool_:
                slot.dsem = pool_.pop()
            else:
                cm = self.nc.semaphore(self.uid("dsem"))
                slot.dsem = cm.__enter__()
                self._persist.append(cm)
                self.dsem_tot[slot.dsem.name] = (slot.dsem, 0)
            if self._scope_dsems:
                self._scope_dsems[-1].append((qname == "pool", slot.dsem))
        inst = eng.h.dma_start(out=out, in_=in_, **kw)
        tot = self.dsem_tot[slot.dsem.name][1] + 16
        self.dsem_tot[slot.dsem.name] = (slot.dsem, tot)
        inst.then_inc(slot.dsem, 16)
        ev = (slot.dsem, tot)
        k = slot.dsem.name
        for w in writes:
            w.lw = {k: ev}
            w.rd = {}
        for r in reads:
            r.rd[k] = ev
        self.ninst += 1
        return inst

    def barrier(self):
        for eng in self.engs.values():
            for e in self.engs.values():
                if e is eng or e.count == 0:
                    continue
                if eng.seen.get(e.sem.name, 0) < e.count:
                    self._emit_wait(eng, e.sem, e.sem.name, e.count)
                    eng.seen[e.sem.name] = e.count
            for k, (s, tot) in self.dsem_tot.items():
                if tot and eng.seen.get(k, 0) < tot:
                    self._emit_wait(eng, s, k, tot)
                    eng.seen[k] = tot


def _consts():
    c = {}
    i = np.arange(128)
    c["c_ident"] = np.eye(128, dtype=np.float32)
    c["c_blk"] = (i[:, None] // 64 == i[None, :] // 64).astype(np.float32)
    c["c_negtri"] = -(i[:, None] >= i[None, :]).astype(np.float32)
    c["c_negones"] = -np.ones((128, 128), np.float32)
    c["c_maskneg"] = np.where(i[:, None] >= i[None, :], NEG, 0.0).astype(np.float32)
    c["c_ones"] = np.full((128, 128), 1.0 / 256.0, np.float32)
    inv = (1.0 / (10000.0 ** np.linspace(0.0, 1.0, 32, dtype=np.float32))).astype(np.float32)
    pos = np.arange(T, dtype=np.float32)
    ang = (pos[:, None] * inv[None, :]).astype(np.float32)
    cos = np.cos(ang).astype(np.float32)
    sin = np.sin(ang).astype(np.float32)
    cos2 = np.repeat(cos, 2, axis=1)
    s2 = np.stack([-sin, sin], axis=-1).reshape(T, 64)
    rot = np.concatenate([cos2, s2, cos2 * 0.125, s2 * 0.125], axis=1).astype(np.float32)
    c["c_rot"] = rot.reshape(NTB, 128, 256)
    h = np.arange(8, dtype=np.float32)
    log_g = np.log(1.0 - np.exp2(-5.0 - h)).astype(np.float32)
    j = np.arange(128, dtype=np.float32)
    rel = j[None, :] - j[:, None]
    dt_ = np.where(rel[None] >= 0, np.exp(log_g[:, None, None] * np.maximum(rel[None], 0.0)), 0.0)
    c["c_dt"] = np.ascontiguousarray(dt_.transpose(1, 0, 2)).astype(np.float32)
    qd = np.exp(log_g[:, None] * (j + 1.0)[None, :]).astype(np.float32)
    qdec = np.zeros((128, 4, 128), np.float32)
    g128 = np.zeros((128, 4, 64), np.float32)
    gC = np.exp(log_g * 128.0).astype(np.float32)
    for p in range(128):
        for pr in range(4):
            hh = pr * 2 + p // 64
            qdec[p, pr, :] = qd[hh]
            g128[p, pr, :] = gC[hh]
    c["c_qdec"] = qdec
    c["c_g128"] = g128
    kd = np.exp(log_g[:, None] * (127.0 - j)[None, :]).astype(np.float32)
    c["c_kdec"] = np.ascontiguousarray(np.repeat(kd.T[:, :, None], 64, axis=2)).astype(np.float32)
    return c


_CONSTS = None
_NC_CACHE = {}


def build(nlayers=DEPTH, dbg=None, skip=(), dump_chunks=range(8), needed=None):
    fw = FW(needed)
    nc = fw.nc

    def din(name, shape):
        shape = list(shape)
        if shape[0] == DEPTH and not name.startswith("c_"):
            shape[0] = nlayers
        return nc.dram_tensor(name, shape, F32, kind="ExternalInput").ap()

    x_in = din("x", [T, D])
    mix_g = din("mix_norm_g", [DEPTH, D])
    w_in = din("w_in", [DEPTH, D, INC])
    sbq_g = din("sb_q_norm_g", [DEPTH, 64])
    sbk_g = din("sb_k_norm_g", [DEPTH, 64])
    ret_g = din("ret_norm_g", [DEPTH, 512])
    cpw_b = din("conv_pw_b", [DEPTH, 512])
    cdw_w = din("conv_dw_w", [DEPTH, 31, 256])
    cdw_b = din("conv_dw_b", [DEPTH, 256])
    cln_g = din("conv_ln_g", [DEPTH, 256])
    cln_b = din("conv_ln_b", [DEPTH, 256])
    w_out = din("w_out", [DEPTH, D, D])
    mlp_g = din("mlp_norm_g", [DEPTH, D])
    w_ff1 = din("w_ff1", [DEPTH, D, DFF])
    w_ff2 = din("w_ff2", [DEPTH, DFF, D])
    c_ident = din("c_ident", [128, 128])
    c_blk = din("c_blk", [128, 128])
    c_negtri = din("c_negtri", [128, 128])
    c_negones = din("c_negones", [128, 128])
    c_maskneg = din("c_maskneg", [128, 128])
    c_ones = din("c_ones", [128, 128])
    c_rot = din("c_rot", [NTB, 128, 256])
    c_dt = din("c_dt", [128, 8, 128])
    c_qdec = din("c_qdec", [128, 4, 128])
    c_g128 = din("c_g128", [128, 4, 64])
    c_kdec = din("c_kdec", [128, 8, 64])
    y_out = nc.dram_tensor("y", [T, D], F32, kind="ExternalOutput").ap()
    s0 = nc.dram_tensor("scr0", [T, D], F32, kind="Internal").ap()
    s1 = nc.dram_tensor("scr1", [T, D], F32, kind="Internal").ap()
    dbg_out = None
    if dbg is not None:
        dbg_out = nc.dram_tensor("dbg", [128, 8, T], F32, kind="ExternalOutput").ap()

    k_dram = {"x": fw.trk("dx"), "s0": fw.trk("ds0"), "s1": fw.trk("ds1"), "y": fw.trk("dy"), "dbg": fw.trk("ddbg")}

    R1 = fw.sb([128, 8, T], BF16, "R1")
    R2 = fw.sb([128, 8, T], BF16, "R2")
    k_hT = [fw.trk("hT%d" % i) for i in range(NTB)]
    k_mx = [[fw.trk("mx%d_%d" % (c, i)) for i in range(8)] for c in range(8)]
    identb = fw.sb([128, 128], BF16, "identb"); k_identb = fw.trk("identb")
    identf = fw.sb([128, 128], F32, "identf"); k_identf = fw.trk("identf")
    blkb = fw.sb([128, 128], BF16, "blkb"); k_blkb = fw.trk("blkb")
    negtri = fw.sb([128, 128], BF16, "negtri"); k_negtri = fw.trk("negtri")
    negones = fw.sb([128, 128], BF16, "negones"); k_negones = fw.trk("negones")
    maskneg = fw.sb([128, 128], BF16, "maskneg"); k_maskneg = fw.trk("maskneg")
    onesf = fw.sb([128, 128], BF16, "onesf"); k_onesf = fw.trk("onesf")
    zerob = fw.sb([128, 128], BF16, "zerob"); k_zerob = fw.trk("zerob")
    fw.op("pool", lambda e: e.memset(zerob[:], 0.0), writes=[k_zerob])
    fw.dma("pool", identb[:], c_ident[:, :], writes=[k_identb], slot=k_identb)
    fw.dma("sp", identf[:], c_ident[:, :], writes=[k_identf], slot=k_identf)
    fw.dma("pool", blkb[:], c_blk[:, :], writes=[k_blkb], slot=k_blkb)
    fw.dma("pool", negtri[:], c_negtri[:, :], writes=[k_negtri], slot=k_negtri)
    fw.dma("pool", negones[:], c_negones[:, :], writes=[k_negones], slot=k_negones)
    fw.dma("pool", maskneg[:], c_maskneg[:, :], writes=[k_maskneg], slot=k_maskneg)
    fw.dma("pool", onesf[:], c_ones[:, :], writes=[k_onesf], slot=k_onesf)

    def load_cols(dst, k_dst, src_row_ap, n):
        with nc.allow_non_contiguous_dma(reason="tiny param vector"):
            fw.dma("sp", dst, src_row_ap.rearrange("(c p) -> p c", p=128), writes=[k_dst], slot=k_dst)

    def rstd_from(ename_unused, out_ap, in_ap, scale, reads, writes):
        fw.op("act", lambda e: e.activation(out=out_ap, in_=in_ap, func=AF.Ln, scale=scale, bias=EPS),
              reads=reads, writes=writes)
        fw.op("act", lambda e: e.activation(out=out_ap, in_=out_ap, func=AF.Exp, scale=-0.5),
              reads=writes, writes=writes)

    def sigmoid_from(out_ap, in_ap, reads, writes, nbias=None):
        if nbias is None:
            fw.op("act", lambda e: e.activation(out=out_ap, in_=in_ap, func=AF.Exp, scale=-1.0), reads=reads, writes=writes)
        else:
            fw.op("act", lambda e: e.activation(out=out_ap, in_=in_ap, func=AF.Exp, scale=-1.0, bias=nbias), reads=reads, writes=writes)
        fw.op("act", lambda e: e.activation(out=out_ap, in_=out_ap, func=AF.Ln, bias=1.0), reads=writes, writes=writes)
        fw.op("act", lambda e: e.activation(out=out_ap, in_=out_ap, func=AF.Exp, scale=-1.0), reads=writes, writes=writes)

    def norm_transpose_block(xt, k_xt, gt, k_gt, res, dst_ap, k_dst):
        junk, k_junk, st, k_st, hb, k_hb, pt, k_pt = res
        fw.op("act", lambda e: e.activation(out=junk[:], in_=xt, func=AF.Square, accum_out=st[:, 0:1]),
              reads=[k_xt], writes=[k_junk, k_st])
        rstd_from("act", st[:, 1:2], st[:, 0:1], 1.0 / D, [k_st], [k_st])
        fw.op("dve", lambda e: e.tensor_scalar(out=hb[:], in0=xt, scalar1=st[:, 1:2], scalar2=None, op0=ALU.mult),
              reads=[k_xt, k_st], writes=[k_hb])
        for dc in range(8):
            fw.op("pe", lambda e, dc=dc: e.transpose(out=pt[:, dc, :], in_=hb[:, dc * 128:(dc + 1) * 128], identity=identb[:]),
                  reads=[k_hb, k_identb], writes=[k_pt])
        fw.op("dve", lambda e: e.tensor_tensor(out=dst_ap, in0=pt[:], in1=gt[:].unsqueeze(2).broadcast_to([128, 8, 128]), op=ALU.mult),
              reads=[k_pt, k_gt], writes=[k_dst])

    def phase_A(l, xin, k_xin):
        m = fw.mark()
        gt = fw.sb([128, 8], F32); k_gt = fw.trk()
        load_cols(gt[:], k_gt, mix_g[l, :], 8)
        nb = 3
        xts = [(fw.sb([128, D], F32), fw.trk()) for _ in range(nb)]
        ress = []
        for _ in range(2):
            ress.append((fw.sb([128, D], BF16), fw.trk(), fw.sb([128, 2], F32), fw.trk(),
                         fw.sb([128, D], BF16), fw.trk(), fw.ps([128, 8, 128], BF16), fw.trk()))
        for tb in range(NTB):
            xt, k_xt = xts[tb % nb]
            fw.dma("sp", xt[:], xin[tb * 128:(tb + 1) * 128, :], reads=[k_xin], writes=[k_xt], slot=k_xt)
            norm_transpose_block(xt[:], k_xt, gt, k_gt, ress[tb % 2], R1[:, :, tb * 128:(tb + 1) * 128], k_hT[tb])
        fw.release(m)

    def phase_SB(l, hp):
        m = fw.mark()
        qT = fw.sb([128, T], BF16); kT = fw.sb([128, 2, T], BF16); vv = fw.sb([128, NTB, 128], BF16)
        k_kT0 = fw.trk()
        fw.op("pool", lambda e: e.memset(kT[:], 0.0), writes=[k_kT0])
        k_qT = [fw.trk() for _ in range(8)]; k_kT = [fw.trk() for _ in range(8)]; k_v = [fw.trk() for _ in range(8)]
        gq = fw.sb([128, 1], F32); k_gq = fw.trk(); gk = fw.sb([128, 1], F32); k_gk = fw.trk()
        with nc.allow_non_contiguous_dma(reason="tiny param vector"):
            for half in range(2):
                fw.dma("sp", gq[half * 64:(half + 1) * 64, :], sbq_g[l, :].rearrange("(p o) -> p o", o=1), writes=[k_gq], slot=k_gq)
                fw.dma("sp", gk[half * 64:(half + 1) * 64, :], sbk_g[l, :].rearrange("(p o) -> p o", o=1), writes=[k_gk], slot=k_gk)
        fw.op("dve", lambda e: e.tensor_scalar(out=gq[:], in0=gq[:], scalar1=0.125, scalar2=None, op0=ALU.mult), reads=[k_gq], writes=[k_gq])
        wts = [(fw.sb([128, 8, 128], BF16), fw.trk()) for _ in range(2)]
        pbank = [(fw.ps([128, 512], F32), fw.trk()) for _ in range(2)]
        pss = [(fw.ps([128, 512], F32), fw.trk()) for _ in range(2)]
        sqb = [(fw.sb([128, 512], BF16), fw.trk()) for _ in range(2)]
        rsd = [(fw.sb([128, 512], F32), fw.trk()) for _ in range(2)]
        it = 0
        for which in range(2):
            col0 = (0 if which == 0 else 256) + hp * 128
            wt, k_wt = wts[which]
            fw.dma("pool", wt[:], w_in[l, :, col0:col0 + 128].rearrange("(dc p) c -> p dc c", p=128), writes=[k_wt], slot=k_wt)
            dst = qT if which == 0 else kT
            kd = k_qT if which == 0 else k_kT
            gcol, k_gcol = (gq, k_gq) if which == 0 else (gk, k_gk)
            for tt in range(8):
                pq, k_pq = pbank[it % 2]; ps2, k_ps2 = pss[it % 2]; sq, k_sq = sqb[it % 2]; rs, k_rs = rsd[it % 2]
                it += 1
                for dc in range(8):
                    fw.op("pe", lambda e, dc=dc: e.matmul(pq[:], lhsT=wt[:, dc, :], rhs=R1[:, dc, tt * 512:(tt + 1) * 512], start=(dc == 0), stop=(dc == 7)),
                          reads=[k_wt] + k_hT[tt * 4:(tt + 1) * 4], writes=[k_pq])
                fw.op("act", lambda e: e.activation(out=sq[:], in_=pq[:], func=AF.Square), reads=[k_pq], writes=[k_sq])
                fw.op("pe", lambda e: e.matmul(ps2[:], lhsT=blkb[:], rhs=sq[:], start=True, stop=True), reads=[k_blkb, k_sq], writes=[k_ps2])
                rstd_from("act", rs[:], ps2[:], 1.0 / 64, [k_ps2], [k_rs])
                if which == 0:
                    fw.op("dve", lambda e: e.scalar_tensor_tensor(out=dst[:, tt * 512:(tt + 1) * 512], in0=pq[:], scalar=gcol[:, 0:1], in1=rs[:], op0=ALU.mult, op1=ALU.mult),
                          reads=[k_pq, k_gcol, k_rs], writes=[kd[tt]])
                else:
                    for hh_ in range(2):
                        fw.op("dve", lambda e, hh_=hh_: e.scalar_tensor_tensor(out=kT[hh_ * 64:(hh_ + 1) * 64, hh_, tt * 512:(tt + 1) * 512], in0=pq[hh_ * 64:(hh_ + 1) * 64, :],
                                                                              scalar=gcol[hh_ * 64:(hh_ + 1) * 64, 0:1], in1=rs[hh_ * 64:(hh_ + 1) * 64, :], op0=ALU.mult, op1=ALU.mult),
                              reads=[k_pq, k_gcol, k_rs, k_kT0], writes=[kd[tt]])
        wt, k_wt = wts[0]
        col0 = 512 + hp * 128
        fw.dma("pool", wt[:], w_in[l, :, col0:col0 + 128].rearrange("(dc p) c -> p dc c", p=128), writes=[k_wt], slot=k_wt)
        for tb in range(NTB):
            pq, k_pq = pbank[tb % 2]
            for dc in range(8):
                fw.op("pe", lambda e, dc=dc: e.matmul(pq[:, 0:128], lhsT=R1[:, dc, tb * 128:(tb + 1) * 128], rhs=wt[:, dc, :], start=(dc == 0), stop=(dc == 7)),
                      reads=[k_wt, k_hT[tb]], writes=[k_pq])
            fw.op("act", lambda e: e.activation(out=vv[:, tb, :], in_=pq[:, 0:128], func=AF.Copy), reads=[k_pq], writes=[k_v[tb // 4]])

        if dbg == 'sbproj':
            fw.release(m)
            return
        pz = [(fw.ps([128, 512], F32), fw.trk()) for _ in range(2)] + [pss[0]]
        plw = [(fw.ps([128, 512], F32), fw.trk()) for _ in range(2)] + [pss[1]]
        Eb = [(fw.sb([128, 512], F32), fw.trk()) for _ in range(3)]
        Lb = [(fw.sb([128, 512], BF16), fw.trk()) for _ in range(3)]
        Rb = fw.sb([128, 3, 512], BF16); k_R = [fw.trk() for _ in range(3)]
        wb = [(fw.sb([128, 512], BF16), fw.trk()) for _ in range(2)]
        for hh in range(2):
            p0 = hh * 64
            for qt in range(8):
                po, k_po = pbank[(hh * 8 + qt) % 2]
                fw.op("pool", lambda e: e.memset(Rb[:], 0.0), writes=k_R)
                fw.op("pe", lambda e: e.matmul(po[:, :], lhsT=zerob[:], rhs=qT[:, qt * 512:(qt + 1) * 512], start=True, stop=False),
                      reads=[k_zerob, k_qT[qt]], writes=[k_po])
                blocks = list(range(4 * qt + 3, -1, -1))
                nblk = len(blocks)
                q0 = qt * 512

                def zmm(dstp, b, last_stop):
                    kb = blocks[b]
                    kl = kb - 4 * qt
                    ksl = kT[:, hh, kb * 128:(kb + 1) * 128]
                    rk = [k_kT[kb // 4], k_qT[qt]]
                    c0 = kl * 128 if kl >= 0 else 0
                    fw.op("pe", lambda e: e.matmul(dstp[0][:, c0:512], lhsT=ksl, rhs=qT[:, q0 + c0:q0 + 512], start=True, stop=(last_stop and kl < 0)),
                          reads=rk, writes=[dstp[1]])
                    if kl >= 0:
                        fw.op("pe", lambda e: e.matmul(dstp[0][:, c0:c0 + 128], lhsT=identb[:], rhs=maskneg[:], start=False, stop=last_stop),
                              reads=[k_identb, k_maskneg], writes=[dstp[1]])
                    return c0

                def stageA(b):
                    c0 = zmm(pz[b % 3], b, True)
                    L, k_L = Lb[b % 3]
                    fw.op("act", lambda e: e.activation(out=pz[b % 3][0][:, c0:], in_=pz[b % 3][0][:, c0:], func=AF.Exp), reads=[pz[b % 3][1]], writes=[pz[b % 3][1]])
                    fw.op("act", lambda e: e.activation(out=L[:, c0:], in_=pz[b % 3][0][:, c0:], func=AF.Ln, bias=1.0), reads=[pz[b % 3][1]], writes=[k_L])
                    if b + 1 < nblk:
                        fw.op("pool", lambda e: e.tensor_tensor(out=Rb[:, (b + 1) % 3, c0:], in0=Rb[:, b % 3, c0:], in1=L[:, c0:], op=ALU.add),
                              reads=[k_R[b % 3], k_L], writes=[k_R[(b + 1) % 3]])

                def stageB(b):
                    c0 = zmm(plw[b % 3], b, False)
                    L, k_L = Lb[b % 3]
                    fw.op("pe", lambda e: e.matmul(plw[b % 3][0][:, c0:], lhsT=negtri[:], rhs=L[:, c0:], start=False, stop=(b == 0)),
                          reads=[k_negtri, k_L], writes=[plw[b % 3][1]])
                    if b > 0:
                        fw.op("pe", lambda e: e.matmul(plw[b % 3][0][:, c0:], lhsT=negones[:], rhs=Rb[:, b % 3, c0:], start=False, stop=True),
                              reads=[k_negones, k_R[b % 3]], writes=[plw[b % 3][1]])
                    w, k_w = wb[b % 2]
                    fw.op("act", lambda e: e.activation(out=w[:, c0:], in_=plw[b % 3][0][:, c0:], func=AF.Exp), reads=[plw[b % 3][1]], writes=[k_w])

                def stageC(b):
                    kb = blocks[b]
                    kl = kb - 4 * qt
                    w, k_w = wb[b % 2]
                    last = (b == nblk - 1)
                    vsl = vv[:, kb, :]
                    rd = [k_v[kb // 4], k_w]
                    c0 = kl * 128 if kl >= 0 else 0
                    fw.op("pe", lambda e: e.matmul(po[:, c0:512], lhsT=vsl, rhs=w[:, c0:512], start=False, stop=last), reads=rd, writes=[k_po])
                    if last:
                        fw.op("dve", lambda e: e.tensor_copy(out=R2[p0:p0 + 64, hp, q0:q0 + 512], in_=po[p0:p0 + 64, :]), reads=[k_po], writes=[k_mx[hp][qt]])

                for i in range(nblk + 2):
                    if i < nblk:
                        stageA(i)
                    if 0 <= i - 1 < nblk:
                        stageB(i - 1)
                    if 0 <= i - 2 < nblk:
                        stageC(i - 2)
        fw.release(m)

    def phase_RET(l):
        m = fw.mark()
        wr = fw.sb([128, 8, 2048], BF16); k_wr = [fw.trk() for _ in range(4)]
        for i in range(4):
            fw.dma("pool", wr[:, :, i * 512:(i + 1) * 512], w_in[l, :, 768 + i * 512:768 + (i + 1) * 512].rearrange("(dc p) c -> p dc c", p=128),
                   writes=[k_wr[i]], slot=k_wr[i])
        dtab = fw.sb([128, 8, 128], F32); k_dtab = fw.trk()
        qdec = fw.sb([128, 4, 128], F32); k_qdec = fw.trk()
        kdec = fw.sb([128, 8, 64], F32); k_kdec = fw.trk()
        g128 = fw.sb([128, 4, 64], F32); k_g128 = fw.trk()
        rgb = fw.sb([128, 512], F32); k_rgb = fw.trk()
        fw.dma("sp", dtab[:], c_dt[:, :, :], writes=[k_dtab], slot=k_dtab)
        fw.dma("sp", qdec[:], c_qdec[:, :, :], writes=[k_qdec], slot=k_qdec)
        fw.dma("sp", kdec[:], c_kdec[:, :, :], writes=[k_kdec], slot=k_kdec)
        fw.dma("sp", g128[:], c_g128[:, :, :], writes=[k_g128], slot=k_g128)
        fw.dma("sp", rgb[:], ret_g[l, :].partition_broadcast(128), writes=[k_rgb], slot=k_rgb)
        rot = [(fw.sb([128, 256], F32), fw.trk()) for _ in range(2)]
        pA = (fw.ps([128, 512], F32), fw.trk()); pB = (fw.ps([128, 512], F32), fw.trk())
        ptq = (fw.ps([128, 4, 128], BF16), fw.trk()); ptk = (fw.ps([128, 4, 128], BF16), fw.trk())
        pS = [(fw.ps([128, 4, 128], F32), fw.trk()) for _ in range(2)]
        pO = (fw.ps([128, 512], F32), fw.trk()); pKV = (fw.ps([128, 4, 128], F32), fw.trk())
        tA = [(fw.sb([128, 512], F32), fw.trk()) for _ in range(2)]
        tB = [(fw.sb([128, 512], F32), fw.trk()) for _ in range(2)]
        rq = (fw.sb([128, 512], BF16), fw.trk()); rk = (fw.sb([128, 512], BF16), fw.trk())
        rkd = (fw.sb([128, 512], BF16), fw.trk()); vsb = (fw.sb([128, 512], BF16), fw.trk())
        gsil = (fw.sb([128, 512], F32), fw.trk())
        qTs = (fw.sb([128, 4, 128], BF16), fw.trk()); qdT = (fw.sb([128, 4, 128], BF16), fw.trk())
        kTz = (fw.sb([128, 4, 2, 128], BF16), fw.trk())
        fw.op("pool", lambda e: e.memset(kTz[0][:], 0.0), writes=[kTz[1]])
        AT = (fw.sb([128, 8, 128], BF16), fw.trk())
        sqo = (fw.sb([128, 512], F32), fw.trk()); ssr = (fw.sb([128, 8], F32), fw.trk())
        t1 = (fw.sb([128, 512], F32), fw.trk()); ytm = (fw.sb([128, 512], BF16), fw.trk())
        stf = (fw.sb([128, 4, 64], F32), fw.trk()); stb = (fw.sb([128, 4, 2, 64], BF16), fw.trk()); stt = (fw.sb([128, 4, 64], F32), fw.trk())
        fw.op("pool", lambda e: e.memset(stf[0][:], 0.0), writes=[stf[1]])
        fw.op("pool", lambda e: e.memset(stb[0][:], 0.0), writes=[stb[1]])

        def proj(pdst, wi, tb):
            for dc in range(8):
                fw.op("pe", lambda e, dc=dc: e.matmul(pdst[0][:], lhsT=R1[:, dc, tb * 128:(tb + 1) * 128], rhs=wr[:, dc, wi * 512:(wi + 1) * 512], start=(dc == 0), stop=(dc == 7)),
                      reads=[k_hT[tb], k_wr[wi]], writes=[pdst[1]])

        def v3(ap):
            return ap.rearrange("p (h e) -> p h e", h=8)

        def rotary(psrc, off, ta, tb_, dst, rt, k_rt):
            cos2 = rt[:, off:off + 64].unsqueeze(1).broadcast_to([128, 8, 64])
            s2 = rt[:, off + 64:off + 128]
            s2e = s2.rearrange("p (i two) -> p i two", two=2)[:, :, 0].unsqueeze(1).broadcast_to([128, 8, 32])
            s2o = s2.rearrange("p (i two) -> p i two", two=2)[:, :, 1].unsqueeze(1).broadcast_to([128, 8, 32])
            ps4 = psrc[0][:].rearrange("p (h i two) -> p h i two", h=8, two=2)
            tb4 = tb_[0][:].rearrange("p (h i two) -> p h i two", h=8, two=2)
            fw.op("dve", lambda e: e.tensor_tensor(out=v3(ta[0][:]), in0=v3(psrc[0][:]), in1=cos2, op=ALU.mult), reads=[psrc[1], k_rt], writes=[ta[1]])
            fw.op("dve", lambda e: e.tensor_tensor(out=tb4[:, :, :, 0], in0=ps4[:, :, :, 1], in1=s2e, op=ALU.mult), reads=[psrc[1], k_rt], writes=[tb_[1]])
            fw.op("dve", lambda e: e.tensor_tensor(out=tb4[:, :, :, 1], in0=ps4[:, :, :, 0], in1=s2o, op=ALU.mult), reads=[psrc[1], k_rt], writes=[tb_[1]])
            fw.op("pool", lambda e: e.tensor_tensor(out=dst[0][:], in0=ta[0][:], in1=tb_[0][:], op=ALU.add), reads=[ta[1], tb_[1]], writes=[dst[1]])

        gsil2 = [gsil, (fw.sb([128, 512], F32), fw.trk())]

        def r_head(n):
            rt, k_rt = rot[n % 2]
            fw.dma("sp", rt[:], c_rot[n, :, :], writes=[k_rt], slot=k_rt)
            proj(pA, 0, n)
            rotary(pA, 0, tA[0], tB[0], rq, rt, k_rt)
            proj(pB, 1, n)
            rotary(pB, 128, tA[1], tB[1], rk, rt, k_rt)
            fw.op("pool", lambda e: e.tensor_tensor(out=v3(rkd[0][:]), in0=v3(rk[0][:]), in1=kdec[:], op=ALU.mult), reads=[rk[1], k_kdec], writes=[rkd[1]])
            proj(pA, 2, n)
            fw.op("act", lambda e: e.activation(out=vsb[0][:], in_=pA[0][:], func=AF.Copy), reads=[pA[1]], writes=[vsb[1]])
            proj(pB, 3, n)
            sigmoid_from(gsil2[n % 2][0][:], pB[0][:], [pB[1]], [gsil2[n % 2][1]])
            fw.op("dve", lambda e: e.tensor_tensor(out=gsil2[n % 2][0][:], in0=pB[0][:], in1=gsil2[n % 2][0][:], op=ALU.mult), reads=[pB[1], gsil2[n % 2][1]], writes=[gsil2[n % 2][1]])
            fw.op("pool", lambda e: e.tensor_tensor(out=gsil2[n % 2][0][:], in0=gsil2[n % 2][0][:], in1=rgb[:], op=ALU.mult), reads=[gsil2[n % 2][1], k_rgb], writes=[gsil2[n % 2][1]])
            for pr in range(4):
                fw.op("pe", lambda e, pr=pr: e.transpose(out=ptq[0][:, pr, :], in_=rq[0][:, pr * 128:(pr + 1) * 128], identity=identb[:]), reads=[rq[1], k_identb], writes=[ptq[1]])
            for pr in range(4):
                fw.op("pe", lambda e, pr=pr: e.transpose(out=ptk[0][:, pr, :], in_=rk[0][:, pr * 128:(pr + 1) * 128], identity=identb[:]), reads=[rk[1], k_identb], writes=[ptk[1]])
            fw.op("act", lambda e: e.activation(out=qTs[0][:], in_=ptq[0][:], func=AF.Copy), reads=[ptq[1]], writes=[qTs[1]])
            fw.op("dve", lambda e: e.tensor_tensor(out=qdT[0][:], in0=ptq[0][:], in1=qdec[:], op=ALU.mult), reads=[ptq[1], k_qdec], writes=[qdT[1]])
            for hh in range(2):
                fw.op("act", lambda e, hh=hh: e.activation(out=kTz[0][hh * 64:(hh + 1) * 64, :, hh, :], in_=ptk[0][hh * 64:(hh + 1) * 64, :, :], func=AF.Copy), reads=[ptk[1]], writes=[kTz[1]])

        def r_mid(n):
            for h in range(8):
                pr, p0 = h // 2, (h % 2) * 64
                fw.op("pe", lambda e, h=h, pr=pr: e.matmul(pS[h // 4][0][:, h % 4, :], lhsT=kTz[0][:, pr, h % 2, :], rhs=qTs[0][:, pr, :], start=True, stop=True),
                      reads=[kTz[1], qTs[1]], writes=[pS[h // 4][1]])
            for half in range(2):
                fw.op("dve", lambda e, half=half: e.tensor_tensor(out=AT[0][:, half * 4:(half + 1) * 4, :], in0=pS[half][0][:], in1=dtab[:, half * 4:(half + 1) * 4, :], op=ALU.mult),
                      reads=[pS[half][1], k_dtab], writes=[AT[1]])
            for h in range(8):
                pr, p0 = h // 2, (h % 2) * 64
                fw.op("pe", lambda e, h=h: e.matmul(pO[0][:, h * 64:(h + 1) * 64], lhsT=AT[0][:, h, :], rhs=vsb[0][:, h * 64:(h + 1) * 64], start=True, stop=False),
                      reads=[AT[1], vsb[1]], writes=[pO[1]])
                fw.op("pe", lambda e, h=h, pr=pr: e.matmul(pO[0][:, h * 64:(h + 1) * 64], lhsT=qdT[0][:, pr, :], rhs=stb[0][:, pr, h % 2, :], start=False, stop=True),
                      reads=[qdT[1], stb[1]], writes=[pO[1]])
            for pr in range(4):
                fw.op("pe", lambda e, pr=pr: e.matmul(pKV[0][:, pr, :], lhsT=rkd[0][:, pr * 128:(pr + 1) * 128], rhs=vsb[0][:, pr * 128:(pr + 1) * 128], start=True, stop=True),
                      reads=[rkd[1], vsb[1]], writes=[pKV[1]])
            fw.op("pool", lambda e: e.tensor_tensor(out=stt[0][:], in0=stf[0][:], in1=g128[:], op=ALU.mult), reads=[stf[1], k_g128], writes=[stt[1]])
            for half in range(2):
                p0 = half * 64
                fw.op("dve", lambda e, p0=p0: e.tensor_tensor(out=stf[0][p0:p0 + 64, :, :], in0=stt[0][p0:p0 + 64, :, :], in1=pKV[0][p0:p0 + 64, :, p0:p0 + 64], op=ALU.add),
                      reads=[stt[1], pKV[1]], writes=[stf[1]])
            for hh in range(2):
                fw.op("act", lambda e, hh=hh: e.activation(out=stb[0][hh * 64:(hh + 1) * 64, :, hh, :], in_=stf[0][hh * 64:(hh + 1) * 64, :, :], func=AF.Copy), reads=[stf[1]], writes=[stb[1]])

        def r_tail(n):
            fw.op("act", lambda e: e.activation(out=sqo[0][:], in_=pO[0][:], func=AF.Square), reads=[pO[1]], writes=[sqo[1]])
            fw.op("dve", lambda e: e.tensor_reduce(out=ssr[0][:], in_=v3(sqo[0][:]), axis=AX.X, op=ALU.add), reads=[sqo[1]], writes=[ssr[1]])
            rstd_from("act", ssr[0][:], ssr[0][:], 1.0 / 64, [ssr[1]], [ssr[1]])
            fw.op("dve", lambda e: e.tensor_tensor(out=v3(t1[0][:]), in0=v3(pO[0][:]), in1=ssr[0][:].unsqueeze(2).broadcast_to([128, 8, 64]), op=ALU.mult),
                  reads=[pO[1], ssr[1]], writes=[t1[1]])
            fw.op("pool", lambda e: e.tensor_tensor(out=ytm[0][:], in0=t1[0][:], in1=gsil2[n % 2][0][:], op=ALU.mult), reads=[t1[1], gsil2[n % 2][1]], writes=[ytm[1]])
            for pr in range(4):
                fw.op("pe", lambda e, pr=pr: e.transpose(out=ptq[0][:, pr, :], in_=ytm[0][:, pr * 128:(pr + 1) * 128], identity=identb[:]), reads=[ytm[1], k_identb], writes=[ptq[1]])
            fw.op("act", lambda e: e.activation(out=R2[:, 2:6, n * 128:(n + 1) * 128], in_=ptq[0][:], func=AF.Copy), reads=[ptq[1]],
                  writes=[k_mx[2 + pr][n // 4] for pr in range(4)])

        r_head(0)
        r_mid(0)
        for n in range(NTB):
            if n + 1 < NTB:
                r_head(n + 1)
            r_tail(n)
            if n + 1 < NTB:
                r_mid(n + 1)
        fw.release(m)

    def phase_CONV(l):
        m = fw.mark()
        PAD = 30
        hg = fw.sb([128, 2, PAD + T], BF16); k_hg = [[fw.trk() for _ in range(9)] for _ in range(2)]
        dw31 = fw.sb([128, 256], F32); k_dw31 = fw.trk()
        fw.op("pool", lambda e: e.memset(dw31[:], 0.0), writes=[k_dw31])
        fw.dma("sp", dw31[0:31, :], cdw_w[l, :, :], writes=[k_dw31], slot=k_dw31)
        dwb16 = fw.sb([128, 256], BF16); k_dwb16 = fw.trk()
        fw.op("dve", lambda e: e.tensor_copy(out=dwb16[:], in_=dw31[:]), reads=[k_dw31], writes=[k_dwb16])
        wcol = fw.sb([128, 2, 31], F32); k_wcol = fw.trk()
        diagw = fw.sb([128, 2, 31, 128], BF16); k_diagw = fw.trk()
        cols = {}
        for name, src in (("ba", cpw_b[l, 0:256]), ("bg", cpw_b[l, 256:512]), ("dwb", cdw_b[l, :]), ("lng", cln_g[l, :]), ("lnb", cln_b[l, :])):
            t = fw.sb([128, 2], F32); k = fw.trk()
            load_cols(t[:], k, src, 2)
            cols[name] = (t, k)
        fw.op("dve", lambda e: e.tensor_scalar(out=cols["bg"][0][:], in0=cols["bg"][0][:], scalar1=-1.0, scalar2=None, op0=ALU.mult), reads=[cols["bg"][1]], writes=[cols["bg"][1]])
        pw = [(fw.ps([128, 512], F32), fw.trk()) for _ in range(2)]
        ptw = (fw.ps([128, 2, 128], BF16), fw.trk())
        for cc in range(2):
            fw.op("pe", lambda e, cc=cc: e.transpose(out=ptw[0][:, cc, :], in_=dwb16[:, cc * 128:(cc + 1) * 128], identity=identb[:]),
                  reads=[k_dwb16, k_identb], writes=[ptw[1]])
        fw.op("dve", lambda e: e.tensor_copy(out=wcol[:], in_=ptw[0][:, :, 0:31]), reads=[ptw[1]], writes=[k_wcol])
        for cc in range(2):
            for j in range(31):
                fw.op("dve", lambda e, cc=cc, j=j: e.tensor_scalar(out=diagw[:, cc, j, :], in0=identf[:], scalar1=wcol[:, cc, j:j + 1], scalar2=None, op0=ALU.mult),
                      reads=[k_identf, k_wcol], writes=[k_diagw])
        for cc in range(2):
            fw.op("pool", lambda e, cc=cc: e.memset(hg[:, cc, 0:PAD], 0.0), writes=[k_hg[cc][0]])
        if CONV_STOP < 2:
            fw.release(m)
            return
        wts = [(fw.sb([128, 8, 128], BF16), fw.trk()) for _ in range(2)]
        pa = [(fw.ps([128, 512], F32), fw.trk()) for _ in range(2)]
        pg = pw
        sg = [(fw.sb([128, 512], F32), fw.trk()) for _ in range(2)]
        it = 0
        for cc in range(2):
            fw.dma("pool", wts[0][0][:], w_in[l, :, 2816 + cc * 128:2816 + (cc + 1) * 128].rearrange("(dc p) c -> p dc c", p=128), writes=[wts[0][1]], slot=wts[0][1])
            fw.dma("pool", wts[1][0][:], w_in[l, :, 3072 + cc * 128:3072 + (cc + 1) * 128].rearrange("(dc p) c -> p dc c", p=128), writes=[wts[1][1]], slot=wts[1][1])
            for tt in range(8):
                a_, g_, s_ = pa[it % 2], pg[it % 2], sg[it % 2]
                it += 1
                for dc in range(8):
                    fw.op("pe", lambda e, dc=dc: e.matmul(a_[0][:], lhsT=wts[0][0][:, dc, :], rhs=R1[:, dc, tt * 512:(tt + 1) * 512], start=(dc == 0), stop=(dc == 7)),
                          reads=[wts[0][1]] + k_hT[tt * 4:(tt + 1) * 4], writes=[a_[1]])
                for dc in range(8):
                    fw.op("pe", lambda e, dc=dc: e.matmul(g_[0][:], lhsT=wts[1][0][:, dc, :], rhs=R1[:, dc, tt * 512:(tt + 1) * 512], start=(dc == 0), stop=(dc == 7)),
                          reads=[wts[1][1]] + k_hT[tt * 4:(tt + 1) * 4], writes=[g_[1]])
                sigmoid_from(s_[0][:], g_[0][:], [g_[1], cols["bg"][1]], [s_[1]], nbias=cols["bg"][0][:, cc:cc + 1])
                fw.op("dve", lambda e: e.scalar_tensor_tensor(out=hg[:, cc, PAD + tt * 512:PAD + (tt + 1) * 512], in0=a_[0][:], scalar=cols["ba"][0][:, cc:cc + 1], in1=s_[0][:], op0=ALU.add, op1=ALU.mult),
                      reads=[a_[1], cols["ba"][1], s_[1]], writes=[k_hg[cc][1 + tt]])
        if CONV_STOP < 3:
            fw.release(m)
            return
        pc = [(fw.ps([128, 512], F32), fw.trk()) for _ in range(2)]
        yc = [(fw.sb([128, 512], F32), fw.trk()) for _ in range(2)]
        ysq = [(fw.sb([128, 512], BF16), fw.trk()) for _ in range(2)]
        ycb = [(fw.sb([128, 512], BF16), fw.trk()) for _ in range(2)]
        mean = (fw.sb([128, 512], F32), fw.trk()); var = (fw.sb([128, 512], F32), fw.trk())
        tt1 = (fw.sb([128, 512], F32), fw.trk()); tt2 = (fw.sb([128, 512], F32), fw.trk())
        for tt in range(8):
            for cc in range(2):
                rd = [k_diagw, k_hg[cc][1 + tt], k_hg[cc][tt]]
                for j in range(31):
                    fw.op("pe", lambda e, cc=cc, j=j: e.matmul(pc[cc][0][:], lhsT=diagw[:, cc, j, :], rhs=hg[:, cc, tt * 512 + j:tt * 512 + j + 512], start=(j == 0), stop=(j == 30)),
                          reads=rd, writes=[pc[cc][1]])
                fw.op("dve", lambda e, cc=cc: e.tensor_scalar(out=yc[cc][0][:], in0=pc[cc][0][:], scalar1=cols["dwb"][0][:, cc:cc + 1], scalar2=None, op0=ALU.add), reads=[pc[cc][1], cols["dwb"][1]], writes=[yc[cc][1]])
                fw.op("act", lambda e, cc=cc: e.activation(out=ysq[cc][0][:], in_=yc[cc][0][:], func=AF.Square), reads=[yc[cc][1]], writes=[ysq[cc][1]])
                fw.op("act", lambda e, cc=cc: e.activation(out=ycb[cc][0][:], in_=yc[cc][0][:], func=AF.Copy), reads=[yc[cc][1]], writes=[ycb[cc][1]])
            if CONV_STOP < 4:
                continue
            for cc in range(2):
                fw.op("pe", lambda e, cc=cc: e.matmul(pw[0][0][:], lhsT=onesf[:], rhs=ycb[cc][0][:], start=(cc == 0), stop=(cc == 1)), reads=[k_onesf, ycb[cc][1]], writes=[pw[0][1]])
            for cc in range(2):
                fw.op("pe", lambda e, cc=cc: e.matmul(pw[1][0][:], lhsT=onesf[:], rhs=ysq[cc][0][:], start=(cc == 0), stop=(cc == 1)), reads=[k_onesf, ysq[cc][1]], writes=[pw[1][1]])
            if CONV_STOP < 5:
                continue
            fw.op("act", lambda e: e.activation(out=mean[0][:], in_=pw[0][0][:], func=AF.Copy), reads=[pw[0][1]], writes=[mean[1]])
            fw.op("dve", lambda e: e.tensor_tensor(out=var[0][:], in0=mean[0][:], in1=mean[0][:], op=ALU.mult), reads=[mean[1]], writes=[var[1]])
            fw.op("dve", lambda e: e.tensor_tensor(out=var[0][:], in0=pw[1][0][:], in1=var[0][:], op=ALU.subtract), reads=[pw[1][1], var[1]], writes=[var[1]])
            rstd_from("act", var[0][:], var[0][:], 1.0, [var[1]], [var[1]])
            for cc in range(2):
                fw.op("dve", lambda e, cc=cc: e.tensor_tensor(out=tt1[0][:], in0=yc[cc][0][:], in1=mean[0][:], op=ALU.subtract), reads=[yc[cc][1], mean[1]], writes=[tt1[1]])
                fw.op("dve", lambda e, cc=cc: e.tensor_tensor(out=tt1[0][:], in0=tt1[0][:], in1=var[0][:], op=ALU.mult), reads=[tt1[1], var[1]], writes=[tt1[1]])
                fw.op("dve", lambda e, cc=cc: e.tensor_scalar(out=tt1[0][:], in0=tt1[0][:], scalar1=cols["lng"][0][:, cc:cc + 1], scalar2=cols["lnb"][0][:, cc:cc + 1], op0=ALU.mult, op1=ALU.add),
                      reads=[tt1[1], cols["lng"][1], cols["lnb"][1]], writes=[tt1[1]])
                sigmoid_from(tt2[0][:], tt1[0][:], [tt1[1]], [tt2[1]])
                fw.op("pool", lambda e, cc=cc: e.tensor_tensor(out=R2[:, 6 + cc, tt * 512:(tt + 1) * 512], in0=tt1[0][:], in1=tt2[0][:], op=ALU.mult),
                      reads=[tt1[1], tt2[1]], writes=[k_mx[6 + cc][tt]])
        fw.release(m)

    def phase_C1(l, xin, k_xin, xmid, k_xmid, k_w1):
        m = fw.mark()
        wo = fw.sb([128, 8, D], BF16); k_wo = [fw.trk() for _ in range(2)]
        for i in range(2):
            fw.dma("pool", wo[:, :, i * 512:(i + 1) * 512], w_out[l, :, i * 512:(i + 1) * 512].rearrange("(cc p) d -> p cc d", p=128), writes=[k_wo[i]], slot=k_wo[i])
        for i in range(8):
            fw.dma("pool", R1[:, :, i * 512:(i + 1) * 512], w_ff1[l, :, i * 512:(i + 1) * 512].rearrange("(dc p) f -> p dc f", p=128), writes=[k_w1[i]], slot=k_w1[i])
        xts = [(fw.sb([128, D], F32), fw.trk()) for _ in range(3)]
        xos = [(fw.sb([128, D], F32), fw.trk()) for _ in range(3)]
        py = [(fw.ps([128, D], F32), fw.trk()) for _ in range(2)]
        for tb in range(NTB):
            xt, k_xt = xts[tb % 3]; xo, k_xo = xos[tb % 3]; p_, k_p = py[tb % 2]
            fw.dma("sp", xt[:], xin[tb * 128:(tb + 1) * 128, :], reads=[k_xin], writes=[k_xt], slot=k_xt)
            for half in range(2):
                for cc in range(8):
                    fw.op("pe", lambda e, cc=cc, half=half: e.matmul(p_[:, half * 512:(half + 1) * 512], lhsT=R2[:, cc, tb * 128:(tb + 1) * 128], rhs=wo[:, cc, half * 512:(half + 1) * 512], start=(cc == 0), stop=(cc == 7)),
                          reads=[k_mx[cc][tb // 4], k_wo[half]], writes=[k_p])
            for half in range(2):
                fw.op("dve", lambda e, half=half: e.tensor_tensor(out=xo[:, half * 512:(half + 1) * 512], in0=p_[:, half * 512:(half + 1) * 512], in1=xt[:, half * 512:(half + 1) * 512], op=ALU.add),
                      reads=[k_p, k_xt], writes=[k_xo])
            fw.dma("sp", xmid[tb * 128:(tb + 1) * 128, :], xo[:], reads=[k_xo], writes=[k_xmid], slot=k_xo)
        fw.release(m)

    def phase_C2(l, xmid, k_xmid, xout, k_xout, k_w1):
        m = fw.mark()
        W1 = R1
        W2 = R2[:].rearrange("p c t -> p (c t)").rearrange("p (fc d) -> p fc d", d=D)
        k_w2 = [fw.trk() for _ in range(8)]
        for i in range(8):
            fw.dma("pool", W2[:, i * 4:(i + 1) * 4, :], w_ff2[l, i * 512:(i + 1) * 512, :].rearrange("(fc p) d -> p fc d", p=128), writes=[k_w2[i]], slot=k_w2[i])
        gt = fw.sb([128, 8], F32); k_gt = fw.trk()
        load_cols(gt[:], k_gt, mlp_g[l, :], 8)
        TT = 256
        xts = [(fw.sb([128, 2, D], F32), [fw.trk(), fw.trk()]) for _ in range(2)]
        xos = [(fw.sb([128, 2, D], F32), [fw.trk(), fw.trk()]) for _ in range(2)]
        res = (fw.sb([128, D], BF16), fw.trk(), fw.sb([128, 2], F32), fw.trk(),
               fw.sb([128, D], BF16), fw.trk(), fw.ps([128, 8, 128], BF16), fw.trk())
        h2T = [(fw.sb([128, 8, TT], BF16), [fw.trk(), fw.trk()]) for _ in range(2)]
        py = [(fw.ps([128, D], F32), fw.trk()) for _ in range(2)]
        pf = [(fw.ps([128, 512], F32), fw.trk()) for _ in range(3)]
        rl = [(fw.sb([128, TT], F32), fw.trk()) for _ in range(3)]
        fb = [(fw.sb([128, TT], BF16), fw.trk()) for _ in range(3)]
        for ti in range(T // TT):
            xt, k_xt = xts[ti % 2]; xo, k_xo = xos[ti % 2]; hT2, k_h2 = h2T[ti % 2]
            for b2 in range(2):
                tb = ti * 2 + b2
                fw.dma("sp", xt[:, b2, :], xmid[tb * 128:(tb + 1) * 128, :], reads=[k_xmid], writes=[k_xt[b2]], slot=k_xt[b2])
                norm_transpose_block(xt[:, b2, :], k_xt[b2], gt, k_gt, res, hT2[:, :, b2 * 128:(b2 + 1) * 128], k_h2[b2])

            def stF(fc):
                p_, k_p = pf[fc % 3]
                for dc in range(8):
                    fw.op("pe", lambda e, dc=dc: e.matmul(p_[:, 0:TT], lhsT=W1[:, dc, fc * 128:(fc + 1) * 128], rhs=hT2[:, dc, :], start=(dc == 0), stop=(dc == 7)),
                          reads=[k_w1[fc // 4]] + k_h2, writes=[k_p])
                r_, k_r = rl[fc % 3]; f_, k_f = fb[fc % 3]
                fw.op("act", lambda e: e.activation(out=r_[:], in_=p_[:, 0:TT], func=AF.Relu), reads=[k_p], writes=[k_r])
                fw.op("pool", lambda e: e.tensor_tensor(out=f_[:], in0=r_[:], in1=r_[:], op=ALU.mult), reads=[k_r], writes=[k_f])

            def stY(fc):
                f_, k_f = fb[fc % 3]
                for b2 in range(2):
                    for half in range(2):
                        fw.op("pe", lambda e, b2=b2, half=half: e.matmul(py[b2][0][:, half * 512:(half + 1) * 512], lhsT=f_[:, b2 * 128:(b2 + 1) * 128], rhs=W2[:, fc, half * 512:(half + 1) * 512], start=(fc == 0), stop=(fc == 31)),
                              reads=[k_f, k_w2[fc // 4]], writes=[py[b2][1]])

            for i in range(32 + 2):
                if i < 32:
                    stF(i)
                if 0 <= i - 2 < 32:
                    stY(i - 2)
            for b2 in range(2):
                tb = ti * 2 + b2
                for half in range(2):
                    fw.op("dve", lambda e, b2=b2, half=half: e.tensor_tensor(out=xo[:, b2, half * 512:(half + 1) * 512], in0=py[b2][0][:, half * 512:(half + 1) * 512], in1=xt[:, b2, half * 512:(half + 1) * 512], op=ALU.add),
                          reads=[py[b2][1], k_xt[b2]], writes=[k_xo[b2]])
                fw.dma("sp", xout[tb * 128:(tb + 1) * 128, :], xo[:, b2, :], reads=[k_xo[b2]], writes=[k_xout], slot=k_xo[b2])
        fw.release(m)

    bufs = {0: (x_in, k_dram["x"], s1, k_dram["s1"], s0, k_dram["s0"]),
            1: (s0, k_dram["s0"], s1, k_dram["s1"], y_out, k_dram["y"])}
    if nlayers == 1:
        bufs[0] = (x_in, k_dram["x"], s1, k_dram["s1"], y_out, k_dram["y"])
    for l in range(nlayers):
        xin, k_xin, xmid, k_xmid, xout, k_xout = bufs[l]
        phase_A(l, xin, k_xin)
        if dbg == "hT":
            break
        for hp in range(2):
            if "sb" not in skip:
                phase_SB(l, hp)
        if dbg in ("sb", "sbproj"):
            break
        if "ret" not in skip:
            phase_RET(l)
        if dbg == "ret":
            break
        if "conv" not in skip:
            phase_CONV(l)
        if dbg == "mixed":
            break
        k_w1 = [fw.trk() for _ in range(8)]
        phase_C1(l, xin, k_xin, xmid, k_xmid, k_w1)
        if dbg == "c1":
            break
        phase_C2(l, xmid, k_xmid, xout, k_xout, k_w1)
    if dbg in ("hT", "sb", "ret", "mixed", "sbproj"):
        src = R1 if dbg == "hT" else R2
        m = fw.mark()
        st = [(fw.sb([128, 2048], F32), fw.trk()) for _ in range(2)]
        i = 0
        for c in dump_chunks:
            for hf in range(2):
                s_, k_s = st[i % 2]; i += 1
                fw.op("dve", lambda e: e.tensor_copy(out=s_[:], in_=src[:, c, hf * 2048:(hf + 1) * 2048]), reads=[], writes=[k_s])
                fw.dma("sp", dbg_out[:, c, hf * 2048:(hf + 1) * 2048], s_[:], reads=[k_s], writes=[k_dram["dbg"]], slot=k_s)
        fw.release(m)
    fw.barrier()
    return fw


def kernel(**inputs):
    global _CONSTS
    if _CONSTS is None:
        _CONSTS = _consts()
    if "nc" not in _NC_CACHE:
        p1 = build()
        _NC_CACHE["nc"] = build(needed=p1.waited)
    fw = _NC_CACHE["nc"]
    x = np.ascontiguousarray(np.asarray(inputs["x"], dtype=np.float32))
    shared = {k: np.ascontiguousarray(np.asarray(v, dtype=np.float32)) for k, v in inputs.items() if k != "x"}
    shared.update(_CONSTS)
    in_maps = []
    for b in range(8):
        mp = dict(shared)
        mp["x"] = x[b]
        in_maps.append(mp)
    res = run_bass_kernel_spmd(fw.nc, in_maps, core_ids=list(range(8)))
    return np.stack([np.asarray(r["y"], dtype=np.float32) for r in res.results], axis=0)
```
